# Optimizing a Trainium2 kernel written in Bass

```python
import math
import jax
import jax.numpy as jnp
from jax import lax
import numpy as np

D_MODEL = 2048
BATCH = 4
SEQ = 2048
DEPTH = 4

GRID_W = 64
CTX_LEN = 256
EPS = 1e-6
MIX_W = D_MODEL // 2

RET_DK = 128
RET_HEADS = MIX_W // RET_DK
RET_CHUNK = 128
ROPE_BASE = 10000.0

SSD_HD = 64
SSD_HEADS = MIX_W // SSD_HD
SSD_GROUPS = 4
SSD_STATE = 128
SSD_CHUNK = 128
CONV_W = 4

LRU_BLOCKS = 8
LRU_C = 8.0

HG_DK = 128
HG_HEADS = MIX_W // HG_DK
HG_CHUNK = 64

N_BRANCH = 4
FFN_DIM = 5632
N_EXPERTS = 8
TOP_K = 2
EXPERT_DIM = 4096
N_MOD = 6

PROJ_LAYOUT = (
    ('ret_q', MIX_W), ('ret_k', MIX_W), ('ret_v', MIX_W), ('ret_g', MIX_W),
    ('ssd_z', MIX_W), ('ssd_xbc', MIX_W + 2 * SSD_GROUPS * SSD_STATE), ('ssd_dt', 2 * SSD_HEADS),
    ('lru_x', MIX_W), ('lru_y', MIX_W),
    ('hg_q', MIX_W), ('hg_f', 2 * MIX_W), ('hg_i', MIX_W), ('hg_g', MIX_W),
    ('gates', N_BRANCH * D_MODEL),
)
PROJ_NAMES = tuple(name for name, _ in PROJ_LAYOUT)
PROJ_SPLITS = tuple(sum(w for _, w in PROJ_LAYOUT[:i + 1]) for i in range(len(PROJ_LAYOUT) - 1))
PROJ_W = sum(w for _, w in PROJ_LAYOUT)

kernel_name = 'hybrid_prefix_flow_trunk'

F32 = jnp.float32


def rms_norm(x, w=None):
    xf = x.astype(F32)
    y = xf * lax.rsqrt(jnp.mean(xf * xf, axis=-1, keepdims=True) + EPS)
    if w is not None:
        y = y * w.astype(F32)
    return y.astype(x.dtype)


def group_rms_norm(x, groups, w):
    shp = x.shape
    return rms_norm(x.reshape(*shp[:-1], groups, shp[-1] // groups)).reshape(shp) * w


def per_token(v_ctx, v_lat, n_ctx, start, total):
    is_ctx = (jnp.arange(start, total) < n_ctx)[None, :, None]
    return jnp.where(is_ctx, v_ctx[:, None, :], v_lat[:, None, :])


def axial_rope_tables(rows):
    r, col = jnp.meshgrid(jnp.arange(rows), jnp.arange(GRID_W), indexing='ij')
    n_freq = RET_DK // 4
    freqs = ROPE_BASE ** (-jnp.arange(n_freq, dtype=F32) / n_freq)
    ang = jnp.concatenate([r.reshape(-1, 1) * freqs, col.reshape(-1, 1) * freqs], axis=-1)
    return jnp.cos(ang), jnp.sin(ang)


def apply_rope(z, cos, sin):
    z1, z2 = jnp.split(z.astype(F32), 2, axis=-1)
    c, s = cos[None, :, None], sin[None, :, None]
    return jnp.concatenate([z1 * c - z2 * s, z1 * s + z2 * c], axis=-1)


def dwconv(u, w, b):
    left = (CONV_W - 1) // 2
    out = lax.conv_general_dilated(u, w[:, None, :].astype(u.dtype), (1,), [(left, CONV_W - 1 - left)],
                                   dimension_numbers=('NWC', 'WIO', 'NWC'), feature_group_count=u.shape[-1])
    return out + b


def masked_decay(diff, mask):
    return jnp.where(mask, jnp.exp(jnp.where(mask, diff, 0.0)), 0.0)


def chunk_scan_scalar(q, k, v, log_a, s0, chunk):
    b, t, h, dk = q.shape
    dv = v.shape[-1]
    n = t // chunk
    q, k, v = (z.astype(F32).reshape(b, n, chunk, h, z.shape[-1]) for z in (q, k, v))
    cum = jnp.cumsum(log_a.astype(F32).reshape(b, n, chunk, h), axis=2)
    causal = jnp.tril(jnp.ones((chunk, chunk), dtype=bool))[None, None, :, :, None]
    decay = masked_decay(cum[:, :, :, None, :] - cum[:, :, None, :, :], causal)
    scores = jnp.einsum('bnihk,bnjhk->bnijh', q, k) * decay
    o = jnp.einsum('bnijh,bnjhv->bnihv', scores, v)
    contrib = jnp.einsum('bnjhk,bnjh,bnjhv->bnhkv', k, jnp.exp(cum[:, :, -1:] - cum), v)
    if s0 is None:
        s0 = jnp.zeros((b, h, dk, dv), F32)

    def step(s, inp):
        a_tot, con = inp
        return a_tot[:, :, None, None] * s + con, s

    s_fin, s_in = lax.scan(step, s0, (jnp.moveaxis(jnp.exp(cum[:, :, -1]), 1, 0), jnp.moveaxis(contrib, 1, 0)))
    o = o + jnp.einsum('bnihk,bnih,bnhkv->bnihv', q, jnp.exp(cum), jnp.moveaxis(s_in, 0, 1))
    return o.reshape(b, t, h, dv), s_fin


def chunk_scan_vector(q, k, v, log_f, s0, chunk):
    b, t, h, dk = q.shape
    dv = v.shape[-1]
    n = t // chunk

    def blocks(z):
        return jnp.moveaxis(z.astype(F32).reshape(b, n, chunk, h, z.shape[-1]), 1, 0)

    causal = jnp.tril(jnp.ones((chunk, chunk), dtype=bool))[None, :, :, None, None]
    if s0 is None:
        s0 = jnp.zeros((b, h, dk, dv), F32)

    def step(s, inp):
        qc, kc, vc, gc = inp
        cum = jnp.cumsum(gc, axis=1)
        decay = masked_decay(cum[:, :, None] - cum[:, None], causal)
        scores = jnp.einsum('bihk,bjhk,bijhk->bijh', qc, kc, decay)
        o = jnp.einsum('bijh,bjhv->bihv', scores, vc) + jnp.einsum('bihk,bhkv->bihv', qc * jnp.exp(cum), s)
        last = cum[:, -1]
        s_new = jnp.exp(last)[..., None] * s + jnp.einsum('bjhk,bjhv->bhkv', kc * jnp.exp(last[:, None] - cum), vc)
        return s_new, o

    s_fin, o = lax.scan(step, s0, tuple(blocks(z) for z in (q, k, v, log_f)))
    return jnp.moveaxis(o, 0, 1).reshape(b, t, h, dv), s_fin


def lru_scan(log_a, b, h0):
    a = jnp.exp(log_a.astype(F32))

    def combine(left, right):
        return left[0] * right[0], right[0] * left[1] + right[1]

    a_cum, h = lax.associative_scan(combine, (a, b.astype(F32)), axis=1)
    if h0 is not None:
        h = h + a_cum * h0[:, None]
    return h, h[:, -1]


def bidirectional(scan, ctx_fwd, ctx_bwd, lat_fwd, lat_bwd):
    flip = lambda args: tuple(jnp.flip(a, axis=1) for a in args)
    oc_f, sc_f = scan(ctx_fwd, None)
    ol_f, _ = scan(lat_fwd, sc_f)
    oc_b, sc_b = scan(flip(ctx_bwd), None)
    ol_b, _ = scan(flip(lat_bwd), sc_b)
    return oc_f + jnp.flip(oc_b, axis=1), ol_f + jnp.flip(ol_b, axis=1)


def retention(pc, pl, cos, sin):
    log_gamma = jnp.log1p(-jnp.exp2(-5.0 - jnp.arange(RET_HEADS, dtype=F32)))

    def prep(p, rotate):
        b, t, _ = p['ret_q'].shape
        q = p['ret_q'].reshape(b, t, RET_HEADS, RET_DK)
        k = p['ret_k'].reshape(b, t, RET_HEADS, RET_DK)
        if rotate:
            q, k = apply_rope(q, cos, sin), apply_rope(k, cos, sin)
        v = p['ret_v'].reshape(b, t, RET_HEADS, RET_DK)
        return (q, k * RET_DK ** -0.5, v, jnp.broadcast_to(log_gamma, (b, t, RET_HEADS)))

    ac, al = prep(pc, False), prep(pl, True)
    scan = lambda args, s0: chunk_scan_scalar(*args, s0, RET_CHUNK)
    oc, ol = bidirectional(scan, ac, ac, al, al)

    def post(o, p):
        b, t = o.shape[:2]
        return rms_norm(o).reshape(b, t, MIX_W) * jax.nn.silu(p['ret_g'])

    return post(oc, pc), post(ol, pl)


def ssd(pc, pl, conv_w, conv_b, a_log, dt_bias, d_skip, norm_w):
    a = -jnp.exp(a_log.astype(F32))
    rep = SSD_HEADS // SSD_GROUPS

    def prep(p):
        b, t, _ = p['ssd_z'].shape
        xbc = jax.nn.silu(dwconv(p['ssd_xbc'], conv_w, conv_b))
        xs, bm, cm = jnp.split(xbc, [MIX_W, MIX_W + SSD_GROUPS * SSD_STATE], axis=-1)
        xs = xs.reshape(b, t, SSD_HEADS, SSD_HD)
        bm = jnp.repeat(bm.reshape(b, t, SSD_GROUPS, SSD_STATE), rep, axis=2)
        cm = jnp.repeat(cm.reshape(b, t, SSD_GROUPS, SSD_STATE), rep, axis=2)
        dt = jax.nn.softplus(p['ssd_dt'].astype(F32).reshape(b, t, 2, SSD_HEADS) + dt_bias)
        dirs = tuple((cm, bm * dt[:, :, d, :, None], xs, dt[:, :, d] * a[d]) for d in range(2))
        return dirs, xs

    (cf, cb), xc = prep(pc)
    (lf, lb), xl = prep(pl)
    scan = lambda args, s0: chunk_scan_scalar(*args, s0, SSD_CHUNK)
    oc, ol = bidirectional(scan, cf, cb, lf, lb)

    def post(o, xs, p):
        b, t = o.shape[:2]
        y = (o + d_skip[:, None] * xs).reshape(b, t, MIX_W)
        return group_rms_norm(y * jax.nn.silu(p['ssd_z']), SSD_GROUPS, norm_w)

    return post(oc, xc, pc), post(ol, xl, pl)


def rglru(pc, pl, conv_w, conv_b, wa, ba, wx, bx, lam):
    log_sig = -jax.nn.softplus(-lam.astype(F32))

    def blockdiag(z, w):
        b, t, _ = z.shape
        return jnp.einsum('btgi,gij->btgj', z.reshape(b, t, LRU_BLOCKS, -1), w).reshape(b, t, MIX_W)

    def prep(p):
        xc = dwconv(p['lru_x'], conv_w, conv_b)

        def direction(d):
            r = jax.nn.sigmoid(blockdiag(xc, wa[d]) + ba[d])
            i = jax.nn.sigmoid(blockdiag(xc, wx[d]) + bx[d])
            log_a = LRU_C * r.astype(F32) * log_sig[d]
            return (log_a, jnp.sqrt(-jnp.expm1(2.0 * log_a)) * (i * xc))

        return direction(0), direction(1)

    cf, cb = prep(pc)
    lf, lb = prep(pl)
    scan = lambda args, s0: lru_scan(*args, s0)
    oc, ol = bidirectional(scan, cf, cb, lf, lb)
    return oc * jax.nn.gelu(pc['lru_y']), ol * jax.nn.gelu(pl['lru_y'])


def hgrn2(pc, pl, lb, norm_w):
    def prep(p):
        b, t, _ = p['hg_q'].shape
        hd = lambda z: z.reshape(b, t, HG_HEADS, HG_DK)
        q = hd(jax.nn.silu(p['hg_q']))
        v = hd(p['hg_i'])
        f = p['hg_f'].astype(F32).reshape(b, t, 2, MIX_W)
        log_f = jnp.log(lb + (1.0 - lb) * jax.nn.sigmoid(f))
        k = (1.0 - lb) * jax.nn.sigmoid(-f)
        return tuple((q, hd(k[:, :, d]), v, hd(log_f[:, :, d])) for d in range(2))

    cf, cb = prep(pc)
    lf, lbk = prep(pl)
    scan = lambda args, s0: chunk_scan_vector(*args, s0, HG_CHUNK)
    oc, ol = bidirectional(scan, cf, cb, lf, lbk)

    def post(o, p):
        b, t = o.shape[:2]
        return group_rms_norm(o.reshape(b, t, MIX_W), HG_HEADS, norm_w) * jax.nn.silu(p['hg_g'])

    return post(oc, pc), post(ol, pl)


def merge(branches, gates, wb, wo):
    g = jax.nn.sigmoid(gates.reshape(*gates.shape[:-1], N_BRANCH, D_MODEL))
    y = sum(g[..., n, :] * (branches[n] @ wb[n]) for n in range(N_BRANCH))
    return y @ wo


def swiglu(x, wgu, w2):
    gate, up = jnp.split(x @ wgu, 2, axis=-1)
    return (jax.nn.silu(gate) * up) @ w2


def moe(x, router_w, wgu, w2):
    b, t, d = x.shape
    xf = x.reshape(b * t, d)
    logits = (xf @ router_w).astype(F32)
    top_val, top_idx = lax.top_k(logits, TOP_K)
    weights = jax.nn.softmax(top_val, axis=-1)
    gate = jnp.einsum('nk,nke->ne', weights, jax.nn.one_hot(top_idx, N_EXPERTS, dtype=F32)).astype(xf.dtype)
    y = sum(gate[:, e:e + 1] * swiglu(xf, wgu[e], w2[e]) for e in range(N_EXPERTS))
    return y.reshape(b, t, d)


def setup_inputs(seed: int = 0) -> dict:
    key = jax.random.key(seed)
    k = jax.random.split(key, 40)
    n_dense, n_moe = (DEPTH + 1) // 2, DEPTH // 2
    xbc_w = MIX_W + 2 * SSD_GROUPS * SSD_STATE
    bw = MIX_W // LRU_BLOCKS

    def nrm(i, shape, scale):
        return jax.random.normal(k[i], shape, F32) * scale

    def gain(i, shape):
        return 1.0 + nrm(i, shape, 0.02)

    a_init = jax.random.uniform(k[10], (DEPTH, 2, SSD_HEADS), F32, 1.0, 16.0)
    dt0 = jnp.exp(jax.random.uniform(k[11], (DEPTH, 2, SSD_HEADS), F32, math.log(1e-3), math.log(1e-1)))
    a_c = jax.random.uniform(k[20], (DEPTH, 2, MIX_W), F32, 0.9, 0.999)
    a_base = a_c ** (1.0 / LRU_C)
    return {
        'x': nrm(0, (BATCH, SEQ, D_MODEL), 1.0),
        'c': nrm(1, (BATCH, D_MODEL), 1.0),
        'ctx': nrm(2, (BATCH, CTX_LEN, D_MODEL), 1.0),
        'c_ctx': nrm(3, (D_MODEL,), 1.0),
        'ada_w': nrm(4, (DEPTH, D_MODEL, N_MOD * D_MODEL), 0.5 * D_MODEL ** -0.5),
        'ada_b': nrm(5, (DEPTH, N_MOD * D_MODEL), 0.01),
        'norm1_w': gain(6, (DEPTH, D_MODEL)),
        'norm2_w': gain(7, (DEPTH, D_MODEL)),
        'w_in': nrm(8, (DEPTH, D_MODEL, PROJ_W), D_MODEL ** -0.5),
        'ssd_conv_w': nrm(9, (DEPTH, CONV_W, xbc_w), CONV_W ** -0.5),
        'ssd_conv_b': nrm(12, (DEPTH, xbc_w), 0.01),
        'ssd_a_log': jnp.log(a_init),
        'ssd_dt_bias': dt0 + jnp.log(-jnp.expm1(-dt0)),
        'ssd_d': 1.0 + nrm(13, (DEPTH, SSD_HEADS), 0.1),
        'ssd_norm_w': gain(14, (DEPTH, MIX_W)),
        'lru_conv_w': nrm(15, (DEPTH, CONV_W, MIX_W), CONV_W ** -0.5),
        'lru_conv_b': nrm(16, (DEPTH, MIX_W), 0.01),
        'lru_wa': nrm(17, (DEPTH, 2, LRU_BLOCKS, bw, bw), bw ** -0.5),
        'lru_ba': nrm(18, (DEPTH, 2, MIX_W), 0.01),
        'lru_wx': nrm(19, (DEPTH, 2, LRU_BLOCKS, bw, bw), bw ** -0.5),
        'lru_bx': nrm(21, (DEPTH, 2, MIX_W), 0.01),
        'lru_lambda': jnp.log(a_base) - jnp.log1p(-a_base),
        'hg_lb_logits': 1.0 + nrm(22, (2, DEPTH, MIX_W), 0.1),
        'hg_norm_w': gain(23, (DEPTH, MIX_W)),
        'w_branch': nrm(24, (DEPTH, N_BRANCH, MIX_W, D_MODEL), MIX_W ** -0.5),
        'w_out': nrm(25, (DEPTH, D_MODEL, D_MODEL), D_MODEL ** -0.5),
        'ffn_wgu': nrm(26, (n_dense, D_MODEL, 2 * FFN_DIM), D_MODEL ** -0.5),
        'ffn_w2': nrm(27, (n_dense, FFN_DIM, D_MODEL), FFN_DIM ** -0.5),
        'router_w': nrm(28, (n_moe, D_MODEL, N_EXPERTS), D_MODEL ** -0.5),
        'moe_wgu': nrm(29, (n_moe, N_EXPERTS, D_MODEL, 2 * EXPERT_DIM), D_MODEL ** -0.5),
        'moe_w2': nrm(30, (n_moe, N_EXPERTS, EXPERT_DIM, D_MODEL), EXPERT_DIM ** -0.5),
        'final_norm_w': gain(31, (D_MODEL,)),
    }


def reference(x, c, ctx, c_ctx, ada_w, ada_b, norm1_w, norm2_w, w_in,
              ssd_conv_w, ssd_conv_b, ssd_a_log, ssd_dt_bias, ssd_d, ssd_norm_w,
              lru_conv_w, lru_conv_b, lru_wa, lru_ba, lru_wx, lru_bx, lru_lambda,
              hg_lb_logits, hg_norm_w, w_branch, w_out, ffn_wgu, ffn_w2,
              router_w, moe_wgu, moe_w2, final_norm_w):
    n_ctx, n_lat = ctx.shape[1], x.shape[1]
    total = n_ctx + n_lat
    rows = n_lat // GRID_W
    cos, sin = axial_rope_tables(rows)
    p_lb = jax.nn.softmax(hg_lb_logits.astype(F32), axis=1)
    lower_bounds = jnp.cumsum(p_lb, axis=1) - p_lb[:, :1]
    cond_lat = jax.nn.silu(c)
    cond_ctx = jax.nn.silu(c_ctx)[None]
    h = jnp.concatenate([ctx.astype(x.dtype), x], axis=1)
    for layer in range(DEPTH):
        start = n_ctx if layer == DEPTH - 1 else 0
        mc = jnp.split(cond_ctx @ ada_w[layer] + ada_b[layer], N_MOD, axis=-1)
        ml = jnp.split(cond_lat @ ada_w[layer] + ada_b[layer], N_MOD, axis=-1)
        xn = rms_norm(h, norm1_w[layer]) * (1.0 + per_token(mc[1], ml[1], n_ctx, 0, total)) \
            + per_token(mc[0], ml[0], n_ctx, 0, total)
        u = dict(zip(PROJ_NAMES, jnp.split(xn @ w_in[layer], PROJ_SPLITS, axis=-1)))
        pc = {name: val[:, :n_ctx] for name, val in u.items()}
        pl = {name: val[:, n_ctx:] for name, val in u.items()}
        branches = (
            retention(pc, pl, cos, sin),
            ssd(pc, pl, ssd_conv_w[layer], ssd_conv_b[layer], ssd_a_log[layer], ssd_dt_bias[layer],
                ssd_d[layer], ssd_norm_w[layer]),
            rglru(pc, pl, lru_conv_w[layer], lru_conv_b[layer], lru_wa[layer], lru_ba[layer],
                  lru_wx[layer], lru_bx[layer], lru_lambda[layer]),
            hgrn2(pc, pl, lower_bounds[:, layer], hg_norm_w[layer]),
        )
        br = [bl if start else jnp.concatenate([bc, bl], axis=1) for bc, bl in branches]
        mixed = merge(br, u['gates'][:, start:], w_branch[layer], w_out[layer])
        h = h[:, start:] + per_token(mc[2], ml[2], n_ctx, start, total) * mixed
        xn = rms_norm(h, norm2_w[layer]) * (1.0 + per_token(mc[4], ml[4], n_ctx, start, total)) \
            + per_token(mc[3], ml[3], n_ctx, start, total)
        if layer % 2 == 0:
            ffn = swiglu(xn, ffn_wgu[layer // 2], ffn_w2[layer // 2])
        else:
            ffn = moe(xn, router_w[layer // 2], moe_wgu[layer // 2], moe_w2[layer // 2])
        h = h + per_token(mc[5], ml[5], n_ctx, start, total) * ffn
    return rms_norm(h, final_norm_w)
```

```python
import numpy as np
import ml_dtypes
from contextlib import ExitStack
import concourse.bass as bass
import concourse.mybir as mybir
from concourse.bass_utils import run_bass_kernel_spmd

F32 = mybir.dt.float32
BF16 = mybir.dt.bfloat16
AF = mybir.ActivationFunctionType
ALU = mybir.AluOpType
AX = mybir.AxisListType

D = 2048
NB = 4
NCTX = 256
NLAT = 2048
T = NCTX + NLAT
DEPTH = 4
MIXW = 1024
KC = D // 128


class Sched:
    def __init__(self, nc, es, ring=8):
        self.nc = nc
        self.engs = {'pe': nc.tensor, 'act': nc.scalar, 'dve': nc.vector, 'pool': nc.gpsimd, 'sp': nc.sync}
        self.csem = {e: es.enter_context(nc.semaphore('c_' + e)) for e in ('pe', 'act', 'dve', 'pool')}
        self.ccnt = {e: 0 for e in self.csem}
        self.ring = ring
        self.dsem = {q: [es.enter_context(nc.semaphore('d_%s%d' % (q, i))) for i in range(ring)]
                     for q in ('sp', 'pool')}
        self.dcnt = {q: [0] * ring for q in self.dsem}
        self.dnext = {q: 0 for q in self.dsem}
        self.seen = {e: {} for e in self.engs}
        self.res = {}
        self.nwait = 0
        self.ninst = 0

    def _ensure(self, e, ev):
        name, sem, val, prod = ev
        if prod == 'pe' and e == 'pe':
            return
        if self.seen[e].get(name, 0) >= val:
            return
        self.engs[e].wait_ge(sem, val)
        self.nwait += 1
        self.seen[e][name] = val

    def _deps(self, e, reads, writes):
        for k in reads:
            r = self.res.get(k)
            if r is not None and r[0] is not None:
                self._ensure(e, r[0])
        for k in writes:
            r = self.res.get(k)
            if r is not None:
                if r[0] is not None:
                    self._ensure(e, r[0])
                for ev in r[1].values():
                    self._ensure(e, ev)

    def _record(self, ev, reads, writes):
        for k in reads:
            r = self.res.setdefault(k, [None, {}])
            old = r[1].get(ev[0])
            if old is None or old[2] < ev[2]:
                r[1][ev[0]] = ev
        for k in writes:
            self.res[k] = [ev, {}]

    def op(self, e, fn, reads=(), writes=()):
        self._deps(e, reads, writes)
        inst = fn(self.engs[e])
        self.ccnt[e] += 1
        inst.then_inc(self.csem[e], 1)
        self.ninst += 1
        ev = ('c_' + e, self.csem[e], self.ccnt[e], e)
        self._record(ev, reads, writes)
        return ev

    def dma(self, q, out, in_, reads=(), writes=(), **kw):
        i = self.dnext[q]
        self.dnext[q] = (i + 1) % self.ring
        sem = self.dsem[q][i]
        name = 'd_%s%d' % (q, i)
        if self.dcnt[q][i] > 0:
            self._ensure(q, (name, sem, self.dcnt[q][i], 'dma'))
        self._deps(q, reads, writes)
        self.engs[q].dma_start(out=out, in_=in_, **kw).then_inc(sem, 16)
        self.dcnt[q][i] += 16
        self.ninst += 1
        ev = (name, sem, self.dcnt[q][i], 'dma')
        self._record(ev, reads, writes)
        return ev

    def finish(self):
        for e in self.csem:
            if self.ccnt[e] > 0:
                self._ensure('sp', ('c_' + e, self.csem[e], self.ccnt[e], e))
        for q in self.dsem:
            for i in range(self.ring):
                if self.dcnt[q][i] > 0:
                    self._ensure('sp', ('d_%s%d' % (q, i), self.dsem[q][i], self.dcnt[q][i], 'dma'))


class Ctx:
    def __init__(self, name):
        self.nc = bass.Bass("TRN2", target_bir_lowering=False)
        self.es = ExitStack()
        self.S = Sched(self.nc, self.es)
        self.nps = 0
        self.psb = [self.es.enter_context(self.nc.psum_tensor('psb%d' % i, [128, 512], F32)) for i in range(7)]
        self.pbf = self.es.enter_context(self.nc.psum_tensor('pbf', [128, 1024], BF16))
        self.psi = 0
        self.nrot = 6
        self.pfx = ''
        self.stacks = [self.es]
        self.inputs = {}

    def sb(self, name, shape, dt=F32):
        return self.stacks[-1].enter_context(self.nc.sbuf_tensor(self.pfx + name, list(shape), dt))

    def din(self, name, shape, dt=F32, shared=False):
        nm = name if shared else self.pfx + name
        if nm not in self.inputs:
            self.inputs[nm] = self.nc.dram_tensor(nm, list(shape), dt, kind="ExternalInput").ap()
        return self.inputs[nm]

    def dint(self, name, shape, dt=F32):
        return self.nc.dram_tensor(name, list(shape), dt, kind="Internal").ap()

    def push(self):
        self.stacks.append(ExitStack())

    def pop(self):
        self.stacks.pop().close()

    def dout(self, name, shape, dt=F32):
        return self.nc.dram_tensor(name, list(shape), dt, kind="ExternalOutput").ap()

    def ps(self):
        i = self.psi
        self.psi = (i + 1) % self.nrot
        return self.psb[i], ('ps', i)

    def close(self):
        self.S.finish()
        self.es.close()
        return self.nc


def run(nc, in_maps):
    res = run_bass_kernel_spmd(nc, in_maps, core_ids=list(range(8)))
    return res.results


def emit_ada(C, layers):
    S = C.S
    C.pfx = 'ada_'
    C.push()
    NJ = 96
    nl = len(layers)
    w = C.din('w', [nl, D, NJ * 128])
    bia = C.din('b', [nl, 128, NJ])
    cond = C.din('cond', [128, KC * 2])
    modd = C.dint('modd', [nl, 128, NJ * 2])
    wt = [C.sb('wt%d' % i, [128, KC, 512]) for i in range(2)]
    bt = C.sb('bt', [128, NJ])
    ct = C.sb('ct', [128, KC * 2])
    cs = C.sb('cs', [128, KC, 2])
    ot = C.sb('ot', [128, NJ, 2])
    S.dma('sp', ct[:], cond, writes=['ct'])
    S.op('act', lambda e: e.activation(out=cs[:].rearrange('p k r -> p (k r)'), in_=ct[:], func=AF.Silu),
         reads=['ct'], writes=['cs'])
    for li in range(nl):
        S.dma('sp', bt[:], bia[li], writes=['bt'])
        wv = w[li].rearrange('(k p) c -> p k c', p=128)
        for blk in range(NJ // 4):
            b = blk % 2
            S.dma('sp', wt[b][:], wv[:, :, blk * 512:(blk + 1) * 512], writes=[('wt', b)])
            for jj in range(4):
                j = blk * 4 + jj
                pt, pk = C.ps()
                for kc in range(KC):
                    S.op('pe', lambda e, kc=kc, jj=jj, pt=pt, b=b: e.matmul(
                        pt[:, 0:2], wt[b][:, kc, jj * 128:(jj + 1) * 128], cs[:, kc, :],
                        start=(kc == 0), stop=(kc == KC - 1)),
                         reads=[('wt', b), 'cs'], writes=[pk])
                S.op('act', lambda e, j=j, pt=pt: e.activation(out=ot[:, j, :], in_=pt[:, 0:2], func=AF.Identity,
                                                               bias=bt[:, j:j + 1], scale=1.0),
                     reads=[pk, 'bt'], writes=['ot'])
        S.dma('sp', modd[li], ot[:].rearrange('p j r -> p (j r)'), reads=['ot'], writes=[('modd', li)])
    barrier(C)
    C.pop()
    return modd


def emit_consts(C):
    S = C.S
    C.ones = C.sb('ones', [128, 128])
    S.op('dve', lambda e: e.memset(C.ones[:], 1.0), writes=['ones'])
    C.ident_d = C.din('ident', [128, 128], shared=True)
    C.ident = C.sb('ident_s', [128, 128])
    S.dma('sp', C.ident[:], C.ident_d, writes=['ident'])


def emit_rmsnorm(C, tag, h_of, hkey, xn_of, xnkey, subs, scale_ap, shift_ap, tmp):
    S = C.S
    for (off, n, idx) in subs:
        pt, pk = C.ps()
        for kc in range(KC):
            b = kc % 2
            S.op('act', lambda e, kc=kc, b=b: e.activation(out=tmp['sq'][b][:, 0:n], in_=h_of(kc, off, n),
                                                           func=AF.Square),
                 reads=[hkey], writes=[(tag, 'sq', b)])
            S.op('pe', lambda e, kc=kc, b=b, pt=pt: e.matmul(pt[:, 0:n], C.ones[:], tmp['sq'][b][:, 0:n],
                                                             start=(kc == 0), stop=(kc == KC - 1)),
                 reads=[(tag, 'sq', b), 'ones'], writes=[pk])
        rs = tmp['rstd']
        S.op('act', lambda e, pt=pt: e.activation(out=rs[:, off:off + n], in_=pt[:, 0:n], func=AF.Sqrt,
                                                  bias=C.epsb[:, 0:1], scale=1.0 / D),
             reads=[pk, 'epsb'], writes=[(tag, 'rstd')])
        S.op('dve', lambda e: e.reciprocal(out=rs[:, off:off + n], in_=rs[:, off:off + n]),
             reads=[(tag, 'rstd')], writes=[(tag, 'rstd')])
        for kc in range(KC):
            b = kc % 2
            if shift_ap is None:
                S.op('dve', lambda e, kc=kc: e.scalar_tensor_tensor(
                    out=xn_of(kc, off, n), in0=h_of(kc, off, n), scalar=scale_ap(kc, idx),
                    in1=rs[:, off:off + n], op0=ALU.mult, op1=ALU.mult),
                     reads=[hkey, (tag, 'rstd'), 'mods'], writes=[xnkey])
            else:
                S.op('dve', lambda e, kc=kc, b=b: e.scalar_tensor_tensor(
                    out=tmp['tm'][b][:, 0:n], in0=h_of(kc, off, n), scalar=scale_ap(kc, idx),
                    in1=rs[:, off:off + n], op0=ALU.mult, op1=ALU.mult),
                     reads=[hkey, (tag, 'rstd'), 'mods'], writes=[(tag, 'tm', b)])
                S.op('act', lambda e, kc=kc, b=b: e.activation(
                    out=xn_of(kc, off, n), in_=tmp['tm'][b][:, 0:n], func=AF.Identity,
                    bias=shift_ap(kc, idx), scale=1.0),
                     reads=[(tag, 'tm', b), 'mods'], writes=[xnkey])


def emit_conv(C, tag, src, skey, dst, dkey, wap, wkey):
    S = C.S
    for (a, e_) in SEGS:
        S.op('dve', lambda e, a=a, e_=e_: e.tensor_scalar(out=dst[:, a:e_], in0=src[:, a:e_], scalar1=wap(1),
                                                          scalar2=wap(4), op0=ALU.mult, op1=ALU.add),
             reads=[skey, wkey], writes=[dkey])
        S.op('dve', lambda e, a=a, e_=e_: e.scalar_tensor_tensor(
            out=dst[:, a + 1:e_], in0=src[:, a:e_ - 1], scalar=wap(0), in1=dst[:, a + 1:e_],
            op0=ALU.mult, op1=ALU.add), reads=[skey, wkey, dkey], writes=[dkey])
        S.op('dve', lambda e, a=a, e_=e_: e.scalar_tensor_tensor(
            out=dst[:, a:e_ - 1], in0=src[:, a + 1:e_], scalar=wap(2), in1=dst[:, a:e_ - 1],
            op0=ALU.mult, op1=ALU.add), reads=[skey, wkey, dkey], writes=[dkey])
        S.op('dve', lambda e, a=a, e_=e_: e.scalar_tensor_tensor(
            out=dst[:, a:e_ - 2], in0=src[:, a + 2:e_], scalar=wap(3), in1=dst[:, a:e_ - 2],
            op0=ALU.mult, op1=ALU.add), reads=[skey, wkey, dkey], writes=[dkey])


def emit_eps(C):
    C.epsb = C.sb('epsb', [128, 1])
    C.S.op('dve', lambda e: e.memset(C.epsb[:], 1e-6), writes=['epsb'])


PASS_T = 576
SUBS_T = [(0, 256), (256, 320)]
FFN_DIM = 5632
EXP_DIM = 4096
NEXP = 8


def emit_token(C, layer, li, moe, final, h_src, h_src_key, h_dst, h_dst_key, brT, modd, yout, hdbg=None):
    S = C.S
    C.pfx = 'L%d_' % layer
    C.push()
    hT = h_src
    n1w_d = C.din('n1w', [128, KC])
    n2w_d = C.din('n2w', [128, KC])
    wg_d = C.din('wg', [KC, 128, KC * 4 * 128])
    wb_d = C.din('wb', [KC, 128, 4 * 8 * 128])
    wo_d = C.din('wo', [KC, 128, KC * 128])
    if moe:
        NBLK = NEXP * (EXP_DIM // 256)
        rw_d = C.din('rw', [128, KC * NEXP])
    else:
        NBLK = FFN_DIM // 256
    wgu_d = C.din('wgu', [NBLK, 128, KC * 2 * 256])
    w2_d = C.din('w2', [NBLK, 128, 2 * D])
    if final:
        fnw_d = C.din('fnw', [128, KC])

    hs = C.sb('hs', [128, KC, PASS_T])
    xn = C.sb('xn', [128, KC, PASS_T], BF16)
    yv = C.sb('yv', [128, KC, PASS_T], BF16)
    br = [C.sb('br%d' % n, [128, 8, PASS_T], BF16) for n in range(4)]
    wA = [C.sb('wA%d' % i, [128, KC * 512], BF16) for i in range(2)]
    wB = [C.sb('wB%d' % i, [128, 4096], BF16) for i in range(2)]
    act = [C.sb('act%d' % i, [128, 2, PASS_T], BF16) for i in range(2)]
    modt = C.sb('modt', [128, 96, 2])
    n1w = C.sb('n1w_s', [128, KC])
    n2w = C.sb('n2w_s', [128, KC])
    sc1 = C.sb('sc1', [128, KC, 2])
    sc2 = C.sb('sc2', [128, KC, 2])

    def ROW(idx):
        return 1 if idx == 0 else 0

    def MOD(m, kc, idx):
        return modt[:, m * 16 + kc, ROW(idx):ROW(idx) + 1]

    def SC(sc, kc, idx):
        return sc[:, kc, ROW(idx):ROW(idx) + 1]
    tmp = {'sq': [C.sb('sq%d' % i, [128, 448]) for i in range(2)],
           'tm': [C.sb('tm%d' % i, [128, 448]) for i in range(2)],
           'rstd': C.sb('rstd', [128, PASS_T])}
    sg = [C.sb('sg%d' % i, [128, 448]) for i in range(2)]
    yacc = C.sb('yacc', [128, 448])
    if moe:
        rw = C.sb('rw_s', [128, KC, NEXP], BF16)
        gbc = C.sb('gbc', [128, NEXP, PASS_T])
        lg = C.sb('lg', [128, 8])
        m8 = C.sb('m8', [128, 8])
        rt = C.sb('rt', [128, 8])
        gt = C.sb('gt', [128, 8])
        gt2 = C.sb('gt2', [128, 8])
        dg = [C.sb('dg%d' % i, [128, 128]) for i in range(2)]
    if final:
        fnw = C.sb('fnw_s', [128, KC])

    S.dma('sp', modt[:].rearrange('p j r -> p (j r)'), modd[li], reads=[('modd', li)], writes=['mods'])
    S.dma('sp', n1w[:], n1w_d, writes=['n1w'])
    S.dma('sp', n2w[:], n2w_d, writes=['n2w'])
    if moe:
        S.dma('pool', rw[:].rearrange('p k e -> p (k e)'), rw_d, writes=['rw'])
    if final:
        S.dma('sp', fnw[:], fnw_d, writes=['fnw'])
    for (sc, nw, nwk, mi) in ((sc1, n1w, 'n1w', 1), (sc2, n2w, 'n2w', 4)):
        for ps_ in range(2):
            S.op('dve', lambda e, sc=sc, nw=nw, ps_=ps_, mi=mi: e.scalar_tensor_tensor(
                out=sc[:, :, ps_], in0=modt[:, mi * 16:(mi + 1) * 16, ps_], scalar=1.0, in1=nw[:],
                op0=ALU.add, op1=ALU.mult),
                 reads=['mods', nwk], writes=['mods'])

    hv = hT.rearrange('(k p) t -> p k t', p=128)
    hov = h_dst.rearrange('(k p) t -> p k t', p=128)
    if final:
        yov = yout.rearrange('(k p) t -> p k t', p=128)
    if hdbg is not None:
        hdv = hdbg.rearrange('(k p) t -> p k t', p=128)
    brv = brT.rearrange('n (k p) t -> n p k t', p=128)
    wctr = {'A': 0, 'B': 0, 'act': 0}

    def nextbuf(kind):
        i = wctr[kind]
        wctr[kind] = i + 1
        return i % 2

    for p in range(T // PASS_T):
        t0 = p * PASS_T
        subs = [(off, n, p * 2 + si) for si, (off, n) in enumerate(SUBS_T)]
        if final and p == 0:
            subs = subs[1:]
        S.dma('sp', hs[:], hv[:, :, t0:t0 + PASS_T], reads=[h_src_key], writes=['hs'])
        for n in range(4):
            S.dma('sp', br[n][:], brv[n][:, :, t0:t0 + PASS_T], reads=['brT'], writes=[('br', n)])
        emit_rmsnorm(C, 'n1', lambda kc, off, n: hs[:, kc, off:off + n], 'hs', lambda kc, off, n: xn[:, kc, off:off + n], 'xn', subs,
                     lambda kc, idx: SC(sc1, kc, idx), lambda kc, idx: MOD(0, kc, idx), tmp)
        for dc in range(KC):
            a = nextbuf('A')
            b = nextbuf('B')
            S.dma('pool', wA[a][:], wg_d[dc], writes=[('wA', a)])
            S.dma('pool', wB[b][:], wb_d[dc], writes=[('wB', b)])
            wgv = wA[a][:].rearrange('p (k n c) -> p k n c', k=KC, n=4)
            wbv = wB[b][:].rearrange('p (n k c) -> p n k c', n=4, k=8)
            for (off, n_, idx) in subs:
                for n in range(4):
                    pg, pgk = C.ps()
                    for kc in range(KC):
                        S.op('pe', lambda e, kc=kc, n=n, pg=pg: e.matmul(
                            pg[:, 0:n_], wgv[:, kc, n, :], xn[:, kc, off:off + n_],
                            start=(kc == 0), stop=(kc == KC - 1)),
                             reads=[('wA', a), 'xn'], writes=[pgk])
                    sb_ = n % 2
                    S.op('act', lambda e, pg=pg, sb_=sb_: e.activation(out=sg[sb_][:, 0:n_], in_=pg[:, 0:n_],
                                                                       func=AF.Sigmoid),
                         reads=[pgk], writes=[('sg', sb_)])
                    pp, ppk = C.ps()
                    for kc in range(8):
                        S.op('pe', lambda e, kc=kc, n=n, pp=pp: e.matmul(
                            pp[:, 0:n_], wbv[:, n, kc, :], br[n][:, kc, off:off + n_],
                            start=(kc == 0), stop=(kc == 7)),
                             reads=[('wB', b), ('br', n)], writes=[ppk])
                    if n == 0:
                        S.op('dve', lambda e, pp=pp, sb_=sb_: e.tensor_tensor(
                            out=yacc[:, 0:n_], in0=sg[sb_][:, 0:n_], in1=pp[:, 0:n_], op=ALU.mult),
                             reads=[('sg', sb_), ppk], writes=['yacc'])
                    else:
                        S.op('dve', lambda e, pp=pp, sb_=sb_: e.tensor_tensor(
                            out=sg[sb_][:, 0:n_], in0=sg[sb_][:, 0:n_], in1=pp[:, 0:n_], op=ALU.mult),
                             reads=[('sg', sb_), ppk], writes=[('sg', sb_)])
                        if n < 3:
                            S.op('dve', lambda e, sb_=sb_: e.tensor_tensor(
                                out=yacc[:, 0:n_], in0=yacc[:, 0:n_], in1=sg[sb_][:, 0:n_], op=ALU.add),
                                 reads=[('sg', sb_), 'yacc'], writes=['yacc'])
                        else:
                            S.op('dve', lambda e, sb_=sb_: e.tensor_tensor(
                                out=yv[:, dc, off:off + n_], in0=yacc[:, 0:n_], in1=sg[sb_][:, 0:n_], op=ALU.add),
                                 reads=[('sg', sb_), 'yacc'], writes=['yv'])
        for dc in range(KC):
            b = nextbuf('B')
            S.dma('pool', wB[b][:, 0:KC * 128], wo_d[dc], writes=[('wB', b)])
            wov = wB[b][:, 0:KC * 128].rearrange('p (k c) -> p k c', k=KC)
            for (off, n_, idx) in subs:
                po, pok = C.ps()
                for kc in range(KC):
                    S.op('pe', lambda e, kc=kc, po=po: e.matmul(
                        po[:, 0:n_], wov[:, kc, :], yv[:, kc, off:off + n_],
                        start=(kc == 0), stop=(kc == KC - 1)),
                         reads=[('wB', b), 'yv'], writes=[pok])
                S.op('dve', lambda e, po=po: e.scalar_tensor_tensor(
                    out=hs[:, dc, off:off + n_], in0=po[:, 0:n_], scalar=MOD(2, dc, idx),
                    in1=hs[:, dc, off:off + n_], op0=ALU.mult, op1=ALU.add),
                     reads=[pok, 'hs', 'mods'], writes=['hs'])
        emit_rmsnorm(C, 'n2', lambda kc, off, n: hs[:, kc, off:off + n], 'hs', lambda kc, off, n: xn[:, kc, off:off + n], 'xn', subs,
                     lambda kc, idx: SC(sc2, kc, idx), lambda kc, idx: MOD(3, kc, idx), tmp)
        if moe:
            chunks = [(0, 128), (128, 128), (256, 128), (384, 128), (512, 64)]
            if final and p == 0:
                chunks = chunks[2:]
            for (co, cn) in chunks:
                pl_, plk = C.ps()
                for kc in range(KC):
                    S.op('pe', lambda e, kc=kc, pl_=pl_: e.matmul(
                        pl_[0:cn, 0:NEXP], xn[:, kc, co:co + cn], rw[:, kc, :],
                        start=(kc == 0), stop=(kc == KC - 1)),
                         reads=['xn', 'rw'], writes=[plk])
                S.op('act', lambda e, pl_=pl_: e.activation(out=lg[0:cn, :], in_=pl_[0:cn, 0:NEXP], func=AF.Identity),
                     reads=[plk], writes=['lg'])
                S.op('dve', lambda e: e.max(out=m8[0:cn, :], in_=lg[0:cn, :]), reads=['lg'], writes=['m8'])
                S.op('dve', lambda e: e.tensor_tensor(out=rt[0:cn, 0:1], in0=m8[0:cn, 1:2], in1=m8[0:cn, 0:1],
                                                      op=ALU.subtract), reads=['m8'], writes=['rt'])
                S.op('act', lambda e: e.activation(out=rt[0:cn, 1:2], in_=rt[0:cn, 0:1], func=AF.Exp),
                     reads=['rt'], writes=['rt'])
                S.op('dve', lambda e: e.tensor_scalar(out=rt[0:cn, 2:3], in0=rt[0:cn, 1:2], scalar1=1.0, scalar2=None,
                                                      op0=ALU.add), reads=['rt'], writes=['rt'])
                S.op('dve', lambda e: e.reciprocal(out=rt[0:cn, 2:3], in_=rt[0:cn, 2:3]), reads=['rt'], writes=['rt'])
                S.op('dve', lambda e: e.tensor_tensor(out=rt[0:cn, 3:4], in0=rt[0:cn, 1:2], in1=rt[0:cn, 2:3],
                                                      op=ALU.mult), reads=['rt'], writes=['rt'])
                S.op('dve', lambda e: e.tensor_scalar(out=gt[0:cn, :], in0=lg[0:cn, :], scalar1=m8[0:cn, 0:1],
                                                      scalar2=rt[0:cn, 2:3], op0=ALU.is_equal, op1=ALU.mult),
                     reads=['lg', 'm8', 'rt'], writes=['gt'])
                S.op('dve', lambda e: e.tensor_scalar(out=gt2[0:cn, :], in0=lg[0:cn, :], scalar1=m8[0:cn, 1:2],
                                                      scalar2=rt[0:cn, 3:4], op0=ALU.is_equal, op1=ALU.mult),
                     reads=['lg', 'm8', 'rt'], writes=['gt2'])
                S.op('dve', lambda e: e.tensor_tensor(out=gt[0:cn, :], in0=gt[0:cn, :], in1=gt2[0:cn, :], op=ALU.add),
                     reads=['gt', 'gt2'], writes=['gt'])
                for ex in range(NEXP):
                    db = ex % 2
                    S.op('dve', lambda e, ex=ex, db=db: e.tensor_scalar(
                        out=dg[db][0:cn, 0:cn], in0=C.ident[0:cn, 0:cn], scalar1=gt[0:cn, ex:ex + 1], scalar2=None,
                        op0=ALU.mult), reads=['ident', 'gt'], writes=[('dg', db)])
                    pb, pbk = C.ps()
                    S.op('pe', lambda e, db=db, pb=pb: e.matmul(pb[:, 0:cn], C.ones[0:cn, :], dg[db][0:cn, 0:cn],
                                                                start=True, stop=True),
                         reads=[('dg', db), 'ones'], writes=[pbk])
                    S.op('act', lambda e, ex=ex, pb=pb: e.activation(out=gbc[:, ex, co:co + cn], in_=pb[:, 0:cn],
                                                                     func=AF.Identity),
                         reads=[pbk], writes=['gbc'])
        for blk in range(NBLK):
            a = nextbuf('A')
            b = nextbuf('B')
            ab = nextbuf('act')
            ex = blk // (EXP_DIM // 256) if moe else 0
            S.dma('pool', wA[a][:], wgu_d[blk], writes=[('wA', a)])
            S.dma('pool', wB[b][:], w2_d[blk], writes=[('wB', b)])
            wguv = wA[a][:].rearrange('p (k g c) -> p k g c', k=KC, g=2)
            w2v = wB[b][:].rearrange('p (f d) -> p f d', f=2)
            for fc in range(2):
                for (off, n_, idx) in subs:
                    pg, pgk = C.ps()
                    pu, puk = C.ps()
                    for g_, (pp_, ppk_) in enumerate(((pg, pgk), (pu, puk))):
                        for kc in range(KC):
                            S.op('pe', lambda e, kc=kc, g_=g_, pp_=pp_: e.matmul(
                                pp_[:, 0:n_], wguv[:, kc, g_, fc * 128:(fc + 1) * 128], xn[:, kc, off:off + n_],
                                start=(kc == 0), stop=(kc == KC - 1)),
                                 reads=[('wA', a), 'xn'], writes=[ppk_])
                    sb_ = fc % 2
                    S.op('act', lambda e, pg=pg, sb_=sb_: e.activation(out=sg[sb_][:, 0:n_], in_=pg[:, 0:n_],
                                                                       func=AF.Silu),
                         reads=[pgk], writes=[('sg', sb_)])
                    if moe:
                        S.op('dve', lambda e, pu=pu, sb_=sb_: e.tensor_tensor(
                            out=sg[sb_][:, 0:n_], in0=sg[sb_][:, 0:n_], in1=pu[:, 0:n_], op=ALU.mult),
                             reads=[('sg', sb_), puk], writes=[('sg', sb_)])
                        S.op('dve', lambda e, sb_=sb_: e.tensor_tensor(
                            out=act[ab][:, fc, off:off + n_], in0=sg[sb_][:, 0:n_], in1=gbc[:, ex, off:off + n_],
                            op=ALU.mult),
                             reads=[('sg', sb_), 'gbc'], writes=[('act', ab)])
                    else:
                        S.op('dve', lambda e, pu=pu, sb_=sb_: e.tensor_tensor(
                            out=act[ab][:, fc, off:off + n_], in0=sg[sb_][:, 0:n_], in1=pu[:, 0:n_], op=ALU.mult),
                             reads=[('sg', sb_), puk], writes=[('act', ab)])
            for dc in range(KC):
                for (off, n_, idx) in subs:
                    po, pok = C.ps()
                    for fc in range(2):
                        S.op('pe', lambda e, fc=fc, po=po: e.matmul(
                            po[:, 0:n_], w2v[:, fc, dc * 128:(dc + 1) * 128], act[ab][:, fc, off:off + n_],
                            start=(fc == 0), stop=(fc == 1)),
                             reads=[('wB', b), ('act', ab)], writes=[pok])
                    S.op('dve', lambda e, po=po: e.scalar_tensor_tensor(
                        out=hs[:, dc, off:off + n_], in0=po[:, 0:n_], scalar=MOD(5, dc, idx),
                        in1=hs[:, dc, off:off + n_], op0=ALU.mult, op1=ALU.add),
                         reads=[pok, 'hs', 'mods'], writes=['hs'])
        S.dma('sp', hov[:, :, t0:t0 + PASS_T], hs[:], reads=['hs'], writes=[h_dst_key])
        if hdbg is not None:
            S.dma('sp', hdv[:, :, t0:t0 + PASS_T], hs[:], reads=['hs'], writes=['hdbg'])
        if final:
            emit_rmsnorm(C, 'nf', lambda kc, off, n: hs[:, kc, off:off + n], 'hs', lambda kc, off, n: hs[:, kc, off:off + n], 'hs', subs, lambda kc, idx: fnw[:, kc:kc + 1], None, tmp)
            S.dma('sp', yov[:, :, t0:t0 + PASS_T], hs[:], reads=['hs'], writes=['yout'])
    barrier(C)
    C.pop()


GATES_OFF = 22560 - 4 * D


def lay_vec(v):
    return np.ascontiguousarray(v.reshape(-1, 128).T)


def lay_token_weights(w_in_l, w_branch_l, w_out_l):
    wg = w_in_l[:, GATES_OFF:].reshape(KC, 128, 4, KC, 128).transpose(3, 1, 0, 2, 4).reshape(KC, 128, KC * 4 * 128)
    wb = w_branch_l.reshape(4, 8, 128, KC, 128).transpose(3, 2, 0, 1, 4).reshape(KC, 128, 4 * 8 * 128)
    wo = w_out_l.reshape(KC, 128, KC, 128).transpose(2, 1, 0, 3).reshape(KC, 128, KC * 128)
    return np.ascontiguousarray(wg), np.ascontiguousarray(wb), np.ascontiguousarray(wo)


def lay_ffn(wgu, w2, F):
    nb = F // 256
    a = wgu.reshape(KC, 128, 2, nb, 256).transpose(3, 1, 0, 2, 4).reshape(nb, 128, KC * 2 * 256)
    b = w2.reshape(nb, 2, 128, D).transpose(0, 2, 1, 3).reshape(nb, 128, 2 * D)
    return np.ascontiguousarray(a), np.ascontiguousarray(b)


IDENT = np.eye(128, dtype=np.float32)


def token_in_map(layer, inp, moe, final):
    wg, wb, wo = lay_token_weights(inp['w_in'][layer], inp['w_branch'][layer], inp['w_out'][layer])
    m = {'n1w': lay_vec(inp['norm1_w'][layer]), 'n2w': lay_vec(inp['norm2_w'][layer]), 'wg': wg, 'wb': wb, 'wo': wo}
    if moe:
        parts = [lay_ffn(inp['moe_wgu'][layer // 2][e], inp['moe_w2'][layer // 2][e], EXP_DIM) for e in range(NEXP)]
        m['wgu'] = np.concatenate([p[0] for p in parts], axis=0)
        m['w2'] = np.concatenate([p[1] for p in parts], axis=0)
        m['rw'] = np.ascontiguousarray(
            inp['router_w'][layer // 2].reshape(KC, 128, NEXP).transpose(1, 0, 2).reshape(128, KC * NEXP))
    else:
        m['wgu'], m['w2'] = lay_ffn(inp['ffn_wgu'][layer // 2], inp['ffn_w2'][layer // 2], FFN_DIM)
    if final:
        m['fnw'] = lay_vec(inp['final_norm_w'])
    return m


NCHK = T // 128
FWD_ORDER = list(range(NCHK))
BWD_ORDER = [1, 0] + list(range(NCHK - 1, 1, -1))
TILES_M = [(0, 256)] + [(256 + i * 512, 512) for i in range(4)]
SEGS = [(0, NCTX), (NCTX, T)]
OFF = {'ret_q': 0, 'ret_k': 1024, 'ret_v': 2048, 'ret_g': 3072, 'ssd_z': 4096, 'ssd_x': 5120, 'ssd_B': 6144,
       'ssd_C': 6656, 'ssd_dt': 7168, 'lru_x': 7200, 'lru_y': 8224, 'hg_q': 9248, 'hg_f': 10272,
       'hg_i': 12320, 'hg_g': 13344}


def mixer_chunks(s):
    L = []
    for j in range(4):
        hh = 4 * s + j
        for nm in ('ret_q', 'ret_k', 'ret_v', 'ret_g'):
            L.append(((nm, j), OFF[nm] + hh * 128))
    for gi in range(2):
        gg = 2 * s + gi
        L.append((('ssd_B', gi), OFF['ssd_B'] + gg * 128))
        L.append((('ssd_C', gi), OFF['ssd_C'] + gg * 128))
    for pi in range(4):
        pp = 4 * s + pi
        L.append((('ssd_x', pi), OFF['ssd_x'] + pp * 128))
        L.append((('ssd_z', pi), OFF['ssd_z'] + pp * 128))
    for j in range(4):
        gb = 4 * s + j
        L.append((('lru_x', j), OFF['lru_x'] + gb * 128))
        L.append((('lru_y', j), OFF['lru_y'] + gb * 128))
    for j in range(4):
        hh = 4 * s + j
        L.append((('hg_q', j), OFF['hg_q'] + hh * 128))
        L.append((('hg_f0', j), OFF['hg_f'] + hh * 128))
        L.append((('hg_f1', j), OFF['hg_f'] + 1024 + hh * 128))
        L.append((('hg_i', j), OFF['hg_i'] + hh * 128))
        L.append((('hg_g', j), OFF['hg_g'] + hh * 128))
    return L


CHIDX = {nm: i for i, (nm, _) in enumerate(mixer_chunks(0))}
NCH_M = len(CHIDX)


def emit_mixer(C, layer, li, s, hT, hkey, brT, modd, xst, which=('ret', 'ssd', 'lru', 'hg')):
    S = C.S
    C.pfx = 'L%ds%d_' % (layer, s)
    C.push()
    n1w_d = C.din('n1w', [128, KC])
    wM_d = C.din('wM', [NCH_M, 128, KC * 128])
    masks_d = C.din('masks', [128, 4 * 128], shared=True)
    identb_d = C.din('identb', [128, 128], BF16, shared=True)
    obT = C.sb('obT', [128, T], BF16)

    def out_tm(n, ch0, src_of_c, skey):
        for c in range(NCHK):
            transpose_to(obT[:, c * 128:(c + 1) * 128], 'obT', src_of_c(c), skey)
        S.dma('sp', brT[n, ch0:ch0 + 128, :], obT[:], reads=['obT'], writes=['brT'])

    xnT = xst['xnT']
    xst['fresh'] = not xst.get('done', False)
    xst['done'] = True
    modt = C.sb('modt', [128, 96, 2])
    n1w = C.sb('n1w_s', [128, KC])
    sc1 = C.sb('sc1', [128, KC, 2])
    masks = C.sb('masks_s', [128, 4, 128])
    identb = C.sb('identb_s', [128, 128], BF16)
    oneb = C.sb('oneb', [128, 1])
    NW = 4
    wbuf = [C.sb('wbuf%d' % i, [128, KC, 128], BF16) for i in range(NW)]
    wstate = {'i': 0}
    M_le, M_ge, M_gt, M_lt = (masks[:, i, :] for i in range(4))

    S.dma('sp', modt[:].rearrange('p j r -> p (j r)'), modd[li], reads=[('modd', li)], writes=['mods'])
    S.dma('sp', n1w[:], n1w_d, writes=['n1w'])
    S.dma('sp', masks[:].rearrange('p a c -> p (a c)'), masks_d, writes=['masks'])
    S.dma('sp', identb[:], identb_d, writes=['identb'])
    S.op('dve', lambda e: e.memset(oneb[:], 1.0), writes=['oneb'])
    for a_ in range(2):
        S.op('dve', lambda e, a_=a_: e.scalar_tensor_tensor(
            out=sc1[:, :, a_], in0=modt[:, 16:32, a_], scalar=1.0, in1=n1w[:], op0=ALU.add, op1=ALU.mult),
             reads=['mods', 'n1w'], writes=['mods'])

    def getw(name):
        i = wstate['i'] % NW
        wstate['i'] += 1
        S.dma('pool', wbuf[i][:].rearrange('p k c -> p (k c)'), wM_d[CHIDX[name]], writes=[('w', i)])
        return wbuf[i], ('w', i)

    def proj_fm(w, wk, t0, n):
        pt, pk = C.ps()
        for kc in range(KC):
            S.op('pe', lambda e, kc=kc: e.matmul(pt[:, 0:n], w[:, kc, :], xnT[:, kc, t0:t0 + n],
                                                 start=(kc == 0), stop=(kc == KC - 1)),
                 reads=[wk, 'xnT'], writes=[pk])
        return pt, pk

    def proj_tm(w_ap_of_kc, wk, c, ncol):
        pt, pk = C.ps()
        for kc in range(KC):
            S.op('pe', lambda e, kc=kc: e.matmul(pt[:, 0:ncol], xnT[:, kc, c * 128:(c + 1) * 128], w_ap_of_kc(kc),
                                                 start=(kc == 0), stop=(kc == KC - 1)),
                 reads=[wk, 'xnT'], writes=[pk])
        return pt, pk

    def transpose_to(dst_ap, dkey, src_ap, skey):
        S.op('pe', lambda e: e.transpose(out=C.pbf[:, 0:128], in_=src_ap, identity=identb[:]),
             reads=[skey, 'identb'], writes=['pbf'])
        S.op('act', lambda e: e.activation(out=dst_ap, in_=C.pbf[:, 0:128], func=AF.Identity),
             reads=['pbf'], writes=[dkey])

    with ExitStack() as es0:
        def sb0(name, shape, dt=F32):
            return es0.enter_context(C.nc.sbuf_tensor(C.pfx + name, list(shape), dt))
        ht = [sb0('ht%d' % i, [128, KC, 256]) for i in range(2)]
        tmp = {'sq': [sb0('sq%d' % i, [128, 256]) for i in range(2)],
               'tm': [sb0('tm%d' % i, [128, 256]) for i in range(2)],
               'rstd': sb0('rstd', [128, 256])}
        hv = hT.rearrange('(k p) t -> p k t', p=128)
        for ti in (range(T // 256) if xst['fresh'] else []):
            b = ti % 2
            S.dma('sp', ht[b][:], hv[:, :, ti * 256:(ti + 1) * 256], reads=[hkey], writes=[('ht', b)])
            a_ = 1 if ti == 0 else 0
            emit_rmsnorm(C, 'n1', lambda kc, off, n, b=b: ht[b][:, kc, off:off + n], ('ht', b),
                         lambda kc, off, n, ti=ti: xnT[:, kc, ti * 256 + off:ti * 256 + off + n], 'xnT',
                         [(0, 256, a_)],
                         lambda kc, idx: sc1[:, kc, idx:idx + 1], lambda kc, idx: modt[:, kc, idx:idx + 1], tmp)
        barrier(C)

    def scalar_scan(sbx, tag, qT, kT, k_tm, v_tm, heads, la_c_of, o_acc, okey):
        nh = len(heads)
        Sst = [sbx('%s_S%d' % (tag, h), [128, 128]) for h in range(nh)]
        Sbf = [sbx('%s_Sb%d' % (tag, h), [128, 128], BF16) for h in range(nh)]
        ec = sbx(tag + '_ec', [128, 4 * nh])
        Am = [sbx('%s_A%d' % (tag, i), [128, 128]) for i in range(2)]
        Lt = [sbx('%s_L%d' % (tag, i), [128, 128]) for i in range(2)]
        Pm = [sbx('%s_P%d' % (tag, i), [128, 128], BF16) for i in range(2)]
        vw = [sbx('%s_vw%d' % (tag, i), [128, 128], BF16) for i in range(2)]
        tmo = [sbx('%s_to%d' % (tag, i), [128, 128]) for i in range(2)]
        cnt = {'i': 0}
        for d, order in ((0, FWD_ORDER), (1, BWD_ORDER)):
            mA = M_le if d == 0 else M_ge
            mB = M_gt if d == 0 else M_lt
            last = 127 if d == 0 else 0
            for h in range(nh):
                S.op('dve', lambda e, h=h: e.memset(Sst[h][:], 0.0), writes=[(tag, 'S', h)])
                S.op('act', lambda e, h=h: e.activation(out=Sbf[h][:], in_=Sst[h][:], func=AF.Identity),
                     reads=[(tag, 'S', h)], writes=[(tag, 'Sb', h)])
            for c in order:
                la_ap, la_key = la_c_of(c, d)
                pe_, pek = C.ps()
                S.op('pe', lambda e: e.matmul(pe_[:, 0:nh], mA, la_ap, start=True, stop=True),
                     reads=['masks', la_key], writes=[pek])
                S.op('pe', lambda e: e.matmul(pe_[:, nh:2 * nh], C.ones[:], la_ap,
                                              start=True, stop=True),
                     reads=['ones', la_key], writes=[pek])
                S.op('act', lambda e: e.activation(out=ec[:, 0:2 * nh], in_=pe_[:, 0:2 * nh], func=AF.Exp),
                     reads=[pek], writes=[(tag, 'ec')])
                psc, psck = C.psb[6], ('ps', 6)
                S.op('pe', lambda e, c=c: e.matmul(psc[:, 0:128], kT[:, c * 128:(c + 1) * 128],
                                                   qT[:, c * 128:(c + 1) * 128], start=True, stop=True),
                     reads=[(tag, 'kT'), (tag, 'qT')], writes=[psck])
                for h, hd in enumerate(heads):
                    i2 = cnt['i'] % 2
                    cnt['i'] += 1
                    v0, dv = hd['v0'], hd['dv']
                    la1 = hd['la'][d](c)
                    dt1 = hd['dt'][d](c)
                    S.op('dve', lambda e, i2=i2, la1=la1: e.tensor_scalar(out=Am[i2][:], in0=mA, scalar1=la1,
                                                                          scalar2=None, op0=ALU.mult),
                         reads=['masks', la_key], writes=[(tag, 'A', i2)])
                    pl_, plk = C.ps()
                    S.op('pe', lambda e, i2=i2: e.matmul(pl_[:, 0:128], mB, Am[i2][:], start=True, stop=True),
                         reads=['masks', (tag, 'A', i2)], writes=[plk])
                    S.op('act', lambda e, i2=i2: e.activation(out=Lt[i2][:], in_=pl_[:, 0:128], func=AF.Exp),
                         reads=[plk], writes=[(tag, 'L', i2)])
                    S.op('dve', lambda e, i2=i2, dt1=dt1: e.scalar_tensor_tensor(
                        out=Lt[i2][:], in0=Lt[i2][:], scalar=dt1, in1=mA, op0=ALU.mult, op1=ALU.mult),
                         reads=[(tag, 'L', i2), 'masks', la_key], writes=[(tag, 'L', i2)])
                    S.op('dve', lambda e, i2=i2: e.tensor_tensor(out=Pm[i2][:], in0=psc[:, 0:128], in1=Lt[i2][:],
                                                                 op=ALU.mult),
                         reads=[psck, (tag, 'L', i2)], writes=[(tag, 'P', i2)])
                    pin, pink = C.ps()
                    S.op('pe', lambda e, i2=i2, c=c: e.matmul(pin[:, 0:dv], Pm[i2][:], v_tm[:, c, v0:v0 + dv],
                                                              start=True, stop=True),
                         reads=[(tag, 'P', i2), (tag, 'v')], writes=[pink])
                    pit, pitk = C.ps()
                    S.op('pe', lambda e, h=h, c=c: e.matmul(pit[:, 0:dv], qT[:, c * 128:(c + 1) * 128],
                                                            Sbf[h][:, 0:dv], start=True, stop=True),
                         reads=[(tag, 'qT'), (tag, 'Sb', h)], writes=[pitk])
                    S.op('dve', lambda e, i2=i2, c=c: e.tensor_tensor(
                        out=tmo[i2][:, 0:dv], in0=pin[:, 0:dv], in1=o_acc[:, c, v0:v0 + dv], op=ALU.add),
                         reads=[pink, okey], writes=[(tag, 'to', i2)])
                    S.op('dve', lambda e, i2=i2, c=c, h=h: e.scalar_tensor_tensor(
                        out=o_acc[:, c, v0:v0 + dv], in0=pit[:, 0:dv], scalar=ec[:, h:h + 1], in1=tmo[i2][:, 0:dv],
                        op0=ALU.mult, op1=ALU.add),
                         reads=[pitk, (tag, 'ec'), (tag, 'to', i2)], writes=[okey])
                    S.op('dve', lambda e, i2=i2, c=c: e.tensor_scalar(
                        out=vw[i2][:, 0:dv], in0=v_tm[:, c, v0:v0 + dv], scalar1=Lt[i2][:, last:last + 1],
                        scalar2=None, op0=ALU.mult),
                         reads=[(tag, 'v'), (tag, 'L', i2)], writes=[(tag, 'vw', i2)])
                    pct, pctk = C.ps()
                    S.op('pe', lambda e, i2=i2, c=c: e.matmul(pct[:, 0:dv], k_tm[:, c, :], vw[i2][:, 0:dv],
                                                              start=True, stop=True),
                         reads=[(tag, 'ktm'), (tag, 'vw', i2)], writes=[pctk])
                    S.op('dve', lambda e, h=h: e.scalar_tensor_tensor(
                        out=Sst[h][:, 0:dv], in0=Sst[h][:, 0:dv], scalar=ec[:, nh + h:nh + h + 1], in1=pct[:, 0:dv],
                        op0=ALU.mult, op1=ALU.add),
                         reads=[(tag, 'S', h), (tag, 'ec'), pctk], writes=[(tag, 'S', h)])
                    S.op('act', lambda e, h=h: e.activation(out=Sbf[h][:, 0:dv], in_=Sst[h][:, 0:dv],
                                                            func=AF.Identity),
                         reads=[(tag, 'S', h)], writes=[(tag, 'Sb', h)])

    def rms_rows(sbx, tag, src, skey, W, nchunk=NCHK):
        ss = sbx(tag + '_ss', [128, nchunk])
        junk = sbx(tag + '_junk', [128, W])
        for c in range(nchunk):
            S.op('act', lambda e, c=c: e.activation(out=junk[:], in_=src[:, c, :], func=AF.Square,
                                                    accum_out=ss[:, c:c + 1]),
                 reads=[skey], writes=[(tag, 'ss'), (tag, 'junk')])
        S.op('act', lambda e: e.activation(out=ss[:], in_=ss[:], func=AF.Sqrt, bias=C.epsb[:, 0:1], scale=1.0 / W),
             reads=[(tag, 'ss'), 'epsb'], writes=[(tag, 'ss')])
        S.op('dve', lambda e: e.reciprocal(out=ss[:], in_=ss[:]), reads=[(tag, 'ss')], writes=[(tag, 'ss')])
        return ss, (tag, 'ss')

    if 'ret' in which:
        rope_d = C.din('rope', [128, 2 * T], shared=True)
        pmat_d = C.din('pmat', [128, 128], shared=True)
        retla_d = C.din('retla', [128, 4])
        with ExitStack() as es1:
            def sbx(name, shape, dt=F32):
                return es1.enter_context(C.nc.sbuf_tensor(C.pfx + name, list(shape), dt))
            rope = sbx('rope_s', [128, 2, T])
            pmat = sbx('pmat_s', [128, 128])
            retla = sbx('retla_s', [128, 4])
            ones2 = sbx('ones2', [128, 2])
            S.dma('sp', rope[:].rearrange('p a t -> p (a t)'), rope_d, writes=['rope'])
            S.dma('sp', pmat[:], pmat_d, writes=['pmat'])
            S.dma('sp', retla[:], retla_d, writes=['retla'])
            S.op('dve', lambda e: e.memset(ones2[:], 1.0), writes=['ones2'])
            qT = sbx('r_qT', [128, T], BF16)
            kT = sbx('r_kT', [128, T], BF16)
            k_tm = sbx('r_ktm', [128, NCHK, 128], BF16)
            v_tm = sbx('r_vtm', [128, NCHK, 128], BF16)
            g_tm = sbx('r_gtm', [128, NCHK, 128], BF16)
            o_acc = sbx('r_oacc', [128, NCHK, 128])
            q32 = [sbx('r_q32%d' % i, [128, 512]) for i in range(2)]
            qr = [sbx('r_qr%d' % i, [128, 512]) for i in range(2)]
            lac = sbx('r_lac', [128, 2])
            ob = sbx('r_ob', [128, NCHK, 128], BF16)
            tag = 'ret'
            for j in range(4):
                for (nm, dst, dkey, scl) in (('ret_q', qT, (tag, 'qT'), 1.0), ('ret_k', kT, (tag, 'kT'), 128.0 ** -0.5)):
                    w, wk = getw((nm, j))
                    for ti, (t0, n) in enumerate(TILES_M):
                        b = ti % 2
                        pt, pk = proj_fm(w, wk, t0, n)
                        S.op('act', lambda e, b=b, pt=pt, n=n, scl=scl: e.activation(
                            out=q32[b][:, 0:n], in_=pt[:, 0:n], func=AF.Identity, scale=scl),
                             reads=[pk], writes=[(tag, 'q32', b)])
                        ps2, ps2k = C.ps()
                        S.op('pe', lambda e, b=b, ps2=ps2, n=n: e.matmul(ps2[:, 0:n], pmat[:], q32[b][:, 0:n],
                                                                         start=True, stop=True),
                             reads=['pmat', (tag, 'q32', b)], writes=[ps2k])
                        S.op('dve', lambda e, b=b, ps2=ps2, n=n, t0=t0: e.tensor_tensor(
                            out=qr[b][:, 0:n], in0=ps2[:, 0:n], in1=rope[:, 1, t0:t0 + n], op=ALU.mult),
                             reads=[ps2k, 'rope'], writes=[(tag, 'qr', b)])
                        S.op('dve', lambda e, b=b, n=n, t0=t0: e.tensor_tensor(
                            out=q32[b][:, 0:n], in0=q32[b][:, 0:n], in1=rope[:, 0, t0:t0 + n], op=ALU.mult),
                             reads=[(tag, 'q32', b), 'rope'], writes=[(tag, 'q32', b)])
                        S.op('dve', lambda e, b=b, n=n, t0=t0, dst=dst: e.tensor_tensor(
                            out=dst[:, t0:t0 + n], in0=q32[b][:, 0:n], in1=qr[b][:, 0:n], op=ALU.add),
                             reads=[(tag, 'q32', b), (tag, 'qr', b)], writes=[dkey])
                for c in range(NCHK):
                    transpose_to(k_tm[:, c, :], (tag, 'ktm'), kT[:, c * 128:(c + 1) * 128], (tag, 'kT'))
                w, wk = getw(('ret_v', j))
                for c in range(NCHK):
                    pt, pk = proj_tm(lambda kc, w=w: w[:, kc, :], wk, c, 128)
                    S.op('act', lambda e, c=c, pt=pt: e.activation(out=v_tm[:, c, :], in_=pt[:, 0:128],
                                                                   func=AF.Identity),
                         reads=[pk], writes=[(tag, 'v')])
                w, wk = getw(('ret_g', j))
                for c in range(NCHK):
                    pt, pk = proj_tm(lambda kc, w=w: w[:, kc, :], wk, c, 128)
                    S.op('act', lambda e, c=c, pt=pt: e.activation(out=g_tm[:, c, :], in_=pt[:, 0:128], func=AF.Silu),
                         reads=[pk], writes=[(tag, 'g')])
                S.op('dve', lambda e: e.memset(o_acc[:], 0.0), writes=[(tag, 'o')])
                S.op('dve', lambda e, j=j: e.tensor_copy(out=lac[:, 0:1], in_=retla[:, j:j + 1]),
                     reads=['retla'], writes=[(tag, 'lac')])
                S.op('dve', lambda e, j=j: e.tensor_copy(out=lac[:, 1:2], in_=retla[:, j:j + 1]),
                     reads=['retla'], writes=[(tag, 'lac')])
                heads = [dict(v0=0, dv=128,
                              la=[lambda c: lac[:, 0:1], lambda c: lac[:, 1:2]],
                              dt=[lambda c: ones2[:, 0:1], lambda c: ones2[:, 1:2]])]
                with ExitStack() as es2:
                    def sby(name, shape, dt=F32):
                        return es2.enter_context(C.nc.sbuf_tensor(C.pfx + name + '_%d' % j, list(shape), dt))
                    scalar_scan(sby, tag, qT, kT, k_tm, v_tm, heads, lambda c, d: (lac[:, d:d + 1], (tag, 'lac')), o_acc, (tag, 'o'))
                    rs, rsk = rms_rows(sby, tag, o_acc, (tag, 'o'), 128)
                    for c in range(NCHK):
                        S.op('dve', lambda e, c=c: e.scalar_tensor_tensor(
                            out=ob[:, c, :], in0=o_acc[:, c, :], scalar=rs[:, c:c + 1], in1=g_tm[:, c, :],
                            op0=ALU.mult, op1=ALU.mult),
                             reads=[(tag, 'o'), rsk, (tag, 'g')], writes=[(tag, 'ob')])
                    out_tm(0, s * 512 + j * 128, lambda c: ob[:, c, :], (tag, 'ob'))
                    barrier(C)
        barrier(C)

    if 'lru' in which:
        lrup_d = C.din('lru_p', [128, 4 * 5])
        lrugb_d = C.din('lru_gb', [128, 16])
        lrulam_d = C.din('lru_lam', [128, 8])
        lruw_d = C.din('lru_w', [128, 16 * 128])
        with ExitStack() as es1:
            def sbx(name, shape, dt=F32):
                return es1.enter_context(C.nc.sbuf_tensor(C.pfx + name, list(shape), dt))
            tag = 'lru'
            lrup = sbx('lrup', [128, 4, 5])
            gb = sbx('lrugb', [128, 2, 2, 4])
            lam = sbx('lrulam', [128, 2, 4])
            ls8 = sbx('lruls8', [128, 2, 4])
            lruw = sbx('lruw', [128, 16, 128], BF16)
            S.dma('sp', lrup[:].rearrange('p j t -> p (j t)'), lrup_d, writes=['lrup'])
            S.dma('sp', gb[:].rearrange('p g d j -> p (g d j)'), lrugb_d, writes=['lrugb'])
            S.dma('sp', lam[:].rearrange('p d j -> p (d j)'), lrulam_d, writes=['lrulam'])
            S.dma('pool', lruw[:].rearrange('p a c -> p (a c)'), lruw_d, writes=['lruw'])
            S.op('act', lambda e: e.activation(out=ls8[:].rearrange('p d j -> p (d j)'),
                                               in_=lam[:].rearrange('p d j -> p (d j)'), func=AF.Sigmoid),
                 reads=['lrulam'], writes=['ls8'])
            S.op('act', lambda e: e.activation(out=ls8[:].rearrange('p d j -> p (d j)'),
                                               in_=ls8[:].rearrange('p d j -> p (d j)'), func=AF.Ln),
                 reads=['ls8'], writes=['ls8'])
            S.op('dve', lambda e: e.tensor_scalar(out=ls8[:].rearrange('p d j -> p (d j)'),
                                                  in0=ls8[:].rearrange('p d j -> p (d j)'), scalar1=8.0, scalar2=None,
                                                  op0=ALU.mult), reads=['ls8'], writes=['ls8'])
            xpre = sbx('l_xpre', [128, T])
            xc = sbx('l_xc', [128, T])
            xcb = sbx('l_xcb', [128, T], BF16)
            gy = sbx('l_gy', [128, T])
            a_t = sbx('l_a', [128, T])
            b_t = sbx('l_b', [128, T])
            hf = sbx('l_hf', [128, T])
            hb = sbx('l_hb', [128, T])
            ob = sbx('l_ob', [128, T], BF16)
            t1 = [sbx('l_t1%d' % i, [128, 512]) for i in range(2)]
            t2 = [sbx('l_t2%d' % i, [128, 512]) for i in range(2)]
            for j in range(4):
                w, wk = getw(('lru_x', j))
                for (t0, n) in TILES_M:
                    pt, pk = proj_fm(w, wk, t0, n)
                    S.op('act', lambda e, pt=pt, t0=t0, n=n: e.activation(out=xpre[:, t0:t0 + n], in_=pt[:, 0:n],
                                                                          func=AF.Identity),
                         reads=[pk], writes=[(tag, 'xpre')])
                emit_conv(C, tag, xpre, (tag, 'xpre'), xc, (tag, 'xc'), lambda tp, j=j: lrup[:, j, tp:tp + 1], 'lrup')
                S.op('act', lambda e: e.activation(out=xcb[:], in_=xc[:], func=AF.Identity),
                     reads=[(tag, 'xc')], writes=[(tag, 'xcb')])
                w, wk = getw(('lru_y', j))
                for ti, (t0, n) in enumerate(TILES_M):
                    b = ti % 2
                    pt, pk = proj_fm(w, wk, t0, n)
                    S.op('act', lambda e, pt=pt, b=b, n=n: e.activation(out=t1[b][:, 0:n], in_=pt[:, 0:n],
                                                                        func=AF.Identity),
                         reads=[pk], writes=[(tag, 't1', b)])
                    S.op('dve', lambda e, b=b, n=n: e.tensor_tensor(out=t2[b][:, 0:n], in0=t1[b][:, 0:n],
                                                                    in1=t1[b][:, 0:n], op=ALU.mult),
                         reads=[(tag, 't1', b)], writes=[(tag, 't2', b)])
                    S.op('dve', lambda e, b=b, n=n: e.tensor_scalar(out=t2[b][:, 0:n], in0=t2[b][:, 0:n],
                                                                    scalar1=0.044715, scalar2=1.0, op0=ALU.mult,
                                                                    op1=ALU.add),
                         reads=[(tag, 't2', b)], writes=[(tag, 't2', b)])
                    S.op('dve', lambda e, b=b, n=n: e.tensor_tensor(out=t2[b][:, 0:n], in0=t2[b][:, 0:n],
                                                                    in1=t1[b][:, 0:n], op=ALU.mult),
                         reads=[(tag, 't2', b), (tag, 't1', b)], writes=[(tag, 't2', b)])
                    S.op('act', lambda e, b=b, n=n: e.activation(out=t2[b][:, 0:n], in_=t2[b][:, 0:n],
                                                                 func=AF.Sigmoid, scale=1.5957691216057308),
                         reads=[(tag, 't2', b)], writes=[(tag, 't2', b)])
                    S.op('dve', lambda e, b=b, n=n, t0=t0: e.tensor_tensor(out=gy[:, t0:t0 + n], in0=t2[b][:, 0:n],
                                                                           in1=t1[b][:, 0:n], op=ALU.mult),
                         reads=[(tag, 't2', b), (tag, 't1', b)], writes=[(tag, 'gy')])
                for d in range(2):
                    for ti, (t0, n) in enumerate(TILES_M):
                        b = ti % 2
                        pr, prk = C.ps()
                        S.op('pe', lambda e, pr=pr, n=n, t0=t0: e.matmul(pr[:, 0:n], lruw[:, (0 * 2 + d) * 4 + j, :],
                                                                         xcb[:, t0:t0 + n], start=True, stop=True),
                             reads=['lruw', (tag, 'xcb')], writes=[prk])
                        S.op('act', lambda e, pr=pr, b=b, n=n: e.activation(out=t1[b][:, 0:n], in_=pr[:, 0:n],
                                                                            func=AF.Sigmoid, bias=gb[:, 0, d, j:j + 1],
                                                                            scale=1.0),
                             reads=[prk, 'lrugb'], writes=[(tag, 't1', b)])
                        S.op('act', lambda e, b=b, n=n, t0=t0: e.activation(out=a_t[:, t0:t0 + n], in_=t1[b][:, 0:n],
                                                                            func=AF.Exp, scale=ls8[:, d, j:j + 1]),
                             reads=[(tag, 't1', b), 'ls8'], writes=[(tag, 'a')])
                        pi_, pik = C.ps()
                        S.op('pe', lambda e, pi_=pi_, n=n, t0=t0: e.matmul(pi_[:, 0:n], lruw[:, (1 * 2 + d) * 4 + j, :],
                                                                           xcb[:, t0:t0 + n], start=True, stop=True),
                             reads=['lruw', (tag, 'xcb')], writes=[pik])
                        S.op('act', lambda e, pi_=pi_, b=b, n=n: e.activation(out=t2[b][:, 0:n], in_=pi_[:, 0:n],
                                                                              func=AF.Sigmoid,
                                                                              bias=gb[:, 1, d, j:j + 1], scale=1.0),
                             reads=[pik, 'lrugb'], writes=[(tag, 't2', b)])
                        S.op('dve', lambda e, b=b, n=n, t0=t0: e.tensor_tensor(out=t1[b][:, 0:n], in0=a_t[:, t0:t0 + n],
                                                                               in1=a_t[:, t0:t0 + n], op=ALU.mult),
                             reads=[(tag, 'a')], writes=[(tag, 't1', b)])
                        S.op('act', lambda e, b=b, n=n: e.activation(out=t1[b][:, 0:n], in_=t1[b][:, 0:n],
                                                                     func=AF.Sqrt, bias=oneb[:, 0:1], scale=-1.0),
                             reads=[(tag, 't1', b), 'oneb'], writes=[(tag, 't1', b)])
                        S.op('dve', lambda e, b=b, n=n, t0=t0: e.tensor_tensor(out=t2[b][:, 0:n], in0=t2[b][:, 0:n],
                                                                               in1=xc[:, t0:t0 + n], op=ALU.mult),
                             reads=[(tag, 't2', b), (tag, 'xc')], writes=[(tag, 't2', b)])
                        S.op('dve', lambda e, b=b, n=n, t0=t0: e.tensor_tensor(out=b_t[:, t0:t0 + n], in0=t2[b][:, 0:n],
                                                                               in1=t1[b][:, 0:n], op=ALU.mult),
                             reads=[(tag, 't2', b), (tag, 't1', b)], writes=[(tag, 'b')])
                    if d == 0:
                        S.op('dve', lambda e: e.tensor_tensor_scan(out=hf[:], data0=a_t[:], data1=b_t[:], initial=0.0,
                                                                   op0=ALU.mult, op1=ALU.add),
                             reads=[(tag, 'a'), (tag, 'b')], writes=[(tag, 'hf')])
                    else:
                        S.op('dve', lambda e: e.tensor_tensor_scan(
                            out=hb[:, NCTX - 1::-1], data0=a_t[:, NCTX - 1::-1], data1=b_t[:, NCTX - 1::-1],
                            initial=0.0, op0=ALU.mult, op1=ALU.add),
                             reads=[(tag, 'a'), (tag, 'b')], writes=[(tag, 'hb')])
                        S.op('dve', lambda e: e.tensor_tensor_scan(
                            out=hb[:, T - 1:NCTX - 1:-1], data0=a_t[:, T - 1:NCTX - 1:-1],
                            data1=b_t[:, T - 1:NCTX - 1:-1], initial=hb[:, 0:1], op0=ALU.mult, op1=ALU.add),
                             reads=[(tag, 'a'), (tag, 'b'), (tag, 'hb')], writes=[(tag, 'hb')])
                S.op('dve', lambda e: e.tensor_tensor(out=hf[:], in0=hf[:], in1=hb[:], op=ALU.add),
                     reads=[(tag, 'hf'), (tag, 'hb')], writes=[(tag, 'hf')])
                S.op('dve', lambda e: e.tensor_tensor(out=ob[:], in0=hf[:], in1=gy[:], op=ALU.mult),
                     reads=[(tag, 'hf'), (tag, 'gy')], writes=[(tag, 'ob')])
                S.dma('sp', brT[2, s * 512 + j * 128:s * 512 + (j + 1) * 128, :], ob[:], reads=[(tag, 'ob')],
                      writes=['brT'])
            barrier(C)

    if 'ssd' in which:
        scw_d = C.din('ssd_cw', [128, 8 * 5])
        swdt_d = C.din('ssd_wdt', [128, KC * 16])
        srow_d = C.din('ssd_row', [128, 2 * 16])
        sdsk_d = C.din('ssd_dsk', [128, 512])
        snw_d = C.din('ssd_nw', [128, 512])
        with ExitStack() as es1:
            def sbx(name, shape, dt=F32):
                return es1.enter_context(C.nc.sbuf_tensor(C.pfx + name, list(shape), dt))
            tag = 'ssd'
            scw = sbx('scw', [128, 8, 5])
            swdt = sbx('swdt', [128, KC, 16], BF16)
            srow = sbx('srow', [128, 2, 16])
            sdsk = sbx('sdsk', [128, 512])
            snw = sbx('snw', [128, 512])
            S.dma('sp', scw[:].rearrange('p a t -> p (a t)'), scw_d, writes=['scw'])
            S.dma('pool', swdt[:].rearrange('p k c -> p (k c)'), swdt_d, writes=['swdt'])
            S.dma('sp', srow[:].rearrange('p a c -> p (a c)'), srow_d, writes=['srow'])
            S.dma('sp', sdsk[:], sdsk_d, writes=['sdsk'])
            S.dma('sp', snw[:], snw_d, writes=['snw'])
            S.op('act', lambda e: e.activation(out=srow[:, 1, :], in_=srow[:, 1, :], func=AF.Exp),
                 reads=['srow'], writes=['srow'])
            S.op('dve', lambda e: e.tensor_scalar(out=srow[:, 1, :], in0=srow[:, 1, :], scalar1=-1.0, scalar2=None,
                                                  op0=ALU.mult), reads=['srow'], writes=['srow'])
            dt_tm = sbx('s_dt', [128, NCHK, 2, 8])
            la_tm = sbx('s_la', [128, NCHK, 2, 8])
            tdt = [sbx('s_tdt%d' % i, [128, 16]) for i in range(2)]
            for c in range(NCHK):
                b = c % 2
                pt, pk = proj_tm(lambda kc: swdt[:, kc, :], 'swdt', c, 16)
                S.op('dve', lambda e, pt=pt, b=b: e.tensor_tensor(out=tdt[b][:], in0=pt[:, 0:16], in1=srow[:, 0, :],
                                                                  op=ALU.add),
                     reads=[pk, 'srow'], writes=[(tag, 'tdt', b)])
                S.op('act', lambda e, b=b: e.activation(out=tdt[b][:], in_=tdt[b][:], func=AF.Exp),
                     reads=[(tag, 'tdt', b)], writes=[(tag, 'tdt', b)])
                S.op('act', lambda e, b=b, c=c: e.activation(out=dt_tm[:, c].rearrange('p d h -> p (d h)'),
                                                             in_=tdt[b][:], func=AF.Ln, bias=oneb[:, 0:1], scale=1.0),
                     reads=[(tag, 'tdt', b), 'oneb'], writes=[(tag, 'dt')])
                S.op('dve', lambda e, c=c: e.tensor_tensor(out=la_tm[:, c].rearrange('p d h -> p (d h)'),
                                                           in0=dt_tm[:, c].rearrange('p d h -> p (d h)'),
                                                           in1=srow[:, 1, :], op=ALU.mult),
                     reads=[(tag, 'dt'), 'srow'], writes=[(tag, 'la')])
            import os
            SSD_STOP = int(os.environ.get('SSD_STOP', '9'))
            xpre = sbx('s_xpre', [128, T])
            xcv = sbx('s_xcv', [128, T])
            BT = sbx('s_BT', [128, T], BF16)
            CT = sbx('s_CT', [128, T], BF16)
            xT = sbx('s_xT', [128, T], BF16)
            B_tm = sbx('s_Btm', [128, NCHK, 128], BF16)
            x_tm = sbx('s_xtm', [128, NCHK, 256], BF16)
            z_tm = sbx('s_ztm', [128, NCHK, 256], BF16)
            o_acc = sbx('s_oacc', [128, NCHK, 256])
            ob = sbx('s_ob', [128, NCHK, 256], BF16)
            ty = [sbx('s_ty%d' % i, [128, 256]) for i in range(2)]

            def conv_silu(name, cwi, dst, dkey):
                w, wk = getw(name)
                for (t0, n) in TILES_M:
                    pt, pk = proj_fm(w, wk, t0, n)
                    S.op('act', lambda e, pt=pt, t0=t0, n=n: e.activation(out=xpre[:, t0:t0 + n], in_=pt[:, 0:n],
                                                                          func=AF.Identity),
                         reads=[pk], writes=[(tag, 'xpre')])
                emit_conv(C, tag, xpre, (tag, 'xpre'), xcv, (tag, 'xcv'), lambda tp: scw[:, cwi, tp:tp + 1], 'scw')
                S.op('act', lambda e: e.activation(out=dst[:], in_=xcv[:], func=AF.Silu),
                     reads=[(tag, 'xcv')], writes=[dkey])

            for gi in range(2 if SSD_STOP > 0 else 0):
                conv_silu(('ssd_B', gi), 4 + gi, BT, (tag, 'kT'))
                conv_silu(('ssd_C', gi), 6 + gi, CT, (tag, 'qT'))
                for c in range(NCHK):
                    transpose_to(B_tm[:, c, :], (tag, 'ktm'), BT[:, c * 128:(c + 1) * 128], (tag, 'kT'))
                S.op('dve', lambda e: e.memset(o_acc[:], 0.0), writes=[(tag, 'o')])
                if SSD_STOP <= 1:
                    continue
                for pl_i in range(2):
                    pi = 2 * gi + pl_i
                    conv_silu(('ssd_x', pi), pi, xT, (tag, 'xT'))
                    for c in range(NCHK):
                        transpose_to(x_tm[:, c, pl_i * 128:(pl_i + 1) * 128], (tag, 'v'),
                                     xT[:, c * 128:(c + 1) * 128], (tag, 'xT'))
                    w, wk = getw(('ssd_z', pi))
                    for c in range(NCHK):
                        pt, pk = proj_tm(lambda kc, w=w: w[:, kc, :], wk, c, 128)
                        S.op('act', lambda e, c=c, pt=pt, pl_i=pl_i: e.activation(
                            out=z_tm[:, c, pl_i * 128:(pl_i + 1) * 128], in_=pt[:, 0:128], func=AF.Silu),
                             reads=[pk], writes=[(tag, 'z')])
                    hl0 = 2 * pi
                    heads = []
                    for e_ in range(2):
                        hl = hl0 + e_
                        heads.append(dict(
                            v0=pl_i * 128 + e_ * 64, dv=64,
                            la=[lambda c, hl=hl: la_tm[:, c, 0, hl:hl + 1], lambda c, hl=hl: la_tm[:, c, 1, hl:hl + 1]],
                            dt=[lambda c, hl=hl: dt_tm[:, c, 0, hl:hl + 1], lambda c, hl=hl: dt_tm[:, c, 1, hl:hl + 1]]))
                    if SSD_STOP <= 2:
                        continue
                    with ExitStack() as es2:
                        def sby(name, shape, dt=F32, pi=pi):
                            return es2.enter_context(C.nc.sbuf_tensor(C.pfx + name + '_%d' % pi, list(shape), dt))
                        scalar_scan(sby, tag, CT, BT, B_tm, x_tm, heads,
                                    lambda c, d, hl0=hl0: (la_tm[:, c, d, hl0:hl0 + 2], (tag, 'la')), o_acc, (tag, 'o'))
                        barrier(C)
                if SSD_STOP <= 3:
                    continue
                gc = slice(gi * 256, (gi + 1) * 256)
                for c in range(NCHK):
                    b = c % 2
                    S.op('dve', lambda e, c=c, b=b: e.tensor_tensor(out=ty[b][:], in0=x_tm[:, c, :], in1=sdsk[:, gc],
                                                                    op=ALU.mult),
                         reads=[(tag, 'v'), 'sdsk'], writes=[(tag, 'ty', b)])
                    S.op('dve', lambda e, c=c, b=b: e.tensor_tensor(out=ty[b][:], in0=ty[b][:], in1=o_acc[:, c, :],
                                                                    op=ALU.add),
                         reads=[(tag, 'ty', b), (tag, 'o')], writes=[(tag, 'ty', b)])
                    S.op('dve', lambda e, c=c, b=b: e.tensor_tensor(out=o_acc[:, c, :], in0=ty[b][:], in1=z_tm[:, c, :],
                                                                    op=ALU.mult),
                         reads=[(tag, 'ty', b), (tag, 'z'), (tag, 'o')], writes=[(tag, 'o')])
                if SSD_STOP <= 4:
                    continue
                with ExitStack() as es2:
                    def sby(name, shape, dt=F32, gi=gi):
                        return es2.enter_context(C.nc.sbuf_tensor(C.pfx + name + '_g%d' % gi, list(shape), dt))
                    rs, rsk = rms_rows(sby, tag, o_acc, (tag, 'o'), 256)
                    for c in range(NCHK):
                        S.op('dve', lambda e, c=c: e.scalar_tensor_tensor(
                            out=ob[:, c, :], in0=o_acc[:, c, :], scalar=rs[:, c:c + 1], in1=snw[:, gc],
                            op0=ALU.mult, op1=ALU.mult),
                             reads=[(tag, 'o'), rsk, 'snw'], writes=[(tag, 'ob')])
                    for hf_ in range(2):
                        c0_ = gi * 256 + hf_ * 128
                        out_tm(1, s * 512 + c0_, lambda c, hf_=hf_: ob[:, c, hf_ * 128:(hf_ + 1) * 128], (tag, 'ob'))
                    barrier(C)
            barrier(C)

    if 'hg' in which:
        hlbl_d = C.din('hg_lbl', [128, 8 * 4])
        hnw_d = C.din('hg_nw', [128, 512])
        with ExitStack() as es1:
            def sbx(name, shape, dt=F32):
                return es1.enter_context(C.nc.sbuf_tensor(C.pfx + name, list(shape), dt))
            tag = 'hg'
            lbl = sbx('h_lbl', [128, 8, 4])
            hnw = sbx('h_nw', [128, 512])
            lbs = sbx('h_lbs', [128, 8, 4])
            S.dma('sp', lbl[:].rearrange('p a l -> p (a l)'), hlbl_d, writes=['hlbl'])
            S.dma('sp', hnw[:], hnw_d, writes=['hnw'])
            for a_ in range(8):
                S.op('dve', lambda e, a_=a_: e.tensor_reduce(out=lbs[:, a_, 0:1], in_=lbl[:, a_, :], axis=AX.X,
                                                             op=ALU.max), reads=['hlbl'], writes=['hlbs'])
                S.op('dve', lambda e, a_=a_: e.tensor_scalar(out=lbl[:, a_, :], in0=lbl[:, a_, :],
                                                             scalar1=lbs[:, a_, 0:1], scalar2=None, op0=ALU.subtract),
                     reads=['hlbl', 'hlbs'], writes=['hlbl'])
            S.op('act', lambda e: e.activation(out=lbl[:].rearrange('p a l -> p (a l)'),
                                               in_=lbl[:].rearrange('p a l -> p (a l)'), func=AF.Exp),
                 reads=['hlbl'], writes=['hlbl'])
            for a_ in range(8):
                S.op('dve', lambda e, a_=a_: e.tensor_reduce(out=lbs[:, a_, 0:1], in_=lbl[:, a_, :], axis=AX.X,
                                                             op=ALU.add), reads=['hlbl'], writes=['hlbs'])
                S.op('dve', lambda e, a_=a_: e.reciprocal(out=lbs[:, a_, 0:1], in_=lbs[:, a_, 0:1]),
                     reads=['hlbs'], writes=['hlbs'])
                if layer == 0:
                    S.op('dve', lambda e, a_=a_: e.memset(lbs[:, a_, 1:2], 0.0), reads=['hlbs'], writes=['hlbs'])
                else:
                    S.op('dve', lambda e, a_=a_: e.tensor_reduce(out=lbs[:, a_, 1:2], in_=lbl[:, a_, 1:layer + 1],
                                                                 axis=AX.X, op=ALU.add),
                         reads=['hlbl', 'hlbs'], writes=['hlbs'])
                S.op('dve', lambda e, a_=a_: e.tensor_tensor(out=lbs[:, a_, 2:3], in0=lbs[:, a_, 1:2],
                                                             in1=lbs[:, a_, 0:1], op=ALU.mult),
                     reads=['hlbs'], writes=['hlbs'])
                S.op('dve', lambda e, a_=a_: e.tensor_scalar(out=lbs[:, a_, 3:4], in0=lbs[:, a_, 2:3], scalar1=-1.0,
                                                             scalar2=1.0, op0=ALU.mult, op1=ALU.add),
                     reads=['hlbs'], writes=['hlbs'])
            qT = sbx('h_qT', [128, T], BF16)
            la_d = [sbx('h_la%d' % d, [128, T]) for d in range(2)]
            kd = [sbx('h_kd%d' % d, [128, T], BF16) for d in range(2)]
            v_tm = sbx('h_vtm', [128, NCHK, 128], BF16)
            g_tm = sbx('h_gtm', [128, NCHK, 128], BF16)
            o_acc = sbx('h_oacc', [128, NCHK, 128])
            ob = sbx('h_ob', [128, NCHK, 128], BF16)
            tf = [sbx('h_tf%d' % i, [128, 512]) for i in range(2)]
            Sst = sbx('h_S', [128, 128])
            Sbf = sbx('h_Sb', [128, 128], BF16)
            cum = [sbx('h_cum%d' % i, [128, 128]) for i in range(2)]
            e1 = [sbx('h_e1%d' % i, [128, 128]) for i in range(2)]
            e2 = [sbx('h_e2%d' % i, [128, 128]) for i in range(2)]
            e3 = [sbx('h_e3%d' % i, [128, 128]) for i in range(2)]
            e4 = [sbx('h_e4%d' % i, [128, 128]) for i in range(2)]
            qt = [sbx('h_qt%d' % i, [128, 128], BF16) for i in range(2)]
            kt = [sbx('h_kt%d' % i, [128, 128], BF16) for i in range(2)]
            qh = [sbx('h_qh%d' % i, [128, 128], BF16) for i in range(2)]
            kh = [sbx('h_kh%d' % i, [128, 128], BF16) for i in range(2)]
            khT = [sbx('h_khT%d' % i, [128, 128], BF16) for i in range(2)]
            Pm = [sbx('h_P%d' % i, [128, 128], BF16) for i in range(2)]
            at = [sbx('h_at%d' % i, [128, 1]) for i in range(2)]
            for j in range(4):
                w, wk = getw(('hg_q', j))
                for (t0, n) in TILES_M:
                    pt, pk = proj_fm(w, wk, t0, n)
                    S.op('act', lambda e, pt=pt, t0=t0, n=n: e.activation(out=qT[:, t0:t0 + n], in_=pt[:, 0:n],
                                                                          func=AF.Silu),
                         reads=[pk], writes=[(tag, 'qT')])
                for d in range(2):
                    a_ = d * 4 + j
                    w, wk = getw(('hg_f%d' % d, j))
                    for ti, (t0, n) in enumerate(TILES_M):
                        b = ti % 2
                        pt, pk = proj_fm(w, wk, t0, n)
                        S.op('act', lambda e, pt=pt, b=b, n=n: e.activation(out=tf[b][:, 0:n], in_=pt[:, 0:n],
                                                                            func=AF.Sigmoid),
                             reads=[pk], writes=[(tag, 'tf', b)])
                        S.op('dve', lambda e, b=b, n=n, a_=a_: e.tensor_scalar(
                            out=tf[b][:, 0:n], in0=tf[b][:, 0:n], scalar1=lbs[:, a_, 3:4], scalar2=lbs[:, a_, 2:3],
                            op0=ALU.mult, op1=ALU.add), reads=[(tag, 'tf', b), 'hlbs'], writes=[(tag, 'tf', b)])
                        S.op('dve', lambda e, b=b, n=n, t0=t0, d=d: e.tensor_scalar(
                            out=kd[d][:, t0:t0 + n], in0=tf[b][:, 0:n], scalar1=-1.0, scalar2=1.0,
                            op0=ALU.mult, op1=ALU.add), reads=[(tag, 'tf', b)], writes=[(tag, 'kd', d)])
                        S.op('dve', lambda e, b=b, n=n: e.tensor_scalar(
                            out=tf[b][:, 0:n], in0=tf[b][:, 0:n], scalar1=1e-18, scalar2=None, op0=ALU.max),
                             reads=[(tag, 'tf', b)], writes=[(tag, 'tf', b)])
                        S.op('act', lambda e, b=b, n=n, t0=t0, d=d: e.activation(out=la_d[d][:, t0:t0 + n],
                                                                                 in_=tf[b][:, 0:n], func=AF.Ln),
                             reads=[(tag, 'tf', b)], writes=[(tag, 'la', d)])
                w, wk = getw(('hg_i', j))
                for c in range(NCHK):
                    pt, pk = proj_tm(lambda kc, w=w: w[:, kc, :], wk, c, 128)
                    S.op('act', lambda e, c=c, pt=pt: e.activation(out=v_tm[:, c, :], in_=pt[:, 0:128],
                                                                   func=AF.Identity),
                         reads=[pk], writes=[(tag, 'v')])
                w, wk = getw(('hg_g', j))
                for c in range(NCHK):
                    pt, pk = proj_tm(lambda kc, w=w: w[:, kc, :], wk, c, 128)
                    S.op('act', lambda e, c=c, pt=pt: e.activation(out=g_tm[:, c, :], in_=pt[:, 0:128], func=AF.Silu),
                         reads=[pk], writes=[(tag, 'g')])
                S.op('dve', lambda e: e.memset(o_acc[:], 0.0), writes=[(tag, 'o')])
                it = 0
                for d, order in ((0, FWD_ORDER), (1, BWD_ORDER)):
                    msk = M_le if d == 0 else M_ge
                    last = 127 if d == 0 else 0
                    S.op('dve', lambda e: e.memset(Sst[:], 0.0), writes=[(tag, 'S')])
                    S.op('act', lambda e: e.activation(out=Sbf[:], in_=Sst[:], func=AF.Identity),
                         reads=[(tag, 'S')], writes=[(tag, 'Sb')])
                    for c in order:
                        i2 = it % 2
                        it += 1
                        c0, c1 = c * 128, (c + 1) * 128
                        if d == 0:
                            src = la_d[d][:, c0:c1]
                            dst = cum[i2][:]
                        else:
                            src = la_d[d][:, c1 - 1:(c0 - 1 if c0 > 0 else None):-1]
                            dst = cum[i2][:, ::-1]
                        S.op('dve', lambda e, src=src, dst=dst: e.tensor_tensor_scan(
                            out=dst, data0=C.ones[:], data1=src, initial=0.0, op0=ALU.mult, op1=ALU.add),
                             reads=[(tag, 'la', d), 'ones'], writes=[(tag, 'cum', i2)])
                        ck = (tag, 'cum', i2)
                        cm = cum[i2]
                        def refcol(I, cm=cm):
                            if d == 0:
                                return cm[:, I * 32 - 1:I * 32] if I > 0 else 0.0
                            return cm[:, (I + 1) * 32:(I + 1) * 32 + 1] if I < 3 else 0.0
                        for I in range(4):
                            S.op('dve', lambda e, cm=cm, i2=i2, I=I: e.tensor_scalar(
                                out=e1[i2][:, I * 32:(I + 1) * 32], in0=cm[:, I * 32:(I + 1) * 32], scalar1=refcol(I),
                                scalar2=None, op0=ALU.subtract), reads=[ck], writes=[(tag, 'e1', i2)])
                        S.op('act', lambda e, i2=i2: e.activation(out=e1[i2][:], in_=e1[i2][:], func=AF.Exp),
                             reads=[(tag, 'e1', i2)], writes=[(tag, 'e1', i2)])
                        S.op('dve', lambda e, i2=i2: e.tensor_tensor(out=qt[i2][:], in0=qT[:, c0:c1], in1=e1[i2][:],
                                                                     op=ALU.mult),
                             reads=[(tag, 'qT'), (tag, 'e1', i2)], writes=[(tag, 'qt', i2)])
                        psc, psck = C.psb[6], ('ps', 6)
                        for I in range(4):
                            ik = I % 2
                            S.op('dve', lambda e, cm=cm, ik=ik, I=I: e.tensor_scalar(
                                out=e2[ik][:], in0=cm[:], scalar1=refcol(I), scalar2=-80.0, op0=ALU.subtract,
                                op1=ALU.max), reads=[ck], writes=[(tag, 'e2', ik)])
                            S.op('act', lambda e, ik=ik: e.activation(out=e2[ik][:], in_=e2[ik][:], func=AF.Exp,
                                                                      scale=-1.0),
                                 reads=[(tag, 'e2', ik)], writes=[(tag, 'e2', ik)])
                            S.op('dve', lambda e, ik=ik, d=d: e.tensor_tensor(out=kt[ik][:], in0=kd[d][:, c0:c1],
                                                                              in1=e2[ik][:], op=ALU.mult),
                                 reads=[(tag, 'kd', d), (tag, 'e2', ik)], writes=[(tag, 'kt', ik)])
                            S.op('pe', lambda e, ik=ik, i2=i2, psc=psc, I=I: e.matmul(
                                psc[:, I * 32:(I + 1) * 32], kt[ik][:], qt[i2][:, I * 32:(I + 1) * 32],
                                start=True, stop=True),
                                 reads=[(tag, 'kt', ik), (tag, 'qt', i2)], writes=[psck])
                        S.op('dve', lambda e, i2=i2, psc=psc: e.tensor_tensor(out=Pm[i2][:], in0=psc[:, 0:128],
                                                                              in1=msk, op=ALU.mult),
                             reads=[psck, 'masks'], writes=[(tag, 'P', i2)])
                        S.op('act', lambda e, cm=cm, i2=i2: e.activation(out=e3[i2][:], in_=cm[:], func=AF.Exp),
                             reads=[ck], writes=[(tag, 'e3', i2)])
                        S.op('dve', lambda e, i2=i2: e.tensor_tensor(out=qh[i2][:], in0=qT[:, c0:c1], in1=e3[i2][:],
                                                                     op=ALU.mult),
                             reads=[(tag, 'qT'), (tag, 'e3', i2)], writes=[(tag, 'qh', i2)])
                        po, pok = C.ps()
                        S.op('pe', lambda e, i2=i2, po=po, c=c: e.matmul(po[:, 0:128], Pm[i2][:], v_tm[:, c, :],
                                                                         start=True, stop=False),
                             reads=[(tag, 'P', i2), (tag, 'v')], writes=[pok])
                        S.op('pe', lambda e, i2=i2, po=po: e.matmul(po[:, 0:128], qh[i2][:], Sbf[:],
                                                                    start=False, stop=True),
                             reads=[(tag, 'qh', i2), (tag, 'Sb')], writes=[pok])
                        S.op('dve', lambda e, po=po, c=c: e.tensor_tensor(out=o_acc[:, c, :], in0=po[:, 0:128],
                                                                          in1=o_acc[:, c, :], op=ALU.add),
                             reads=[pok, (tag, 'o')], writes=[(tag, 'o')])
                        S.op('dve', lambda e, cm=cm, i2=i2: e.tensor_scalar(
                            out=e4[i2][:], in0=cm[:], scalar1=cm[:, last:last + 1], scalar2=None, op0=ALU.subtract),
                             reads=[ck], writes=[(tag, 'e4', i2)])
                        S.op('act', lambda e, i2=i2: e.activation(out=e4[i2][:], in_=e4[i2][:], func=AF.Exp,
                                                                  scale=-1.0),
                             reads=[(tag, 'e4', i2)], writes=[(tag, 'e4', i2)])
                        S.op('dve', lambda e, i2=i2, d=d: e.tensor_tensor(out=kh[i2][:], in0=kd[d][:, c0:c1],
                                                                          in1=e4[i2][:], op=ALU.mult),
                             reads=[(tag, 'kd', d), (tag, 'e4', i2)], writes=[(tag, 'kh', i2)])
                        transpose_to(khT[i2][:], (tag, 'khT', i2), kh[i2][:], (tag, 'kh', i2))
                        pc_, pck = C.ps()
                        S.op('pe', lambda e, i2=i2, pc_=pc_, c=c: e.matmul(pc_[:, 0:128], khT[i2][:], v_tm[:, c, :],
                                                                           start=True, stop=True),
                             reads=[(tag, 'khT', i2), (tag, 'v')], writes=[pck])
                        S.op('act', lambda e, cm=cm, i2=i2: e.activation(out=at[i2][:], in_=cm[:, last:last + 1],
                                                                         func=AF.Exp),
                             reads=[ck], writes=[(tag, 'at', i2)])
                        S.op('dve', lambda e, i2=i2, pc_=pc_: e.scalar_tensor_tensor(
                            out=Sst[:], in0=Sst[:], scalar=at[i2][:, 0:1], in1=pc_[:, 0:128], op0=ALU.mult,
                            op1=ALU.add), reads=[(tag, 'S'), (tag, 'at', i2), pck], writes=[(tag, 'S')])
                        S.op('act', lambda e: e.activation(out=Sbf[:], in_=Sst[:], func=AF.Identity),
                             reads=[(tag, 'S')], writes=[(tag, 'Sb')])
                with ExitStack() as es2:
                    def sby(name, shape, dt=F32, j=j):
                        return es2.enter_context(C.nc.sbuf_tensor(C.pfx + name + '_%d' % j, list(shape), dt))
                    rs, rsk = rms_rows(sby, tag, o_acc, (tag, 'o'), 128)
                    for c in range(NCHK):
                        S.op('dve', lambda e, c=c: e.scalar_tensor_tensor(
                            out=o_acc[:, c, :], in0=o_acc[:, c, :], scalar=rs[:, c:c + 1],
                            in1=hnw[:, j * 128:(j + 1) * 128], op0=ALU.mult, op1=ALU.mult),
                             reads=[(tag, 'o'), rsk, 'hnw'], writes=[(tag, 'o')])
                        S.op('dve', lambda e, c=c: e.tensor_tensor(out=ob[:, c, :], in0=o_acc[:, c, :],
                                                                   in1=g_tm[:, c, :], op=ALU.mult),
                             reads=[(tag, 'o'), (tag, 'g')], writes=[(tag, 'ob')])
                    out_tm(3, s * 512 + j * 128, lambda c: ob[:, c, :], (tag, 'ob'))
                    barrier(C)
            barrier(C)
    barrier(C)
    C.pop()


def barrier(C):
    S = C.S
    evs = []
    for e in S.csem:
        if S.ccnt[e] > 0:
            evs.append(('c_' + e, S.csem[e], S.ccnt[e], 'x'))
    for q in S.dsem:
        for i in range(S.ring):
            if S.dcnt[q][i] > 0:
                evs.append(('d_%s%d' % (q, i), S.dsem[q][i], S.dcnt[q][i], 'dma'))
    for e in ('pe', 'act', 'dve', 'pool', 'sp'):
        for ev in evs:
            S._ensure(e, ev)


def _const_tables():
    a = np.arange(128)
    le = (a[:, None] <= a[None, :]).astype(np.float32)
    ge = (a[:, None] >= a[None, :]).astype(np.float32)
    gt = (a[:, None] > a[None, :]).astype(np.float32)
    lt = (a[:, None] < a[None, :]).astype(np.float32)
    masks = np.concatenate([le, ge, gt, lt], axis=1)
    pmat = np.zeros((128, 128), np.float32)
    for m in range(64):
        pmat[m + 64, m] = -1.0
        pmat[m, m + 64] = 1.0
    t = np.arange(NLAT)
    r, col = (t // 64).astype(np.float32), (t % 64).astype(np.float32)
    freqs = (np.float32(10000.0) ** (-np.arange(32, dtype=np.float32) / np.float32(32))).astype(np.float32)
    ang = np.concatenate([r[:, None] * freqs[None], col[:, None] * freqs[None]], axis=1).astype(np.float32)
    cos = np.ones((T, 64), np.float32)
    sin = np.zeros((T, 64), np.float32)
    cos[NCTX:] = np.cos(ang)
    sin[NCTX:] = np.sin(ang)
    rope = np.zeros((128, 2, T), np.float32)
    rope[:, 0, :] = np.concatenate([cos.T, cos.T], axis=0)
    rope[:, 1, :] = np.concatenate([sin.T, sin.T], axis=0)
    lg = np.log1p(-np.exp2(-5.0 - np.arange(8, dtype=np.float32))).astype(np.float32)
    return masks, pmat, rope.reshape(128, 2 * T), lg


_CT = _const_tables()
IDENTB = np.eye(128, dtype=np.float32).astype(ml_dtypes.bfloat16)


def lay_wM(w_in_l, s):
    out = np.empty((NCH_M, 128, KC * 128), np.float32)
    for i, (nm, c0) in enumerate(mixer_chunks(s)):
        out[i] = w_in_l[:, c0:c0 + 128].reshape(KC, 128, 128).transpose(1, 0, 2).reshape(128, KC * 128)
    return out


def mixer_in_map(layer, s, inp):
    which = ('ret', 'ssd', 'lru', 'hg')
    masks, pmat, rope, lg = _CT
    m = {'n1w': lay_vec(inp['norm1_w'][layer]), 'wM': lay_wM(inp['w_in'][layer], s)}
    m['retla'] = np.ascontiguousarray(np.broadcast_to(lg[4 * s:4 * s + 4][None, :], (128, 4)))
    if 'ssd' in which:
        cw = inp['ssd_conv_w'][layer]
        cb = inp['ssd_conv_b'][layer]
        ch0 = [(4 * s + pi) * 128 for pi in range(4)] + [1024 + (2 * s + gi) * 128 for gi in range(2)] + \
              [1536 + (2 * s + gi) * 128 for gi in range(2)]
        p = np.zeros((128, 8, 5), np.float32)
        for i, c0 in enumerate(ch0):
            p[:, i, 0:4] = cw[:, c0:c0 + 128].T
            p[:, i, 4] = cb[c0:c0 + 128]
        m['ssd_cw'] = p.reshape(128, 40)
        cols = np.concatenate([OFF['ssd_dt'] + d * 16 + 8 * s + np.arange(8) for d in range(2)])
        wdt = inp['w_in'][layer][:, cols]
        m['ssd_wdt'] = np.ascontiguousarray(wdt.reshape(KC, 128, 16).transpose(1, 0, 2).reshape(128, KC * 16))
        row = np.concatenate([inp['ssd_dt_bias'][layer][:, 8 * s:8 * s + 8].reshape(-1),
                              inp['ssd_a_log'][layer][:, 8 * s:8 * s + 8].reshape(-1)])
        m['ssd_row'] = np.ascontiguousarray(np.broadcast_to(row[None, :], (128, 32)))
        dsk = np.repeat(inp['ssd_d'][layer][8 * s:8 * s + 8], 64)
        m['ssd_dsk'] = np.ascontiguousarray(np.broadcast_to(dsk[None, :], (128, 512)))
        m['ssd_nw'] = np.ascontiguousarray(np.broadcast_to(inp['ssd_norm_w'][layer][512 * s:512 * s + 512][None, :], (128, 512)))
    if 'hg' in which:
        lg_ = inp['hg_lb_logits']
        a = np.zeros((128, 2, 4, DEPTH), np.float32)
        for d in range(2):
            for j in range(4):
                hh = 4 * s + j
                a[:, d, j, :] = lg_[d, :, hh * 128:(hh + 1) * 128].T
        m['hg_lbl'] = a.reshape(128, 32)
        m['hg_nw'] = np.ascontiguousarray(np.broadcast_to(inp['hg_norm_w'][layer][512 * s:512 * s + 512][None, :], (128, 512)))
    if 'lru' in which:
        blk = [4 * s + j for j in range(4)]
        cw = inp['lru_conv_w'][layer]
        cb = inp['lru_conv_b'][layer]
        p = np.zeros((128, 4, 5), np.float32)
        for j, g in enumerate(blk):
            p[:, j, 0:4] = cw[:, g * 128:(g + 1) * 128].T
            p[:, j, 4] = cb[g * 128:(g + 1) * 128]
        m['lru_p'] = p.reshape(128, 20)
        gbv = np.zeros((128, 2, 2, 4), np.float32)
        lw = np.zeros((128, 2, 2, 4, 128), np.float32)
        lam = np.zeros((128, 2, 4), np.float32)
        for d in range(2):
            for j, g in enumerate(blk):
                gbv[:, 0, d, j] = inp['lru_ba'][layer][d, g * 128:(g + 1) * 128]
                gbv[:, 1, d, j] = inp['lru_bx'][layer][d, g * 128:(g + 1) * 128]
                lw[:, 0, d, j, :] = inp['lru_wa'][layer][d, g]
                lw[:, 1, d, j, :] = inp['lru_wx'][layer][d, g]
                lam[:, d, j] = inp['lru_lambda'][layer][d, g * 128:(g + 1) * 128]
        m['lru_gb'] = gbv.reshape(128, 16)
        m['lru_w'] = lw.reshape(128, 16 * 128)
        m['lru_lam'] = lam.reshape(128, 8)
    return m


def build_fused(layers, dbg=False):
    C = Ctx('fused')
    emit_consts(C)
    emit_eps(C)
    modd = emit_ada(C, layers)
    C.pfx = ''
    hT0 = C.din('hT0', [D, T], shared=True)
    hS = C.dint('hS', [D, T])
    brT = C.dint('brT', [4, MIXW, T], BF16)
    yout = C.dout('yout', [D, T])
    hdbg = C.dout('hdbg', [D, T]) if dbg else None
    h_src, hkey = hT0, 'hT0'
    for li, l in enumerate(layers):
        C.push()
        C.pfx = 'L%dm_' % l
        xst = {'xnT': C.sb('xnT', [128, KC, T], BF16)}
        for s in (0, 1):
            emit_mixer(C, l, li, s, h_src, hkey, brT, modd, xst)
        C.pop()
        final = (li == len(layers) - 1)
        emit_token(C, l, li, l % 2 == 1, final, h_src, hkey, hS, 'hS', brT, modd,
                   yout if final else None, hdbg if final else None)
        h_src, hkey = hS, 'hS'
    names = list(C.inputs.keys())
    print('fused program: ninst', C.S.ninst, 'nwait', C.S.nwait)
    return C.close(), names


def fused_in_maps(inp, layers, names):
    masks, pmat, rope, lg = _CT
    sh = {'ident': IDENT, 'masks': masks, 'identb': IDENTB, 'rope': rope, 'pmat': pmat}
    sh['ada_w'] = np.ascontiguousarray(inp['ada_w'][list(layers)])
    sh['ada_b'] = np.ascontiguousarray(np.stack([inp['ada_b'][l].reshape(96, 128).T for l in layers]))
    for li, l in enumerate(layers):
        for s in (0, 1):
            for k, v in mixer_in_map(l, s, inp).items():
                sh['L%ds%d_%s' % (l, s, k)] = v
        for k, v in token_in_map(l, inp, l % 2 == 1, li == len(layers) - 1).items():
            sh['L%d_%s' % (l, k)] = v
    in_maps = []
    for i in range(8):
        b = i % NB
        cond2 = np.stack([inp['c'][b], inp['c_ctx']], axis=0)
        m = {k: sh[k] for k in names if k in sh}
        m['ada_cond'] = np.ascontiguousarray(cond2.T.reshape(KC, 128, 2).transpose(1, 0, 2).reshape(128, KC * 2))
        m['hT0'] = np.ascontiguousarray(np.concatenate([inp['ctx'][b], inp['x'][b]], axis=0).T)
        missing = [k for k in names if k not in m]
        assert not missing, missing
        in_maps.append(m)
    return in_maps


def kernel(**inputs):
    inp = {k: np.asarray(v) for k, v in inputs.items()}
    layers = list(range(DEPTH))
    nc, names = build_fused(layers)
    res = run(nc, fused_in_maps(inp, layers, names))
    out = np.stack([np.ascontiguousarray(res[b]['yout'][:, NCTX:].T) for b in range(NB)], axis=0)
    return out.astype(np.float32)
```

```python
import numpy as np
import ml_dtypes
from contextlib import ExitStack
import concourse.bass as bass
import concourse.mybir as mybir
from concourse.bass_utils import run_bass_kernel_spmd

F32 = mybir.dt.float32
BF16 = mybir.dt.bfloat16
AF = mybir.ActivationFunctionType
ALU = mybir.AluOpType
AX = mybir.AxisListType

D = 2048
NB = 4
NCTX = 256
NLAT = 2048
T = NCTX + NLAT
DEPTH = 4
MIXW = 1024
KC = D // 128


class Sched:
    def __init__(self, nc, es, ring=8):
        self.nc = nc
        self.engs = {'pe': nc.tensor, 'act': nc.scalar, 'dve': nc.vector, 'pool': nc.gpsimd, 'sp': nc.sync}
        self.csem = {e: es.enter_context(nc.semaphore('c_' + e)) for e in ('pe', 'act', 'dve', 'pool')}
        self.ccnt = {e: 0 for e in self.csem}
        self.ring = ring
        self.dsem = {q: [es.enter_context(nc.semaphore('d_%s%d' % (q, i))) for i in range(ring)]
                     for q in ('sp', 'pool')}
        self.dcnt = {q: [0] * ring for q in self.dsem}
        self.dnext = {q: 0 for q in self.dsem}
        self.seen = {e: {} for e in self.engs}
        self.res = {}
        self.nwait = 0
        self.ninst = 0

    def _ensure(self, e, ev):
        name, sem, val, prod = ev
        if prod == 'pe' and e == 'pe':
            return
        if self.seen[e].get(name, 0) >= val:
            return
        self.engs[e].wait_ge(sem, val)
        self.nwait += 1
        self.seen[e][name] = val

    def _deps(self, e, reads, writes):
        for k in reads:
            r = self.res.get(k)
            if r is not None and r[0] is not None:
                self._ensure(e, r[0])
        for k in writes:
            r = self.res.get(k)
            if r is not None:
                if r[0] is not None:
                    self._ensure(e, r[0])
                for ev in r[1].values():
                    self._ensure(e, ev)

    def _record(self, ev, reads, writes):
        for k in reads:
            r = self.res.setdefault(k, [None, {}])
            old = r[1].get(ev[0])
            if old is None or old[2] < ev[2]:
                r[1][ev[0]] = ev
        for k in writes:
            self.res[k] = [ev, {}]

    def op(self, e, fn, reads=(), writes=()):
        self._deps(e, reads, writes)
        inst = fn(self.engs[e])
        self.ccnt[e] += 1
        inst.then_inc(self.csem[e], 1)
        self.ninst += 1
        ev = ('c_' + e, self.csem[e], self.ccnt[e], e)
        self._record(ev, reads, writes)
        return ev

    def dma(self, q, out, in_, reads=(), writes=(), **kw):
        i = self.dnext[q]
        self.dnext[q] = (i + 1) % self.ring
        sem = self.dsem[q][i]
        name = 'd_%s%d' % (q, i)
        if self.dcnt[q][i] > 0:
            self._ensure(q, (name, sem, self.dcnt[q][i], 'dma'))
        self._deps(q, reads, writes)
        self.engs[q].dma_start(out=out, in_=in_, **kw).then_inc(sem, 16)
        self.dcnt[q][i] += 16
        self.ninst += 1
        ev = (name, sem, self.dcnt[q][i], 'dma')
        self._record(ev, reads, writes)
        return ev

    def finish(self):
        for e in self.csem:
            if self.ccnt[e] > 0:
                self._ensure('sp', ('c_' + e, self.csem[e], self.ccnt[e], e))
        for q in self.dsem:
            for i in range(self.ring):
                if self.dcnt[q][i] > 0:
                    self._ensure('sp', ('d_%s%d' % (q, i), self.dsem[q][i], self.dcnt[q][i], 'dma'))


class Ctx:
    def __init__(self, name):
        self.nc = bass.Bass("TRN2", target_bir_lowering=False)
        self.es = ExitStack()
        self.S = Sched(self.nc, self.es)
        self.nps = 0
        self.psb = [self.es.enter_context(self.nc.psum_tensor('psb%d' % i, [128, 512], F32)) for i in range(7)]
        self.pbf = self.es.enter_context(self.nc.psum_tensor('pbf', [128, 1024], BF16))
        self.psi = 0
        self.nrot = 6
        self.pfx = ''
        self.stacks = [self.es]
        self.inputs = {}

    def sb(self, name, shape, dt=F32):
        return self.stacks[-1].enter_context(self.nc.sbuf_tensor(self.pfx + name, list(shape), dt))

    def din(self, name, shape, dt=F32, shared=False):
        nm = name if shared else self.pfx + name
        if nm not in self.inputs:
            self.inputs[nm] = self.nc.dram_tensor(nm, list(shape), dt, kind="ExternalInput").ap()
        return self.inputs[nm]

    def dint(self, name, shape, dt=F32):
        return self.nc.dram_tensor(name, list(shape), dt, kind="Internal").ap()

    def push(self):
        self.stacks.append(ExitStack())

    def pop(self):
        self.stacks.pop().close()

    def dout(self, name, shape, dt=F32):
        return self.nc.dram_tensor(name, list(shape), dt, kind="ExternalOutput").ap()

    def ps(self):
        i = self.psi
        self.psi = (i + 1) % self.nrot
        return self.psb[i], ('ps', i)

    def close(self):
        self.S.finish()
        self.es.close()
        return self.nc


def run(nc, in_maps):
    res = run_bass_kernel_spmd(nc, in_maps, core_ids=list(range(8)))
    return res.results


def emit_ada(C, layers):
    S = C.S
    C.pfx = 'ada_'
    C.push()
    NJ = 96
    nl = len(layers)
    w = C.din('w', [nl, D, NJ * 128])
    bia = C.din('b', [nl, 128, NJ])
    cond = C.din('cond', [128, KC * 2])
    modd = C.dint('modd', [nl, 128, NJ * 2])
    wt = [C.sb('wt%d' % i, [128, KC, 512]) for i in range(2)]
    bt = C.sb('bt', [128, NJ])
    ct = C.sb('ct', [128, KC * 2])
    cs = C.sb('cs', [128, KC, 2])
    ot = C.sb('ot', [128, NJ, 2])
    S.dma('sp', ct[:], cond, writes=['ct'])
    S.op('act', lambda e: e.activation(out=cs[:].rearrange('p k r -> p (k r)'), in_=ct[:], func=AF.Silu),
         reads=['ct'], writes=['cs'])
    for li in range(nl):
        S.dma('sp', bt[:], bia[li], writes=['bt'])
        wv = w[li].rearrange('(k p) c -> p k c', p=128)
        for blk in range(NJ // 4):
            b = blk % 2
            S.dma('sp', wt[b][:], wv[:, :, blk * 512:(blk + 1) * 512], writes=[('wt', b)])
            for jj in range(4):
                j = blk * 4 + jj
                pt, pk = C.ps()
                for kc in range(KC):
                    S.op('pe', lambda e, kc=kc, jj=jj, pt=pt, b=b: e.matmul(
                        pt[:, 0:2], wt[b][:, kc, jj * 128:(jj + 1) * 128], cs[:, kc, :],
                        start=(kc == 0), stop=(kc == KC - 1)),
                         reads=[('wt', b), 'cs'], writes=[pk])
                S.op('act', lambda e, j=j, pt=pt: e.activation(out=ot[:, j, :], in_=pt[:, 0:2], func=AF.Identity,
                                                               bias=bt[:, j:j + 1], scale=1.0),
                     reads=[pk, 'bt'], writes=['ot'])
        S.dma('sp', modd[li], ot[:].rearrange('p j r -> p (j r)'), reads=['ot'], writes=[('modd', li)])
    barrier(C)
    C.pop()
    return modd


def emit_consts(C):
    S = C.S
    C.ones = C.sb('ones', [128, 128])
    S.op('dve', lambda e: e.memset(C.ones[:], 1.0), writes=['ones'])
    C.ident_d = C.din('ident', [128, 128], shared=True)
    C.ident = C.sb('ident_s', [128, 128])
    S.dma('sp', C.ident[:], C.ident_d, writes=['ident'])


def emit_rmsnorm(C, tag, h_of, hkey, xn_of, xnkey, subs, scale_ap, shift_ap, tmp):
    S = C.S
    for (off, n, idx) in subs:
        pt, pk = C.ps()
        for kc in range(KC):
            b = kc % 2
            S.op('act', lambda e, kc=kc, b=b: e.activation(out=tmp['sq'][b][:, 0:n], in_=h_of(kc, off, n),
                                                           func=AF.Square),
                 reads=[hkey], writes=[(tag, 'sq', b)])
            S.op('pe', lambda e, kc=kc, b=b, pt=pt: e.matmul(pt[:, 0:n], C.ones[:], tmp['sq'][b][:, 0:n],
                                                             start=(kc == 0), stop=(kc == KC - 1)),
                 reads=[(tag, 'sq', b), 'ones'], writes=[pk])
        rs = tmp['rstd']
        S.op('act', lambda e, pt=pt: e.activation(out=rs[:, off:off + n], in_=pt[:, 0:n], func=AF.Sqrt,
                                                  bias=C.epsb[:, 0:1], scale=1.0 / D),
             reads=[pk, 'epsb'], writes=[(tag, 'rstd')])
        S.op('dve', lambda e: e.reciprocal(out=rs[:, off:off + n], in_=rs[:, off:off + n]),
             reads=[(tag, 'rstd')], writes=[(tag, 'rstd')])
        for kc in range(KC):
            b = kc % 2
            if shift_ap is None:
                S.op('dve', lambda e, kc=kc: e.scalar_tensor_tensor(
                    out=xn_of(kc, off, n), in0=h_of(kc, off, n), scalar=scale_ap(kc, idx),
                    in1=rs[:, off:off + n], op0=ALU.mult, op1=ALU.mult),
                     reads=[hkey, (tag, 'rstd'), 'mods'], writes=[xnkey])
            else:
                S.op('dve', lambda e, kc=kc, b=b: e.scalar_tensor_tensor(
                    out=tmp['tm'][b][:, 0:n], in0=h_of(kc, off, n), scalar=scale_ap(kc, idx),
                    in1=rs[:, off:off + n], op0=ALU.mult, op1=ALU.mult),
                     reads=[hkey, (tag, 'rstd'), 'mods'], writes=[(tag, 'tm', b)])
                S.op('act', lambda e, kc=kc, b=b: e.activation(
                    out=xn_of(kc, off, n), in_=tmp['tm'][b][:, 0:n], func=AF.Identity,
                    bias=shift_ap(kc, idx), scale=1.0),
                     reads=[(tag, 'tm', b), 'mods'], writes=[xnkey])


def emit_conv(C, tag, src, skey, dst, dkey, wap, wkey):
    S = C.S
    for (a, e_) in SEGS:
        S.op('dve', lambda e, a=a, e_=e_: e.tensor_scalar(out=dst[:, a:e_], in0=src[:, a:e_], scalar1=wap(1),
                                                          scalar2=wap(4), op0=ALU.mult, op1=ALU.add),
             reads=[skey, wkey], writes=[dkey])
        S.op('dve', lambda e, a=a, e_=e_: e.scalar_tensor_tensor(
            out=dst[:, a + 1:e_], in0=src[:, a:e_ - 1], scalar=wap(0), in1=dst[:, a + 1:e_],
            op0=ALU.mult, op1=ALU.add), reads=[skey, wkey, dkey], writes=[dkey])
        S.op('dve', lambda e, a=a, e_=e_: e.scalar_tensor_tensor(
            out=dst[:, a:e_ - 1], in0=src[:, a + 1:e_], scalar=wap(2), in1=dst[:, a:e_ - 1],
            op0=ALU.mult, op1=ALU.add), reads=[skey, wkey, dkey], writes=[dkey])
        S.op('dve', lambda e, a=a, e_=e_: e.scalar_tensor_tensor(
            out=dst[:, a:e_ - 2], in0=src[:, a + 2:e_], scalar=wap(3), in1=dst[:, a:e_ - 2],
            op0=ALU.mult, op1=ALU.add), reads=[skey, wkey, dkey], writes=[dkey])


def emit_eps(C):
    C.epsb = C.sb('epsb', [128, 1])
    C.S.op('dve', lambda e: e.memset(C.epsb[:], 1e-6), writes=['epsb'])


PASS_T = 576
SUBS_T = [(0, 256), (256, 320)]
FFN_DIM = 5632
EXP_DIM = 4096
NEXP = 8


def emit_token(C, layer, li, moe, final, h_src, h_src_key, h_dst, h_dst_key, brT, modd, yout, hdbg=None):
    S = C.S
    C.pfx = 'L%d_' % layer
    C.push()
    C.nrot = 7
    hT = h_src
    n1w_d = C.din('n1w', [128, KC])
    n2w_d = C.din('n2w', [128, KC])
    wg_d = C.din('wg', [KC, 128, KC * 4 * 128])
    wb_d = C.din('wb', [KC, 128, 4 * 8 * 128])
    wo_d = C.din('wo', [KC, 128, KC * 128])
    if moe:
        NBLK = NEXP * (EXP_DIM // 256)
        rw_d = C.din('rw', [128, KC * NEXP])
    else:
        NBLK = FFN_DIM // 256
    wgu_d = C.din('wgu', [NBLK, 128, KC * 2 * 256])
    w2_d = C.din('w2', [NBLK, 128, 2 * D])
    if final:
        fnw_d = C.din('fnw', [128, KC])

    hs = C.sb('hs', [128, KC, PASS_T])
    xn = C.sb('xn', [128, KC, PASS_T], BF16)
    yv = C.sb('yv', [128, KC, PASS_T], BF16)
    br = [C.sb('br%d' % n, [128, 8, PASS_T], BF16) for n in range(4)]
    wA = [C.sb('wA%d' % i, [128, KC * 512], BF16) for i in range(2)]
    wB = [C.sb('wB%d' % i, [128, 4096], BF16) for i in range(2)]
    act = [C.sb('act%d' % i, [128, 2, PASS_T], BF16) for i in range(2)]
    modt = C.sb('modt', [128, 96, 2])
    n1w = C.sb('n1w_s', [128, KC])
    n2w = C.sb('n2w_s', [128, KC])
    sc1 = C.sb('sc1', [128, KC, 2])
    sc2 = C.sb('sc2', [128, KC, 2])

    def ROW(idx):
        return 1 if idx == 0 else 0

    def MOD(m, kc, idx):
        return modt[:, m * 16 + kc, ROW(idx):ROW(idx) + 1]

    def SC(sc, kc, idx):
        return sc[:, kc, ROW(idx):ROW(idx) + 1]
    tmp = {'sq': [C.sb('sq%d' % i, [128, 448]) for i in range(2)],
           'tm': [C.sb('tm%d' % i, [128, 448]) for i in range(2)],
           'rstd': C.sb('rstd', [128, PASS_T])}
    sg = [C.sb('sg%d' % i, [128, 448]) for i in range(2)]
    yacc = C.sb('yacc', [128, 448])
    if moe:
        rw = C.sb('rw_s', [128, KC, NEXP], BF16)
        gbc = C.sb('gbc', [128, NEXP, PASS_T])
        lg = C.sb('lg', [128, 8])
        m8 = C.sb('m8', [128, 8])
        rt = C.sb('rt', [128, 8])
        gt = C.sb('gt', [128, 8])
        gt2 = C.sb('gt2', [128, 8])
        dg = [C.sb('dg%d' % i, [128, 128]) for i in range(2)]
    if final:
        fnw = C.sb('fnw_s', [128, KC])

    S.dma('sp', modt[:].rearrange('p j r -> p (j r)'), modd[li], reads=[('modd', li)], writes=['mods'])
    S.dma('sp', n1w[:], n1w_d, writes=['n1w'])
    S.dma('sp', n2w[:], n2w_d, writes=['n2w'])
    if moe:
        S.dma('pool', rw[:].rearrange('p k e -> p (k e)'), rw_d, writes=['rw'])
    if final:
        S.dma('sp', fnw[:], fnw_d, writes=['fnw'])
    for (sc, nw, nwk, mi) in ((sc1, n1w, 'n1w', 1), (sc2, n2w, 'n2w', 4)):
        for ps_ in range(2):
            S.op('dve', lambda e, sc=sc, nw=nw, ps_=ps_, mi=mi: e.scalar_tensor_tensor(
                out=sc[:, :, ps_], in0=modt[:, mi * 16:(mi + 1) * 16, ps_], scalar=1.0, in1=nw[:],
                op0=ALU.add, op1=ALU.mult),
                 reads=['mods', nwk], writes=['mods'])

    hv = hT.rearrange('(k p) t -> p k t', p=128)
    hov = h_dst.rearrange('(k p) t -> p k t', p=128)
    if final:
        yov = yout.rearrange('(k p) t -> p k t', p=128)
    if hdbg is not None:
        hdv = hdbg.rearrange('(k p) t -> p k t', p=128)
    brv = brT.rearrange('n (k p) t -> n p k t', p=128)
    wctr = {'A': 0, 'B': 0, 'act': 0}

    def nextbuf(kind):
        i = wctr[kind]
        wctr[kind] = i + 1
        return i % 2

    for p in range(T // PASS_T):
        t0 = p * PASS_T
        subs = [(off, n, p * 2 + si) for si, (off, n) in enumerate(SUBS_T)]
        if final and p == 0:
            subs = subs[1:]
        S.dma('sp', hs[:], hv[:, :, t0:t0 + PASS_T], reads=[h_src_key], writes=['hs'])
        for n in range(4):
            S.dma('sp', br[n][:], brv[n][:, :, t0:t0 + PASS_T], reads=['brT'], writes=[('br', n)])
        emit_rmsnorm(C, 'n1', lambda kc, off, n: hs[:, kc, off:off + n], 'hs', lambda kc, off, n: xn[:, kc, off:off + n], 'xn', subs,
                     lambda kc, idx: SC(sc1, kc, idx), lambda kc, idx: MOD(0, kc, idx), tmp)
        for dc in range(KC):
            a = nextbuf('A')
            b = nextbuf('B')
            S.dma('pool', wA[a][:], wg_d[dc], writes=[('wA', a)])
            S.dma('pool', wB[b][:], wb_d[dc], writes=[('wB', b)])
            wgv = wA[a][:].rearrange('p (k n c) -> p k n c', k=KC, n=4)
            wbv = wB[b][:].rearrange('p (n k c) -> p n k c', n=4, k=8)
            for (off, n_, idx) in subs:
                for n in range(4):
                    pg, pgk = C.ps()
                    for kc in range(KC):
                        S.op('pe', lambda e, kc=kc, n=n, pg=pg: e.matmul(
                            pg[:, 0:n_], wgv[:, kc, n, :], xn[:, kc, off:off + n_],
                            start=(kc == 0), stop=(kc == KC - 1)),
                             reads=[('wA', a), 'xn'], writes=[pgk])
                    sb_ = n % 2
                    S.op('act', lambda e, pg=pg, sb_=sb_: e.activation(out=sg[sb_][:, 0:n_], in_=pg[:, 0:n_],
                                                                       func=AF.Sigmoid),
                         reads=[pgk], writes=[('sg', sb_)])
                    pp, ppk = C.ps()
                    for kc in range(8):
                        S.op('pe', lambda e, kc=kc, n=n, pp=pp: e.matmul(
                            pp[:, 0:n_], wbv[:, n, kc, :], br[n][:, kc, off:off + n_],
                            start=(kc == 0), stop=(kc == 7)),
                             reads=[('wB', b), ('br', n)], writes=[ppk])
                    if n == 0:
                        S.op('dve', lambda e, pp=pp, sb_=sb_: e.tensor_tensor(
                            out=yacc[:, 0:n_], in0=sg[sb_][:, 0:n_], in1=pp[:, 0:n_], op=ALU.mult),
                             reads=[('sg', sb_), ppk], writes=['yacc'])
                    else:
                        S.op('dve', lambda e, pp=pp, sb_=sb_: e.tensor_tensor(
                            out=sg[sb_][:, 0:n_], in0=sg[sb_][:, 0:n_], in1=pp[:, 0:n_], op=ALU.mult),
                             reads=[('sg', sb_), ppk], writes=[('sg', sb_)])
                        if n < 3:
                            S.op('dve', lambda e, sb_=sb_: e.tensor_tensor(
                                out=yacc[:, 0:n_], in0=yacc[:, 0:n_], in1=sg[sb_][:, 0:n_], op=ALU.add),
                                 reads=[('sg', sb_), 'yacc'], writes=['yacc'])
                        else:
                            S.op('dve', lambda e, sb_=sb_: e.tensor_tensor(
                                out=yv[:, dc, off:off + n_], in0=yacc[:, 0:n_], in1=sg[sb_][:, 0:n_], op=ALU.add),
                                 reads=[('sg', sb_), 'yacc'], writes=['yv'])
        for dc in range(KC):
            b = nextbuf('B')
            S.dma('pool', wB[b][:, 0:KC * 128], wo_d[dc], writes=[('wB', b)])
            wov = wB[b][:, 0:KC * 128].rearrange('p (k c) -> p k c', k=KC)
            for (off, n_, idx) in subs:
                po, pok = C.ps()
                for kc in range(KC):
                    S.op('pe', lambda e, kc=kc, po=po: e.matmul(
                        po[:, 0:n_], wov[:, kc, :], yv[:, kc, off:off + n_],
                        start=(kc == 0), stop=(kc == KC - 1)),
                         reads=[('wB', b), 'yv'], writes=[pok])
                S.op('dve', lambda e, po=po: e.scalar_tensor_tensor(
                    out=hs[:, dc, off:off + n_], in0=po[:, 0:n_], scalar=MOD(2, dc, idx),
                    in1=hs[:, dc, off:off + n_], op0=ALU.mult, op1=ALU.add),
                     reads=[pok, 'hs', 'mods'], writes=['hs'])
        emit_rmsnorm(C, 'n2', lambda kc, off, n: hs[:, kc, off:off + n], 'hs', lambda kc, off, n: xn[:, kc, off:off + n], 'xn', subs,
                     lambda kc, idx: SC(sc2, kc, idx), lambda kc, idx: MOD(3, kc, idx), tmp)
        if moe:
            chunks = [(0, 128), (128, 128), (256, 128), (384, 128), (512, 64)]
            if final and p == 0:
                chunks = chunks[2:]
            for (co, cn) in chunks:
                pl_, plk = C.ps()
                for kc in range(KC):
                    S.op('pe', lambda e, kc=kc, pl_=pl_: e.matmul(
                        pl_[0:cn, 0:NEXP], xn[:, kc, co:co + cn], rw[:, kc, :],
                        start=(kc == 0), stop=(kc == KC - 1)),
                         reads=['xn', 'rw'], writes=[plk])
                S.op('act', lambda e, pl_=pl_: e.activation(out=lg[0:cn, :], in_=pl_[0:cn, 0:NEXP], func=AF.Identity),
                     reads=[plk], writes=['lg'])
                S.op('dve', lambda e: e.max(out=m8[0:cn, :], in_=lg[0:cn, :]), reads=['lg'], writes=['m8'])
                S.op('dve', lambda e: e.tensor_tensor(out=rt[0:cn, 0:1], in0=m8[0:cn, 1:2], in1=m8[0:cn, 0:1],
                                                      op=ALU.subtract), reads=['m8'], writes=['rt'])
                S.op('act', lambda e: e.activation(out=rt[0:cn, 1:2], in_=rt[0:cn, 0:1], func=AF.Exp),
                     reads=['rt'], writes=['rt'])
                S.op('dve', lambda e: e.tensor_scalar(out=rt[0:cn, 2:3], in0=rt[0:cn, 1:2], scalar1=1.0, scalar2=None,
                                                      op0=ALU.add), reads=['rt'], writes=['rt'])
                S.op('dve', lambda e: e.reciprocal(out=rt[0:cn, 2:3], in_=rt[0:cn, 2:3]), reads=['rt'], writes=['rt'])
                S.op('dve', lambda e: e.tensor_tensor(out=rt[0:cn, 3:4], in0=rt[0:cn, 1:2], in1=rt[0:cn, 2:3],
                                                      op=ALU.mult), reads=['rt'], writes=['rt'])
                S.op('dve', lambda e: e.tensor_scalar(out=gt[0:cn, :], in0=lg[0:cn, :], scalar1=m8[0:cn, 0:1],
                                                      scalar2=rt[0:cn, 2:3], op0=ALU.is_equal, op1=ALU.mult),
                     reads=['lg', 'm8', 'rt'], writes=['gt'])
                S.op('dve', lambda e: e.tensor_scalar(out=gt2[0:cn, :], in0=lg[0:cn, :], scalar1=m8[0:cn, 1:2],
                                                      scalar2=rt[0:cn, 3:4], op0=ALU.is_equal, op1=ALU.mult),
                     reads=['lg', 'm8', 'rt'], writes=['gt2'])
                S.op('dve', lambda e: e.tensor_tensor(out=gt[0:cn, :], in0=gt[0:cn, :], in1=gt2[0:cn, :], op=ALU.add),
                     reads=['gt', 'gt2'], writes=['gt'])
                for ex in range(NEXP):
                    db = ex % 2
                    S.op('dve', lambda e, ex=ex, db=db: e.tensor_scalar(
                        out=dg[db][0:cn, 0:cn], in0=C.ident[0:cn, 0:cn], scalar1=gt[0:cn, ex:ex + 1], scalar2=None,
                        op0=ALU.mult), reads=['ident', 'gt'], writes=[('dg', db)])
                    pb, pbk = C.ps()
                    S.op('pe', lambda e, db=db, pb=pb: e.matmul(pb[:, 0:cn], C.ones[0:cn, :], dg[db][0:cn, 0:cn],
                                                                start=True, stop=True),
                         reads=[('dg', db), 'ones'], writes=[pbk])
                    S.op('act', lambda e, ex=ex, pb=pb: e.activation(out=gbc[:, ex, co:co + cn], in_=pb[:, 0:cn],
                                                                     func=AF.Identity),
                         reads=[pbk], writes=['gbc'])
        for blk in range(NBLK):
            a = nextbuf('A')
            b = nextbuf('B')
            ab = nextbuf('act')
            ex = blk // (EXP_DIM // 256) if moe else 0
            S.dma('pool', wA[a][:], wgu_d[blk], writes=[('wA', a)])
            S.dma('pool', wB[b][:], w2_d[blk], writes=[('wB', b)])
            wguv = wA[a][:].rearrange('p (k g c) -> p k g c', k=KC, g=2)
            w2v = wB[b][:].rearrange('p (f d) -> p f d', f=2)
            for fc in range(2):
                for (off, n_, idx) in subs:
                    pg, pgk = C.ps()
                    pu, puk = C.ps()
                    for g_, (pp_, ppk_) in enumerate(((pg, pgk), (pu, puk))):
                        for kc in range(KC):
                            S.op('pe', lambda e, kc=kc, g_=g_, pp_=pp_: e.matmul(
                                pp_[:, 0:n_], wguv[:, kc, g_, fc * 128:(fc + 1) * 128], xn[:, kc, off:off + n_],
                                start=(kc == 0), stop=(kc == KC - 1)),
                                 reads=[('wA', a), 'xn'], writes=[ppk_])
                    sb_ = fc % 2
                    S.op('act', lambda e, pg=pg, sb_=sb_: e.activation(out=sg[sb_][:, 0:n_], in_=pg[:, 0:n_],
                                                                       func=AF.Silu),
                         reads=[pgk], writes=[('sg', sb_)])
                    if moe:
                        S.op('dve', lambda e, pu=pu, sb_=sb_: e.tensor_tensor(
                            out=sg[sb_][:, 0:n_], in0=sg[sb_][:, 0:n_], in1=pu[:, 0:n_], op=ALU.mult),
                             reads=[('sg', sb_), puk], writes=[('sg', sb_)])
                        S.op('dve', lambda e, sb_=sb_: e.tensor_tensor(
                            out=act[ab][:, fc, off:off + n_], in0=sg[sb_][:, 0:n_], in1=gbc[:, ex, off:off + n_],
                            op=ALU.mult),
                             reads=[('sg', sb_), 'gbc'], writes=[('act', ab)])
                    else:
                        S.op('dve', lambda e, pu=pu, sb_=sb_: e.tensor_tensor(
                            out=act[ab][:, fc, off:off + n_], in0=sg[sb_][:, 0:n_], in1=pu[:, 0:n_], op=ALU.mult),
                             reads=[('sg', sb_), puk], writes=[('act', ab)])
            for dc in range(KC):
                for (off, n_, idx) in subs:
                    po, pok = C.ps()
                    for fc in range(2):
                        S.op('pe', lambda e, fc=fc, po=po: e.matmul(
                            po[:, 0:n_], w2v[:, fc, dc * 128:(dc + 1) * 128], act[ab][:, fc, off:off + n_],
                            start=(fc == 0), stop=(fc == 1)),
                             reads=[('wB', b), ('act', ab)], writes=[pok])
                    S.op('dve', lambda e, po=po: e.scalar_tensor_tensor(
                        out=hs[:, dc, off:off + n_], in0=po[:, 0:n_], scalar=MOD(5, dc, idx),
                        in1=hs[:, dc, off:off + n_], op0=ALU.mult, op1=ALU.add),
                         reads=[pok, 'hs', 'mods'], writes=['hs'])
        S.dma('sp', hov[:, :, t0:t0 + PASS_T], hs[:], reads=['hs'], writes=[h_dst_key])
        if hdbg is not None:
            S.dma('sp', hdv[:, :, t0:t0 + PASS_T], hs[:], reads=['hs'], writes=['hdbg'])
        if final:
            emit_rmsnorm(C, 'nf', lambda kc, off, n: hs[:, kc, off:off + n], 'hs', lambda kc, off, n: hs[:, kc, off:off + n], 'hs', subs, lambda kc, idx: fnw[:, kc:kc + 1], None, tmp)
            S.dma('sp', yov[:, :, t0:t0 + PASS_T], hs[:], reads=['hs'], writes=['yout'])
    barrier(C)
    C.pop()


GATES_OFF = 22560 - 4 * D


def lay_vec(v):
    return np.ascontiguousarray(v.reshape(-1, 128).T)


def lay_token_weights(w_in_l, w_branch_l, w_out_l):
    wg = w_in_l[:, GATES_OFF:].reshape(KC, 128, 4, KC, 128).transpose(3, 1, 0, 2, 4).reshape(KC, 128, KC * 4 * 128)
    wb = w_branch_l.reshape(4, 8, 128, KC, 128).transpose(3, 2, 0, 1, 4).reshape(KC, 128, 4 * 8 * 128)
    wo = w_out_l.reshape(KC, 128, KC, 128).transpose(2, 1, 0, 3).reshape(KC, 128, KC * 128)
    return np.ascontiguousarray(wg), np.ascontiguousarray(wb), np.ascontiguousarray(wo)


def lay_ffn(wgu, w2, F):
    nb = F // 256
    a = wgu.reshape(KC, 128, 2, nb, 256).transpose(3, 1, 0, 2, 4).reshape(nb, 128, KC * 2 * 256)
    b = w2.reshape(nb, 2, 128, D).transpose(0, 2, 1, 3).reshape(nb, 128, 2 * D)
    return np.ascontiguousarray(a), np.ascontiguousarray(b)


IDENT = np.eye(128, dtype=np.float32)


def token_in_map(layer, inp, moe, final):
    wg, wb, wo = lay_token_weights(inp['w_in'][layer], inp['w_branch'][layer], inp['w_out'][layer])
    m = {'n1w': lay_vec(inp['norm1_w'][layer]), 'n2w': lay_vec(inp['norm2_w'][layer]), 'wg': wg, 'wb': wb, 'wo': wo}
    if moe:
        parts = [lay_ffn(inp['moe_wgu'][layer // 2][e], inp['moe_w2'][layer // 2][e], EXP_DIM) for e in range(NEXP)]
        m['wgu'] = np.concatenate([p[0] for p in parts], axis=0)
        m['w2'] = np.concatenate([p[1] for p in parts], axis=0)
        m['rw'] = np.ascontiguousarray(
            inp['router_w'][layer // 2].reshape(KC, 128, NEXP).transpose(1, 0, 2).reshape(128, KC * NEXP))
    else:
        m['wgu'], m['w2'] = lay_ffn(inp['ffn_wgu'][layer // 2], inp['ffn_w2'][layer // 2], FFN_DIM)
    if final:
        m['fnw'] = lay_vec(inp['final_norm_w'])
    return m


NCHK = T // 128
FWD_ORDER = list(range(NCHK))
BWD_ORDER = [1, 0] + list(range(NCHK - 1, 1, -1))
TILES_M = [(0, 256)] + [(256 + i * 512, 512) for i in range(4)]
SEGS = [(0, NCTX), (NCTX, T)]
OFF = {'ret_q': 0, 'ret_k': 1024, 'ret_v': 2048, 'ret_g': 3072, 'ssd_z': 4096, 'ssd_x': 5120, 'ssd_B': 6144,
       'ssd_C': 6656, 'ssd_dt': 7168, 'lru_x': 7200, 'lru_y': 8224, 'hg_q': 9248, 'hg_f': 10272,
       'hg_i': 12320, 'hg_g': 13344}


def mixer_chunks(s):
    L = []
    for j in range(4):
        hh = 4 * s + j
        for nm in ('ret_q', 'ret_k', 'ret_v', 'ret_g'):
            L.append(((nm, j), OFF[nm] + hh * 128))
    for gi in range(2):
        gg = 2 * s + gi
        L.append((('ssd_B', gi), OFF['ssd_B'] + gg * 128))
        L.append((('ssd_C', gi), OFF['ssd_C'] + gg * 128))
    for pi in range(4):
        pp = 4 * s + pi
        L.append((('ssd_x', pi), OFF['ssd_x'] + pp * 128))
        L.append((('ssd_z', pi), OFF['ssd_z'] + pp * 128))
    for j in range(4):
        gb = 4 * s + j
        L.append((('lru_x', j), OFF['lru_x'] + gb * 128))
        L.append((('lru_y', j), OFF['lru_y'] + gb * 128))
    for j in range(4):
        hh = 4 * s + j
        L.append((('hg_q', j), OFF['hg_q'] + hh * 128))
        L.append((('hg_f0', j), OFF['hg_f'] + hh * 128))
        L.append((('hg_f1', j), OFF['hg_f'] + 1024 + hh * 128))
        L.append((('hg_i', j), OFF['hg_i'] + hh * 128))
        L.append((('hg_g', j), OFF['hg_g'] + hh * 128))
    return L


CHIDX = {nm: i for i, (nm, _) in enumerate(mixer_chunks(0))}
NCH_M = len(CHIDX)


def emit_mixer(C, layer, li, s, hT, hkey, brT, modd, xst, which=('ret', 'ssd', 'lru', 'hg')):
    S = C.S
    C.nrot = 6
    C.psi = 0
    C.pfx = 'L%ds%d_' % (layer, s)
    C.push()
    n1w_d = C.din('n1w', [128, KC])
    wM_d = C.din('wM', [NCH_M, 128, KC * 128])
    masks_d = C.din('masks', [128, 4 * 128], shared=True)
    identb_d = C.din('identb', [128, 128], BF16, shared=True)
    obT = C.sb('obT', [128, T], BF16)

    def out_tm(n, ch0, src_of_c, skey):
        for c in range(NCHK):
            transpose_to(obT[:, c * 128:(c + 1) * 128], 'obT', src_of_c(c), skey)
        S.dma('sp', brT[n, ch0:ch0 + 128, :], obT[:], reads=['obT'], writes=['brT'])

    xnT = xst['xnT']
    xst['fresh'] = not xst.get('done', False)
    xst['done'] = True
    modt = C.sb('modt', [128, 96, 2])
    n1w = C.sb('n1w_s', [128, KC])
    sc1 = C.sb('sc1', [128, KC, 2])
    masks = C.sb('masks_s', [128, 4, 128])
    identb = C.sb('identb_s', [128, 128], BF16)
    oneb = C.sb('oneb', [128, 1])
    NW = 4
    wbuf = [C.sb('wbuf%d' % i, [128, KC, 128], BF16) for i in range(NW)]
    wstate = {'i': 0}
    M_le, M_ge, M_gt, M_lt = (masks[:, i, :] for i in range(4))

    S.dma('sp', modt[:].rearrange('p j r -> p (j r)'), modd[li], reads=[('modd', li)], writes=['mods'])
    S.dma('sp', n1w[:], n1w_d, writes=['n1w'])
    S.dma('sp', masks[:].rearrange('p a c -> p (a c)'), masks_d, writes=['masks'])
    S.dma('sp', identb[:], identb_d, writes=['identb'])
    S.op('dve', lambda e: e.memset(oneb[:], 1.0), writes=['oneb'])
    for a_ in range(2):
        S.op('dve', lambda e, a_=a_: e.scalar_tensor_tensor(
            out=sc1[:, :, a_], in0=modt[:, 16:32, a_], scalar=1.0, in1=n1w[:], op0=ALU.add, op1=ALU.mult),
             reads=['mods', 'n1w'], writes=['mods'])

    def getw(name):
        i = wstate['i'] % NW
        wstate['i'] += 1
        S.dma('pool', wbuf[i][:].rearrange('p k c -> p (k c)'), wM_d[CHIDX[name]], writes=[('w', i)])
        return wbuf[i], ('w', i)

    def proj_fm(w, wk, t0, n):
        pt, pk = C.ps()
        for kc in range(KC):
            S.op('pe', lambda e, kc=kc: e.matmul(pt[:, 0:n], w[:, kc, :], xnT[:, kc, t0:t0 + n],
                                                 start=(kc == 0), stop=(kc == KC - 1)),
                 reads=[wk, 'xnT'], writes=[pk])
        return pt, pk

    def proj_tm(w_ap_of_kc, wk, c, ncol):
        pt, pk = C.ps()
        for kc in range(KC):
            S.op('pe', lambda e, kc=kc: e.matmul(pt[:, 0:ncol], xnT[:, kc, c * 128:(c + 1) * 128], w_ap_of_kc(kc),
                                                 start=(kc == 0), stop=(kc == KC - 1)),
                 reads=[wk, 'xnT'], writes=[pk])
        return pt, pk

    def transpose_to(dst_ap, dkey, src_ap, skey):
        S.op('pe', lambda e: e.transpose(out=C.pbf[:, 0:128], in_=src_ap, identity=identb[:]),
             reads=[skey, 'identb'], writes=['pbf'])
        S.op('act', lambda e: e.activation(out=dst_ap, in_=C.pbf[:, 0:128], func=AF.Identity),
             reads=['pbf'], writes=[dkey])

    with ExitStack() as es0:
        def sb0(name, shape, dt=F32):
            return es0.enter_context(C.nc.sbuf_tensor(C.pfx + name, list(shape), dt))
        ht = [sb0('ht%d' % i, [128, KC, 256]) for i in range(2)]
        tmp = {'sq': [sb0('sq%d' % i, [128, 256]) for i in range(2)],
               'tm': [sb0('tm%d' % i, [128, 256]) for i in range(2)],
               'rstd': sb0('rstd', [128, 256])}
        hv = hT.rearrange('(k p) t -> p k t', p=128)
        for ti in (range(T // 256) if xst['fresh'] else []):
            b = ti % 2
            S.dma('sp', ht[b][:], hv[:, :, ti * 256:(ti + 1) * 256], reads=[hkey], writes=[('ht', b)])
            a_ = 1 if ti == 0 else 0
            emit_rmsnorm(C, 'n1', lambda kc, off, n, b=b: ht[b][:, kc, off:off + n], ('ht', b),
                         lambda kc, off, n, ti=ti: xnT[:, kc, ti * 256 + off:ti * 256 + off + n], 'xnT',
                         [(0, 256, a_)],
                         lambda kc, idx: sc1[:, kc, idx:idx + 1], lambda kc, idx: modt[:, kc, idx:idx + 1], tmp)
        barrier(C)

    def scalar_scan(sbx, tag, qT, kT, k_tm, v_tm, heads, la_c_of, o_acc, okey):
        nh = len(heads)
        Sst = [sbx('%s_S%d' % (tag, h), [128, 128]) for h in range(nh)]
        Sbf = [sbx('%s_Sb%d' % (tag, h), [128, 128], BF16) for h in range(nh)]
        ec = sbx(tag + '_ec', [128, 4 * nh])
        Am = [sbx('%s_A%d' % (tag, i), [128, 128]) for i in range(2)]
        Lt = [sbx('%s_L%d' % (tag, i), [128, 128]) for i in range(2)]
        Pm = [sbx('%s_P%d' % (tag, i), [128, 128], BF16) for i in range(2)]
        vw = [sbx('%s_vw%d' % (tag, i), [128, 128], BF16) for i in range(2)]
        tmo = [sbx('%s_to%d' % (tag, i), [128, 128]) for i in range(2)]
        cnt = {'i': 0}
        for d, order in ((0, FWD_ORDER), (1, BWD_ORDER)):
            mA = M_le if d == 0 else M_ge
            mB = M_gt if d == 0 else M_lt
            last = 127 if d == 0 else 0
            for h in range(nh):
                S.op('dve', lambda e, h=h: e.memset(Sst[h][:], 0.0), writes=[(tag, 'S', h)])
                S.op('act', lambda e, h=h: e.activation(out=Sbf[h][:], in_=Sst[h][:], func=AF.Identity),
                     reads=[(tag, 'S', h)], writes=[(tag, 'Sb', h)])
            for c in order:
                la_ap, la_key = la_c_of(c, d)
                pe_, pek = C.ps()
                S.op('pe', lambda e: e.matmul(pe_[:, 0:nh], mA, la_ap, start=True, stop=True),
                     reads=['masks', la_key], writes=[pek])
                S.op('pe', lambda e: e.matmul(pe_[:, nh:2 * nh], C.ones[:], la_ap,
                                              start=True, stop=True),
                     reads=['ones', la_key], writes=[pek])
                S.op('act', lambda e: e.activation(out=ec[:, 0:2 * nh], in_=pe_[:, 0:2 * nh], func=AF.Exp),
                     reads=[pek], writes=[(tag, 'ec')])
                psc, psck = C.psb[6], ('ps', 6)
                S.op('pe', lambda e, c=c: e.matmul(psc[:, 0:128], kT[:, c * 128:(c + 1) * 128],
                                                   qT[:, c * 128:(c + 1) * 128], start=True, stop=True),
                     reads=[(tag, 'kT'), (tag, 'qT')], writes=[psck])
                for h, hd in enumerate(heads):
                    i2 = cnt['i'] % 2
                    cnt['i'] += 1
                    v0, dv = hd['v0'], hd['dv']
                    la1 = hd['la'][d](c)
                    dt1 = hd['dt'][d](c)
                    S.op('dve', lambda e, i2=i2, la1=la1: e.tensor_scalar(out=Am[i2][:], in0=mA, scalar1=la1,
                                                                          scalar2=None, op0=ALU.mult),
                         reads=['masks', la_key], writes=[(tag, 'A', i2)])
                    pl_, plk = C.ps()
                    S.op('pe', lambda e, i2=i2: e.matmul(pl_[:, 0:128], mB, Am[i2][:], start=True, stop=True),
                         reads=['masks', (tag, 'A', i2)], writes=[plk])
                    S.op('act', lambda e, i2=i2: e.activation(out=Lt[i2][:], in_=pl_[:, 0:128], func=AF.Exp),
                         reads=[plk], writes=[(tag, 'L', i2)])
                    S.op('dve', lambda e, i2=i2, dt1=dt1: e.scalar_tensor_tensor(
                        out=Lt[i2][:], in0=Lt[i2][:], scalar=dt1, in1=mA, op0=ALU.mult, op1=ALU.mult),
                         reads=[(tag, 'L', i2), 'masks', la_key], writes=[(tag, 'L', i2)])
                    S.op('dve', lambda e, i2=i2: e.tensor_tensor(out=Pm[i2][:], in0=psc[:, 0:128], in1=Lt[i2][:],
                                                                 op=ALU.mult),
                         reads=[psck, (tag, 'L', i2)], writes=[(tag, 'P', i2)])
                    pin, pink = C.ps()
                    S.op('pe', lambda e, i2=i2, c=c: e.matmul(pin[:, 0:dv], Pm[i2][:], v_tm[:, c, v0:v0 + dv],
                                                              start=True, stop=True),
                         reads=[(tag, 'P', i2), (tag, 'v')], writes=[pink])
                    pit, pitk = C.ps()
                    S.op('pe', lambda e, h=h, c=c: e.matmul(pit[:, 0:dv], qT[:, c * 128:(c + 1) * 128],
                                                            Sbf[h][:, 0:dv], start=True, stop=True),
                         reads=[(tag, 'qT'), (tag, 'Sb', h)], writes=[pitk])
                    S.op('dve', lambda e, i2=i2, c=c: e.tensor_tensor(
                        out=tmo[i2][:, 0:dv], in0=pin[:, 0:dv], in1=o_acc[:, c, v0:v0 + dv], op=ALU.add),
                         reads=[pink, okey], writes=[(tag, 'to', i2)])
                    S.op('dve', lambda e, i2=i2, c=c, h=h: e.scalar_tensor_tensor(
                        out=o_acc[:, c, v0:v0 + dv], in0=pit[:, 0:dv], scalar=ec[:, h:h + 1], in1=tmo[i2][:, 0:dv],
                        op0=ALU.mult, op1=ALU.add),
                         reads=[pitk, (tag, 'ec'), (tag, 'to', i2)], writes=[okey])
                    S.op('dve', lambda e, i2=i2, c=c: e.tensor_scalar(
                        out=vw[i2][:, 0:dv], in0=v_tm[:, c, v0:v0 + dv], scalar1=Lt[i2][:, last:last + 1],
                        scalar2=None, op0=ALU.mult),
                         reads=[(tag, 'v'), (tag, 'L', i2)], writes=[(tag, 'vw', i2)])
                    pct, pctk = C.ps()
                    S.op('pe', lambda e, i2=i2, c=c: e.matmul(pct[:, 0:dv], k_tm[:, c, :], vw[i2][:, 0:dv],
                                                              start=True, stop=True),
                         reads=[(tag, 'ktm'), (tag, 'vw', i2)], writes=[pctk])
                    S.op('dve', lambda e, h=h: e.scalar_tensor_tensor(
                        out=Sst[h][:, 0:dv], in0=Sst[h][:, 0:dv], scalar=ec[:, nh + h:nh + h + 1], in1=pct[:, 0:dv],
                        op0=ALU.mult, op1=ALU.add),
                         reads=[(tag, 'S', h), (tag, 'ec'), pctk], writes=[(tag, 'S', h)])
                    S.op('act', lambda e, h=h: e.activation(out=Sbf[h][:, 0:dv], in_=Sst[h][:, 0:dv],
                                                            func=AF.Identity),
                         reads=[(tag, 'S', h)], writes=[(tag, 'Sb', h)])

    def rms_rows(sbx, tag, src, skey, W, nchunk=NCHK):
        ss = sbx(tag + '_ss', [128, nchunk])
        junk = sbx(tag + '_junk', [128, W])
        for c in range(nchunk):
            S.op('act', lambda e, c=c: e.activation(out=junk[:], in_=src[:, c, :], func=AF.Square,
                                                    accum_out=ss[:, c:c + 1]),
                 reads=[skey], writes=[(tag, 'ss'), (tag, 'junk')])
        S.op('act', lambda e: e.activation(out=ss[:], in_=ss[:], func=AF.Sqrt, bias=C.epsb[:, 0:1], scale=1.0 / W),
             reads=[(tag, 'ss'), 'epsb'], writes=[(tag, 'ss')])
        S.op('dve', lambda e: e.reciprocal(out=ss[:], in_=ss[:]), reads=[(tag, 'ss')], writes=[(tag, 'ss')])
        return ss, (tag, 'ss')

    if 'ret' in which:
        rope_d = C.din('rope', [128, 2 * T], shared=True)
        pmat_d = C.din('pmat', [128, 128], shared=True)
        retla_d = C.din('retla', [128, 4])
        with ExitStack() as es1:
            def sbx(name, shape, dt=F32):
                return es1.enter_context(C.nc.sbuf_tensor(C.pfx + name, list(shape), dt))
            rope = sbx('rope_s', [128, 2, T])
            pmat = sbx('pmat_s', [128, 128])
            retla = sbx('retla_s', [128, 4])
            ones2 = sbx('ones2', [128, 2])
            S.dma('sp', rope[:].rearrange('p a t -> p (a t)'), rope_d, writes=['rope'])
            S.dma('sp', pmat[:], pmat_d, writes=['pmat'])
            S.dma('sp', retla[:], retla_d, writes=['retla'])
            S.op('dve', lambda e: e.memset(ones2[:], 1.0), writes=['ones2'])
            qT = sbx('r_qT', [128, T], BF16)
            kT = sbx('r_kT', [128, T], BF16)
            k_tm = sbx('r_ktm', [128, NCHK, 128], BF16)
            v_tm = sbx('r_vtm', [128, NCHK, 128], BF16)
            g_tm = sbx('r_gtm', [128, NCHK, 128], BF16)
            o_acc = sbx('r_oacc', [128, NCHK, 128])
            q32 = [sbx('r_q32%d' % i, [128, 512]) for i in range(2)]
            qr = [sbx('r_qr%d' % i, [128, 512]) for i in range(2)]
            lac = sbx('r_lac', [128, 2])
            ob = sbx('r_ob', [128, NCHK, 128], BF16)
            tag = 'ret'
            for j in range(4):
                for (nm, dst, dkey, scl) in (('ret_q', qT, (tag, 'qT'), 1.0), ('ret_k', kT, (tag, 'kT'), 128.0 ** -0.5)):
                    w, wk = getw((nm, j))
                    for ti, (t0, n) in enumerate(TILES_M):
                        b = ti % 2
                        pt, pk = proj_fm(w, wk, t0, n)
                        S.op('act', lambda e, b=b, pt=pt, n=n, scl=scl: e.activation(
                            out=q32[b][:, 0:n], in_=pt[:, 0:n], func=AF.Identity, scale=scl),
                             reads=[pk], writes=[(tag, 'q32', b)])
                        ps2, ps2k = C.ps()
                        S.op('pe', lambda e, b=b, ps2=ps2, n=n: e.matmul(ps2[:, 0:n], pmat[:], q32[b][:, 0:n],
                                                                         start=True, stop=True),
                             reads=['pmat', (tag, 'q32', b)], writes=[ps2k])
                        S.op('dve', lambda e, b=b, ps2=ps2, n=n, t0=t0: e.tensor_tensor(
                            out=qr[b][:, 0:n], in0=ps2[:, 0:n], in1=rope[:, 1, t0:t0 + n], op=ALU.mult),
                             reads=[ps2k, 'rope'], writes=[(tag, 'qr', b)])
                        S.op('dve', lambda e, b=b, n=n, t0=t0: e.tensor_tensor(
                            out=q32[b][:, 0:n], in0=q32[b][:, 0:n], in1=rope[:, 0, t0:t0 + n], op=ALU.mult),
                             reads=[(tag, 'q32', b), 'rope'], writes=[(tag, 'q32', b)])
                        S.op('dve', lambda e, b=b, n=n, t0=t0, dst=dst: e.tensor_tensor(
                            out=dst[:, t0:t0 + n], in0=q32[b][:, 0:n], in1=qr[b][:, 0:n], op=ALU.add),
                             reads=[(tag, 'q32', b), (tag, 'qr', b)], writes=[dkey])
                for c in range(NCHK):
                    transpose_to(k_tm[:, c, :], (tag, 'ktm'), kT[:, c * 128:(c + 1) * 128], (tag, 'kT'))
                w, wk = getw(('ret_v', j))
                for c in range(NCHK):
                    pt, pk = proj_tm(lambda kc, w=w: w[:, kc, :], wk, c, 128)
                    S.op('act', lambda e, c=c, pt=pt: e.activation(out=v_tm[:, c, :], in_=pt[:, 0:128],
                                                                   func=AF.Identity),
                         reads=[pk], writes=[(tag, 'v')])
                w, wk = getw(('ret_g', j))
                for c in range(NCHK):
                    pt, pk = proj_tm(lambda kc, w=w: w[:, kc, :], wk, c, 128)
                    S.op('act', lambda e, c=c, pt=pt: e.activation(out=g_tm[:, c, :], in_=pt[:, 0:128], func=AF.Silu),
                         reads=[pk], writes=[(tag, 'g')])
                S.op('dve', lambda e: e.memset(o_acc[:], 0.0), writes=[(tag, 'o')])
                S.op('dve', lambda e, j=j: e.tensor_copy(out=lac[:, 0:1], in_=retla[:, j:j + 1]),
                     reads=['retla'], writes=[(tag, 'lac')])
                S.op('dve', lambda e, j=j: e.tensor_copy(out=lac[:, 1:2], in_=retla[:, j:j + 1]),
                     reads=['retla'], writes=[(tag, 'lac')])
                heads = [dict(v0=0, dv=128,
                              la=[lambda c: lac[:, 0:1], lambda c: lac[:, 1:2]],
                              dt=[lambda c: ones2[:, 0:1], lambda c: ones2[:, 1:2]])]
                with ExitStack() as es2:
                    def sby(name, shape, dt=F32):
                        return es2.enter_context(C.nc.sbuf_tensor(C.pfx + name + '_%d' % j, list(shape), dt))
                    scalar_scan(sby, tag, qT, kT, k_tm, v_tm, heads, lambda c, d: (lac[:, d:d + 1], (tag, 'lac')), o_acc, (tag, 'o'))
                    rs, rsk = rms_rows(sby, tag, o_acc, (tag, 'o'), 128)
                    for c in range(NCHK):
                        S.op('dve', lambda e, c=c: e.scalar_tensor_tensor(
                            out=ob[:, c, :], in0=o_acc[:, c, :], scalar=rs[:, c:c + 1], in1=g_tm[:, c, :],
                            op0=ALU.mult, op1=ALU.mult),
                             reads=[(tag, 'o'), rsk, (tag, 'g')], writes=[(tag, 'ob')])
                    out_tm(0, s * 512 + j * 128, lambda c: ob[:, c, :], (tag, 'ob'))
                    barrier(C)
        barrier(C)

    if 'lru' in which:
        lrup_d = C.din('lru_p', [128, 4 * 5])
        lrugb_d = C.din('lru_gb', [128, 16])
        lrulam_d = C.din('lru_lam', [128, 8])
        lruw_d = C.din('lru_w', [128, 16 * 128])
        with ExitStack() as es1:
            def sbx(name, shape, dt=F32):
                return es1.enter_context(C.nc.sbuf_tensor(C.pfx + name, list(shape), dt))
            tag = 'lru'
            lrup = sbx('lrup', [128, 4, 5])
            gb = sbx('lrugb', [128, 2, 2, 4])
            lam = sbx('lrulam', [128, 2, 4])
            ls8 = sbx('lruls8', [128, 2, 4])
            lruw = sbx('lruw', [128, 16, 128], BF16)
            S.dma('sp', lrup[:].rearrange('p j t -> p (j t)'), lrup_d, writes=['lrup'])
            S.dma('sp', gb[:].rearrange('p g d j -> p (g d j)'), lrugb_d, writes=['lrugb'])
            S.dma('sp', lam[:].rearrange('p d j -> p (d j)'), lrulam_d, writes=['lrulam'])
            S.dma('pool', lruw[:].rearrange('p a c -> p (a c)'), lruw_d, writes=['lruw'])
            S.op('act', lambda e: e.activation(out=ls8[:].rearrange('p d j -> p (d j)'),
                                               in_=lam[:].rearrange('p d j -> p (d j)'), func=AF.Sigmoid),
                 reads=['lrulam'], writes=['ls8'])
            S.op('act', lambda e: e.activation(out=ls8[:].rearrange('p d j -> p (d j)'),
                                               in_=ls8[:].rearrange('p d j -> p (d j)'), func=AF.Ln),
                 reads=['ls8'], writes=['ls8'])
            S.op('dve', lambda e: e.tensor_scalar(out=ls8[:].rearrange('p d j -> p (d j)'),
                                                  in0=ls8[:].rearrange('p d j -> p (d j)'), scalar1=8.0, scalar2=None,
                                                  op0=ALU.mult), reads=['ls8'], writes=['ls8'])
            xpre = sbx('l_xpre', [128, T])
            xc = sbx('l_xc', [128, T])
            xcb = sbx('l_xcb', [128, T], BF16)
            gy = sbx('l_gy', [128, T])
            a_t = sbx('l_a', [128, T])
            b_t = sbx('l_b', [128, T])
            hf = sbx('l_hf', [128, T])
            hb = sbx('l_hb', [128, T])
            ob = sbx('l_ob', [128, T], BF16)
            t1 = [sbx('l_t1%d' % i, [128, 512]) for i in range(2)]
            t2 = [sbx('l_t2%d' % i, [128, 512]) for i in range(2)]
            for j in range(4):
                w, wk = getw(('lru_x', j))
                for (t0, n) in TILES_M:
                    pt, pk = proj_fm(w, wk, t0, n)
                    S.op('act', lambda e, pt=pt, t0=t0, n=n: e.activation(out=xpre[:, t0:t0 + n], in_=pt[:, 0:n],
                                                                          func=AF.Identity),
                         reads=[pk], writes=[(tag, 'xpre')])
                emit_conv(C, tag, xpre, (tag, 'xpre'), xc, (tag, 'xc'), lambda tp, j=j: lrup[:, j, tp:tp + 1], 'lrup')
                S.op('act', lambda e: e.activation(out=xcb[:], in_=xc[:], func=AF.Identity),
                     reads=[(tag, 'xc')], writes=[(tag, 'xcb')])
                w, wk = getw(('lru_y', j))
                for ti, (t0, n) in enumerate(TILES_M):
                    b = ti % 2
                    pt, pk = proj_fm(w, wk, t0, n)
                    S.op('act', lambda e, pt=pt, b=b, n=n: e.activation(out=t1[b][:, 0:n], in_=pt[:, 0:n],
                                                                        func=AF.Identity),
                         reads=[pk], writes=[(tag, 't1', b)])
                    S.op('dve', lambda e, b=b, n=n: e.tensor_tensor(out=t2[b][:, 0:n], in0=t1[b][:, 0:n],
                                                                    in1=t1[b][:, 0:n], op=ALU.mult),
                         reads=[(tag, 't1', b)], writes=[(tag, 't2', b)])
                    S.op('dve', lambda e, b=b, n=n: e.tensor_scalar(out=t2[b][:, 0:n], in0=t2[b][:, 0:n],
                                                                    scalar1=0.044715, scalar2=1.0, op0=ALU.mult,
                                                                    op1=ALU.add),
                         reads=[(tag, 't2', b)], writes=[(tag, 't2', b)])
                    S.op('dve', lambda e, b=b, n=n: e.tensor_tensor(out=t2[b][:, 0:n], in0=t2[b][:, 0:n],
                                                                    in1=t1[b][:, 0:n], op=ALU.mult),
                         reads=[(tag, 't2', b), (tag, 't1', b)], writes=[(tag, 't2', b)])
                    S.op('act', lambda e, b=b, n=n: e.activation(out=t2[b][:, 0:n], in_=t2[b][:, 0:n],
                                                                 func=AF.Sigmoid, scale=1.5957691216057308),
                         reads=[(tag, 't2', b)], writes=[(tag, 't2', b)])
                    S.op('dve', lambda e, b=b, n=n, t0=t0: e.tensor_tensor(out=gy[:, t0:t0 + n], in0=t2[b][:, 0:n],
                                                                           in1=t1[b][:, 0:n], op=ALU.mult),
                         reads=[(tag, 't2', b), (tag, 't1', b)], writes=[(tag, 'gy')])
                for d in range(2):
                    for ti, (t0, n) in enumerate(TILES_M):
                        b = ti % 2
                        pr, prk = C.ps()
                        S.op('pe', lambda e, pr=pr, n=n, t0=t0: e.matmul(pr[:, 0:n], lruw[:, (0 * 2 + d) * 4 + j, :],
                                                                         xcb[:, t0:t0 + n], start=True, stop=True),
                             reads=['lruw', (tag, 'xcb')], writes=[prk])
                        S.op('act', lambda e, pr=pr, b=b, n=n: e.activation(out=t1[b][:, 0:n], in_=pr[:, 0:n],
                                                                            func=AF.Sigmoid, bias=gb[:, 0, d, j:j + 1],
                                                                            scale=1.0),
                             reads=[prk, 'lrugb'], writes=[(tag, 't1', b)])
                        S.op('act', lambda e, b=b, n=n, t0=t0: e.activation(out=a_t[:, t0:t0 + n], in_=t1[b][:, 0:n],
                                                                            func=AF.Exp, scale=ls8[:, d, j:j + 1]),
                             reads=[(tag, 't1', b), 'ls8'], writes=[(tag, 'a')])
                        pi_, pik = C.ps()
                        S.op('pe', lambda e, pi_=pi_, n=n, t0=t0: e.matmul(pi_[:, 0:n], lruw[:, (1 * 2 + d) * 4 + j, :],
                                                                           xcb[:, t0:t0 + n], start=True, stop=True),
                             reads=['lruw', (tag, 'xcb')], writes=[pik])
                        S.op('act', lambda e, pi_=pi_, b=b, n=n: e.activation(out=t2[b][:, 0:n], in_=pi_[:, 0:n],
                                                                              func=AF.Sigmoid,
                                                                              bias=gb[:, 1, d, j:j + 1], scale=1.0),
                             reads=[pik, 'lrugb'], writes=[(tag, 't2', b)])
                        S.op('dve', lambda e, b=b, n=n, t0=t0: e.tensor_tensor(out=t1[b][:, 0:n], in0=a_t[:, t0:t0 + n],
                                                                               in1=a_t[:, t0:t0 + n], op=ALU.mult),
                             reads=[(tag, 'a')], writes=[(tag, 't1', b)])
                        S.op('act', lambda e, b=b, n=n: e.activation(out=t1[b][:, 0:n], in_=t1[b][:, 0:n],
                                                                     func=AF.Sqrt, bias=oneb[:, 0:1], scale=-1.0),
                             reads=[(tag, 't1', b), 'oneb'], writes=[(tag, 't1', b)])
                        S.op('dve', lambda e, b=b, n=n, t0=t0: e.tensor_tensor(out=t2[b][:, 0:n], in0=t2[b][:, 0:n],
                                                                               in1=xc[:, t0:t0 + n], op=ALU.mult),
                             reads=[(tag, 't2', b), (tag, 'xc')], writes=[(tag, 't2', b)])
                        S.op('dve', lambda e, b=b, n=n, t0=t0: e.tensor_tensor(out=b_t[:, t0:t0 + n], in0=t2[b][:, 0:n],
                                                                               in1=t1[b][:, 0:n], op=ALU.mult),
                             reads=[(tag, 't2', b), (tag, 't1', b)], writes=[(tag, 'b')])
                    if d == 0:
                        S.op('dve', lambda e: e.tensor_tensor_scan(out=hf[:], data0=a_t[:], data1=b_t[:], initial=0.0,
                                                                   op0=ALU.mult, op1=ALU.add),
                             reads=[(tag, 'a'), (tag, 'b')], writes=[(tag, 'hf')])
                    else:
                        S.op('dve', lambda e: e.tensor_tensor_scan(
                            out=hb[:, NCTX - 1::-1], data0=a_t[:, NCTX - 1::-1], data1=b_t[:, NCTX - 1::-1],
                            initial=0.0, op0=ALU.mult, op1=ALU.add),
                             reads=[(tag, 'a'), (tag, 'b')], writes=[(tag, 'hb')])
                        S.op('dve', lambda e: e.tensor_tensor_scan(
                            out=hb[:, T - 1:NCTX - 1:-1], data0=a_t[:, T - 1:NCTX - 1:-1],
                            data1=b_t[:, T - 1:NCTX - 1:-1], initial=hb[:, 0:1], op0=ALU.mult, op1=ALU.add),
                             reads=[(tag, 'a'), (tag, 'b'), (tag, 'hb')], writes=[(tag, 'hb')])
                S.op('dve', lambda e: e.tensor_tensor(out=hf[:], in0=hf[:], in1=hb[:], op=ALU.add),
                     reads=[(tag, 'hf'), (tag, 'hb')], writes=[(tag, 'hf')])
                S.op('dve', lambda e: e.tensor_tensor(out=ob[:], in0=hf[:], in1=gy[:], op=ALU.mult),
                     reads=[(tag, 'hf'), (tag, 'gy')], writes=[(tag, 'ob')])
                S.dma('sp', brT[2, s * 512 + j * 128:s * 512 + (j + 1) * 128, :], ob[:], reads=[(tag, 'ob')],
                      writes=['brT'])
            barrier(C)

    if 'ssd' in which:
        scw_d = C.din('ssd_cw', [128, 8 * 5])
        swdt_d = C.din('ssd_wdt', [128, KC * 16])
        srow_d = C.din('ssd_row', [128, 2 * 16])
        sdsk_d = C.din('ssd_dsk', [128, 512])
        snw_d = C.din('ssd_nw', [128, 512])
        with ExitStack() as es1:
            def sbx(name, shape, dt=F32):
                return es1.enter_context(C.nc.sbuf_tensor(C.pfx + name, list(shape), dt))
            tag = 'ssd'
            scw = sbx('scw', [128, 8, 5])
            swdt = sbx('swdt', [128, KC, 16], BF16)
            srow = sbx('srow', [128, 2, 16])
            sdsk = sbx('sdsk', [128, 512])
            snw = sbx('snw', [128, 512])
            S.dma('sp', scw[:].rearrange('p a t -> p (a t)'), scw_d, writes=['scw'])
            S.dma('pool', swdt[:].rearrange('p k c -> p (k c)'), swdt_d, writes=['swdt'])
            S.dma('sp', srow[:].rearrange('p a c -> p (a c)'), srow_d, writes=['srow'])
            S.dma('sp', sdsk[:], sdsk_d, writes=['sdsk'])
            S.dma('sp', snw[:], snw_d, writes=['snw'])
            S.op('act', lambda e: e.activation(out=srow[:, 1, :], in_=srow[:, 1, :], func=AF.Exp),
                 reads=['srow'], writes=['srow'])
            S.op('dve', lambda e: e.tensor_scalar(out=srow[:, 1, :], in0=srow[:, 1, :], scalar1=-1.0, scalar2=None,
                                                  op0=ALU.mult), reads=['srow'], writes=['srow'])
            dt_tm = sbx('s_dt', [128, NCHK, 2, 8])
            la_tm = sbx('s_la', [128, NCHK, 2, 8])
            tdt = [sbx('s_tdt%d' % i, [128, 16]) for i in range(2)]
            for c in range(NCHK):
                b = c % 2
                pt, pk = proj_tm(lambda kc: swdt[:, kc, :], 'swdt', c, 16)
                S.op('dve', lambda e, pt=pt, b=b: e.tensor_tensor(out=tdt[b][:], in0=pt[:, 0:16], in1=srow[:, 0, :],
                                                                  op=ALU.add),
                     reads=[pk, 'srow'], writes=[(tag, 'tdt', b)])
                S.op('act', lambda e, b=b: e.activation(out=tdt[b][:], in_=tdt[b][:], func=AF.Exp),
                     reads=[(tag, 'tdt', b)], writes=[(tag, 'tdt', b)])
                S.op('act', lambda e, b=b, c=c: e.activation(out=dt_tm[:, c].rearrange('p d h -> p (d h)'),
                                                             in_=tdt[b][:], func=AF.Ln, bias=oneb[:, 0:1], scale=1.0),
                     reads=[(tag, 'tdt', b), 'oneb'], writes=[(tag, 'dt')])
                S.op('dve', lambda e, c=c: e.tensor_tensor(out=la_tm[:, c].rearrange('p d h -> p (d h)'),
                                                           in0=dt_tm[:, c].rearrange('p d h -> p (d h)'),
                                                           in1=srow[:, 1, :], op=ALU.mult),
                     reads=[(tag, 'dt'), 'srow'], writes=[(tag, 'la')])
            import os
            SSD_STOP = int(os.environ.get('SSD_STOP', '9'))
            xpre = sbx('s_xpre', [128, T])
            xcv = sbx('s_xcv', [128, T])
            BT = sbx('s_BT', [128, T], BF16)
            CT = sbx('s_CT', [128, T], BF16)
            xT = sbx('s_xT', [128, T], BF16)
            B_tm = sbx('s_Btm', [128, NCHK, 128], BF16)
            x_tm = sbx('s_xtm', [128, NCHK, 256], BF16)
            z_tm = sbx('s_ztm', [128, NCHK, 256], BF16)
            o_acc = sbx('s_oacc', [128, NCHK, 256])
            ob = sbx('s_ob', [128, NCHK, 256], BF16)
            ty = [sbx('s_ty%d' % i, [128, 256]) for i in range(2)]

            def conv_silu(name, cwi, dst, dkey):
                w, wk = getw(name)
                for (t0, n) in TILES_M:
                    pt, pk = proj_fm(w, wk, t0, n)
                    S.op('act', lambda e, pt=pt, t0=t0, n=n: e.activation(out=xpre[:, t0:t0 + n], in_=pt[:, 0:n],
                                                                          func=AF.Identity),
                         reads=[pk], writes=[(tag, 'xpre')])
                emit_conv(C, tag, xpre, (tag, 'xpre'), xcv, (tag, 'xcv'), lambda tp: scw[:, cwi, tp:tp + 1], 'scw')
                S.op('act', lambda e: e.activation(out=dst[:], in_=xcv[:], func=AF.Silu),
                     reads=[(tag, 'xcv')], writes=[dkey])

            for gi in range(2 if SSD_STOP > 0 else 0):
                conv_silu(('ssd_B', gi), 4 + gi, BT, (tag, 'kT'))
                conv_silu(('ssd_C', gi), 6 + gi, CT, (tag, 'qT'))
                for c in range(NCHK):
                    transpose_to(B_tm[:, c, :], (tag, 'ktm'), BT[:, c * 128:(c + 1) * 128], (tag, 'kT'))
                S.op('dve', lambda e: e.memset(o_acc[:], 0.0), writes=[(tag, 'o')])
                if SSD_STOP <= 1:
                    continue
                for pl_i in range(2):
                    pi = 2 * gi + pl_i
                    conv_silu(('ssd_x', pi), pi, xT, (tag, 'xT'))
                    for c in range(NCHK):
                        transpose_to(x_tm[:, c, pl_i * 128:(pl_i + 1) * 128], (tag, 'v'),
                                     xT[:, c * 128:(c + 1) * 128], (tag, 'xT'))
                    w, wk = getw(('ssd_z', pi))
                    for c in range(NCHK):
                        pt, pk = proj_tm(lambda kc, w=w: w[:, kc, :], wk, c, 128)
                        S.op('act', lambda e, c=c, pt=pt, pl_i=pl_i: e.activation(
                            out=z_tm[:, c, pl_i * 128:(pl_i + 1) * 128], in_=pt[:, 0:128], func=AF.Silu),
                             reads=[pk], writes=[(tag, 'z')])
                    hl0 = 2 * pi
                    heads = []
                    for e_ in range(2):
                        hl = hl0 + e_
                        heads.append(dict(
                            v0=pl_i * 128 + e_ * 64, dv=64,
                            la=[lambda c, hl=hl: la_tm[:, c, 0, hl:hl + 1], lambda c, hl=hl: la_tm[:, c, 1, hl:hl + 1]],
                            dt=[lambda c, hl=hl: dt_tm[:, c, 0, hl:hl + 1], lambda c, hl=hl: dt_tm[:, c, 1, hl:hl + 1]]))
                    if SSD_STOP <= 2:
                        continue
                    with ExitStack() as es2:
                        def sby(name, shape, dt=F32, pi=pi):
                            return es2.enter_context(C.nc.sbuf_tensor(C.pfx + name + '_%d' % pi, list(shape), dt))
                        scalar_scan(sby, tag, CT, BT, B_tm, x_tm, heads,
                                    lambda c, d, hl0=hl0: (la_tm[:, c, d, hl0:hl0 + 2], (tag, 'la')), o_acc, (tag, 'o'))
                        barrier(C)
                if SSD_STOP <= 3:
                    continue
                gc = slice(gi * 256, (gi + 1) * 256)
                for c in range(NCHK):
                    b = c % 2
                    S.op('dve', lambda e, c=c, b=b: e.tensor_tensor(out=ty[b][:], in0=x_tm[:, c, :], in1=sdsk[:, gc],
                                                                    op=ALU.mult),
                         reads=[(tag, 'v'), 'sdsk'], writes=[(tag, 'ty', b)])
                    S.op('dve', lambda e, c=c, b=b: e.tensor_tensor(out=ty[b][:], in0=ty[b][:], in1=o_acc[:, c, :],
                                                                    op=ALU.add),
                         reads=[(tag, 'ty', b), (tag, 'o')], writes=[(tag, 'ty', b)])
                    S.op('dve', lambda e, c=c, b=b: e.tensor_tensor(out=o_acc[:, c, :], in0=ty[b][:], in1=z_tm[:, c, :],
                                                                    op=ALU.mult),
                         reads=[(tag, 'ty', b), (tag, 'z'), (tag, 'o')], writes=[(tag, 'o')])
                if SSD_STOP <= 4:
                    continue
                with ExitStack() as es2:
                    def sby(name, shape, dt=F32, gi=gi):
                        return es2.enter_context(C.nc.sbuf_tensor(C.pfx + name + '_g%d' % gi, list(shape), dt))
                    rs, rsk = rms_rows(sby, tag, o_acc, (tag, 'o'), 256)
                    for c in range(NCHK):
                        S.op('dve', lambda e, c=c: e.scalar_tensor_tensor(
                            out=ob[:, c, :], in0=o_acc[:, c, :], scalar=rs[:, c:c + 1], in1=snw[:, gc],
                            op0=ALU.mult, op1=ALU.mult),
                             reads=[(tag, 'o'), rsk, 'snw'], writes=[(tag, 'ob')])
                    for hf_ in range(2):
                        c0_ = gi * 256 + hf_ * 128
                        out_tm(1, s * 512 + c0_, lambda c, hf_=hf_: ob[:, c, hf_ * 128:(hf_ + 1) * 128], (tag, 'ob'))
                    barrier(C)
            barrier(C)

    if 'hg' in which:
        hlbl_d = C.din('hg_lbl', [128, 8 * 4])
        hnw_d = C.din('hg_nw', [128, 512])
        with ExitStack() as es1:
            def sbx(name, shape, dt=F32):
                return es1.enter_context(C.nc.sbuf_tensor(C.pfx + name, list(shape), dt))
            tag = 'hg'
            lbl = sbx('h_lbl', [128, 8, 4])
            hnw = sbx('h_nw', [128, 512])
            lbs = sbx('h_lbs', [128, 8, 4])
            S.dma('sp', lbl[:].rearrange('p a l -> p (a l)'), hlbl_d, writes=['hlbl'])
            S.dma('sp', hnw[:], hnw_d, writes=['hnw'])
            for a_ in range(8):
                S.op('dve', lambda e, a_=a_: e.tensor_reduce(out=lbs[:, a_, 0:1], in_=lbl[:, a_, :], axis=AX.X,
                                                             op=ALU.max), reads=['hlbl'], writes=['hlbs'])
                S.op('dve', lambda e, a_=a_: e.tensor_scalar(out=lbl[:, a_, :], in0=lbl[:, a_, :],
                                                             scalar1=lbs[:, a_, 0:1], scalar2=None, op0=ALU.subtract),
                     reads=['hlbl', 'hlbs'], writes=['hlbl'])
            S.op('act', lambda e: e.activation(out=lbl[:].rearrange('p a l -> p (a l)'),
                                               in_=lbl[:].rearrange('p a l -> p (a l)'), func=AF.Exp),
                 reads=['hlbl'], writes=['hlbl'])
            for a_ in range(8):
                S.op('dve', lambda e, a_=a_: e.tensor_reduce(out=lbs[:, a_, 0:1], in_=lbl[:, a_, :], axis=AX.X,
                                                             op=ALU.add), reads=['hlbl'], writes=['hlbs'])
                S.op('dve', lambda e, a_=a_: e.reciprocal(out=lbs[:, a_, 0:1], in_=lbs[:, a_, 0:1]),
                     reads=['hlbs'], writes=['hlbs'])
                if layer == 0:
                    S.op('dve', lambda e, a_=a_: e.memset(lbs[:, a_, 1:2], 0.0), reads=['hlbs'], writes=['hlbs'])
                else:
                    S.op('dve', lambda e, a_=a_: e.tensor_reduce(out=lbs[:, a_, 1:2], in_=lbl[:, a_, 1:layer + 1],
                                                                 axis=AX.X, op=ALU.add),
                         reads=['hlbl', 'hlbs'], writes=['hlbs'])
                S.op('dve', lambda e, a_=a_: e.tensor_tensor(out=lbs[:, a_, 2:3], in0=lbs[:, a_, 1:2],
                                                             in1=lbs[:, a_, 0:1], op=ALU.mult),
                     reads=['hlbs'], writes=['hlbs'])
                S.op('dve', lambda e, a_=a_: e.tensor_scalar(out=lbs[:, a_, 3:4], in0=lbs[:, a_, 2:3], scalar1=-1.0,
                                                             scalar2=1.0, op0=ALU.mult, op1=ALU.add),
                     reads=['hlbs'], writes=['hlbs'])
            qT = sbx('h_qT', [128, T], BF16)
            la_d = [sbx('h_la%d' % d, [128, T]) for d in range(2)]
            kd = [sbx('h_kd%d' % d, [128, T], BF16) for d in range(2)]
            v_tm = sbx('h_vtm', [128, NCHK, 128], BF16)
            g_tm = sbx('h_gtm', [128, NCHK, 128], BF16)
            o_acc = sbx('h_oacc', [128, NCHK, 128])
            ob = sbx('h_ob', [128, NCHK, 128], BF16)
            tf = [sbx('h_tf%d' % i, [128, 512]) for i in range(2)]
            Sst = sbx('h_S', [128, 128])
            Sbf = sbx('h_Sb', [128, 128], BF16)
            cum = [sbx('h_cum%d' % i, [128, 128]) for i in range(2)]
            e1 = [sbx('h_e1%d' % i, [128, 128]) for i in range(2)]
            e2 = [sbx('h_e2%d' % i, [128, 128]) for i in range(2)]
            e3 = [sbx('h_e3%d' % i, [128, 128]) for i in range(2)]
            e4 = [sbx('h_e4%d' % i, [128, 128]) for i in range(2)]
            qt = [sbx('h_qt%d' % i, [128, 128], BF16) for i in range(2)]
            kt = [sbx('h_kt%d' % i, [128, 128], BF16) for i in range(2)]
            qh = [sbx('h_qh%d' % i, [128, 128], BF16) for i in range(2)]
            kh = [sbx('h_kh%d' % i, [128, 128], BF16) for i in range(2)]
            khT = [sbx('h_khT%d' % i, [128, 128], BF16) for i in range(2)]
            Pm = [sbx('h_P%d' % i, [128, 128], BF16) for i in range(2)]
            at = [sbx('h_at%d' % i, [128, 1]) for i in range(2)]
            for j in range(4):
                w, wk = getw(('hg_q', j))
                for (t0, n) in TILES_M:
                    pt, pk = proj_fm(w, wk, t0, n)
                    S.op('act', lambda e, pt=pt, t0=t0, n=n: e.activation(out=qT[:, t0:t0 + n], in_=pt[:, 0:n],
                                                                          func=AF.Silu),
                         reads=[pk], writes=[(tag, 'qT')])
                for d in range(2):
                    a_ = d * 4 + j
                    w, wk = getw(('hg_f%d' % d, j))
                    for ti, (t0, n) in enumerate(TILES_M):
                        b = ti % 2
                        pt, pk = proj_fm(w, wk, t0, n)
                        S.op('act', lambda e, pt=pt, b=b, n=n: e.activation(out=tf[b][:, 0:n], in_=pt[:, 0:n],
                                                                            func=AF.Sigmoid),
                             reads=[pk], writes=[(tag, 'tf', b)])
                        S.op('dve', lambda e, b=b, n=n, a_=a_: e.tensor_scalar(
                            out=tf[b][:, 0:n], in0=tf[b][:, 0:n], scalar1=lbs[:, a_, 3:4], scalar2=lbs[:, a_, 2:3],
                            op0=ALU.mult, op1=ALU.add), reads=[(tag, 'tf', b), 'hlbs'], writes=[(tag, 'tf', b)])
                        S.op('dve', lambda e, b=b, n=n, t0=t0, d=d: e.tensor_scalar(
                            out=kd[d][:, t0:t0 + n], in0=tf[b][:, 0:n], scalar1=-1.0, scalar2=1.0,
                            op0=ALU.mult, op1=ALU.add), reads=[(tag, 'tf', b)], writes=[(tag, 'kd', d)])
                        S.op('dve', lambda e, b=b, n=n: e.tensor_scalar(
                            out=tf[b][:, 0:n], in0=tf[b][:, 0:n], scalar1=1e-18, scalar2=None, op0=ALU.max),
                             reads=[(tag, 'tf', b)], writes=[(tag, 'tf', b)])
                        S.op('act', lambda e, b=b, n=n, t0=t0, d=d: e.activation(out=la_d[d][:, t0:t0 + n],
                                                                                 in_=tf[b][:, 0:n], func=AF.Ln),
                             reads=[(tag, 'tf', b)], writes=[(tag, 'la', d)])
                w, wk = getw(('hg_i', j))
                for c in range(NCHK):
                    pt, pk = proj_tm(lambda kc, w=w: w[:, kc, :], wk, c, 128)
                    S.op('act', lambda e, c=c, pt=pt: e.activation(out=v_tm[:, c, :], in_=pt[:, 0:128],
                                                                   func=AF.Identity),
                         reads=[pk], writes=[(tag, 'v')])
                w, wk = getw(('hg_g', j))
                for c in range(NCHK):
                    pt, pk = proj_tm(lambda kc, w=w: w[:, kc, :], wk, c, 128)
                    S.op('act', lambda e, c=c, pt=pt: e.activation(out=g_tm[:, c, :], in_=pt[:, 0:128], func=AF.Silu),
                         reads=[pk], writes=[(tag, 'g')])
                S.op('dve', lambda e: e.memset(o_acc[:], 0.0), writes=[(tag, 'o')])
                it = 0
                for d, order in ((0, FWD_ORDER), (1, BWD_ORDER)):
                    msk = M_le if d == 0 else M_ge
                    last = 127 if d == 0 else 0
                    S.op('dve', lambda e: e.memset(Sst[:], 0.0), writes=[(tag, 'S')])
                    S.op('act', lambda e: e.activation(out=Sbf[:], in_=Sst[:], func=AF.Identity),
                         reads=[(tag, 'S')], writes=[(tag, 'Sb')])
                    for c in order:
                        i2 = it % 2
                        it += 1
                        c0, c1 = c * 128, (c + 1) * 128
                        if d == 0:
                            src = la_d[d][:, c0:c1]
                            dst = cum[i2][:]
                        else:
                            src = la_d[d][:, c1 - 1:(c0 - 1 if c0 > 0 else None):-1]
                            dst = cum[i2][:, ::-1]
                        S.op('dve', lambda e, src=src, dst=dst: e.tensor_tensor_scan(
                            out=dst, data0=C.ones[:], data1=src, initial=0.0, op0=ALU.mult, op1=ALU.add),
                             reads=[(tag, 'la', d), 'ones'], writes=[(tag, 'cum', i2)])
                        ck = (tag, 'cum', i2)
                        cm = cum[i2]
                        SB = 64
                        NSB = 128 // SB

                        def refcol(I, cm=cm):
                            if d == 0:
                                return cm[:, I * SB - 1:I * SB] if I > 0 else 0.0
                            return cm[:, (I + 1) * SB:(I + 1) * SB + 1] if I < NSB - 1 else 0.0
                        for I in range(NSB):
                            S.op('dve', lambda e, cm=cm, i2=i2, I=I: e.tensor_scalar(
                                out=e1[i2][:, I * SB:(I + 1) * SB], in0=cm[:, I * SB:(I + 1) * SB], scalar1=refcol(I),
                                scalar2=None, op0=ALU.subtract), reads=[ck], writes=[(tag, 'e1', i2)])
                        S.op('act', lambda e, i2=i2: e.activation(out=e1[i2][:], in_=e1[i2][:], func=AF.Exp),
                             reads=[(tag, 'e1', i2)], writes=[(tag, 'e1', i2)])
                        S.op('dve', lambda e, i2=i2: e.tensor_tensor(out=qt[i2][:], in0=qT[:, c0:c1], in1=e1[i2][:],
                                                                     op=ALU.mult),
                             reads=[(tag, 'qT'), (tag, 'e1', i2)], writes=[(tag, 'qt', i2)])
                        psc, psck = C.psb[6], ('ps', 6)
                        for I in range(NSB):
                            ik = I % 2
                            S.op('dve', lambda e, cm=cm, ik=ik, I=I: e.tensor_scalar(
                                out=e2[ik][:], in0=cm[:], scalar1=refcol(I), scalar2=-80.0, op0=ALU.subtract,
                                op1=ALU.max), reads=[ck], writes=[(tag, 'e2', ik)])
                            S.op('act', lambda e, ik=ik: e.activation(out=e2[ik][:], in_=e2[ik][:], func=AF.Exp,
                                                                      scale=-1.0),
                                 reads=[(tag, 'e2', ik)], writes=[(tag, 'e2', ik)])
                            S.op('dve', lambda e, ik=ik, d=d: e.tensor_tensor(out=kt[ik][:], in0=kd[d][:, c0:c1],
                                                                              in1=e2[ik][:], op=ALU.mult),
                                 reads=[(tag, 'kd', d), (tag, 'e2', ik)], writes=[(tag, 'kt', ik)])
                            S.op('pe', lambda e, ik=ik, i2=i2, psc=psc, I=I: e.matmul(
                                psc[:, I * SB:(I + 1) * SB], kt[ik][:], qt[i2][:, I * SB:(I + 1) * SB],
                                start=True, stop=True),
                                 reads=[(tag, 'kt', ik), (tag, 'qt', i2)], writes=[psck])
                        S.op('dve', lambda e, i2=i2, psc=psc: e.tensor_tensor(out=Pm[i2][:], in0=psc[:, 0:128],
                                                                              in1=msk, op=ALU.mult),
                             reads=[psck, 'masks'], writes=[(tag, 'P', i2)])
                        S.op('act', lambda e, cm=cm, i2=i2: e.activation(out=e3[i2][:], in_=cm[:], func=AF.Exp),
                             reads=[ck], writes=[(tag, 'e3', i2)])
                        S.op('dve', lambda e, i2=i2: e.tensor_tensor(out=qh[i2][:], in0=qT[:, c0:c1], in1=e3[i2][:],
                                                                     op=ALU.mult),
                             reads=[(tag, 'qT'), (tag, 'e3', i2)], writes=[(tag, 'qh', i2)])
                        po, pok = C.ps()
                        S.op('pe', lambda e, i2=i2, po=po, c=c: e.matmul(po[:, 0:128], Pm[i2][:], v_tm[:, c, :],
                                                                         start=True, stop=False),
                             reads=[(tag, 'P', i2), (tag, 'v')], writes=[pok])
                        S.op('pe', lambda e, i2=i2, po=po: e.matmul(po[:, 0:128], qh[i2][:], Sbf[:],
                                                                    start=False, stop=True),
                             reads=[(tag, 'qh', i2), (tag, 'Sb')], writes=[pok])
                        S.op('dve', lambda e, po=po, c=c: e.tensor_tensor(out=o_acc[:, c, :], in0=po[:, 0:128],
                                                                          in1=o_acc[:, c, :], op=ALU.add),
                             reads=[pok, (tag, 'o')], writes=[(tag, 'o')])
                        S.op('dve', lambda e, cm=cm, i2=i2: e.tensor_scalar(
                            out=e4[i2][:], in0=cm[:], scalar1=cm[:, last:last + 1], scalar2=None, op0=ALU.subtract),
                             reads=[ck], writes=[(tag, 'e4', i2)])
                        S.op('act', lambda e, i2=i2: e.activation(out=e4[i2][:], in_=e4[i2][:], func=AF.Exp,
                                                                  scale=-1.0),
                             reads=[(tag, 'e4', i2)], writes=[(tag, 'e4', i2)])
                        S.op('dve', lambda e, i2=i2, d=d: e.tensor_tensor(out=kh[i2][:], in0=kd[d][:, c0:c1],
                                                                          in1=e4[i2][:], op=ALU.mult),
                             reads=[(tag, 'kd', d), (tag, 'e4', i2)], writes=[(tag, 'kh', i2)])
                        transpose_to(khT[i2][:], (tag, 'khT', i2), kh[i2][:], (tag, 'kh', i2))
                        pc_, pck = C.ps()
                        S.op('pe', lambda e, i2=i2, pc_=pc_, c=c: e.matmul(pc_[:, 0:128], khT[i2][:], v_tm[:, c, :],
                                                                           start=True, stop=True),
                             reads=[(tag, 'khT', i2), (tag, 'v')], writes=[pck])
                        S.op('act', lambda e, cm=cm, i2=i2: e.activation(out=at[i2][:], in_=cm[:, last:last + 1],
                                                                         func=AF.Exp),
                             reads=[ck], writes=[(tag, 'at', i2)])
                        S.op('dve', lambda e, i2=i2, pc_=pc_: e.scalar_tensor_tensor(
                            out=Sst[:], in0=Sst[:], scalar=at[i2][:, 0:1], in1=pc_[:, 0:128], op0=ALU.mult,
                            op1=ALU.add), reads=[(tag, 'S'), (tag, 'at', i2), pck], writes=[(tag, 'S')])
                        S.op('act', lambda e: e.activation(out=Sbf[:], in_=Sst[:], func=AF.Identity),
                             reads=[(tag, 'S')], writes=[(tag, 'Sb')])
                with ExitStack() as es2:
                    def sby(name, shape, dt=F32, j=j):
                        return es2.enter_context(C.nc.sbuf_tensor(C.pfx + name + '_%d' % j, list(shape), dt))
                    rs, rsk = rms_rows(sby, tag, o_acc, (tag, 'o'), 128)
                    for c in range(NCHK):
                        S.op('dve', lambda e, c=c: e.scalar_tensor_tensor(
                            out=o_acc[:, c, :], in0=o_acc[:, c, :], scalar=rs[:, c:c + 1],
                            in1=hnw[:, j * 128:(j + 1) * 128], op0=ALU.mult, op1=ALU.mult),
                             reads=[(tag, 'o'), rsk, 'hnw'], writes=[(tag, 'o')])
                        S.op('dve', lambda e, c=c: e.tensor_tensor(out=ob[:, c, :], in0=o_acc[:, c, :],
                                                                   in1=g_tm[:, c, :], op=ALU.mult),
                             reads=[(tag, 'o'), (tag, 'g')], writes=[(tag, 'ob')])
                    out_tm(3, s * 512 + j * 128, lambda c: ob[:, c, :], (tag, 'ob'))
                    barrier(C)
            barrier(C)
    barrier(C)
    C.pop()


def barrier(C):
    S = C.S
    evs = []
    for e in S.csem:
        if S.ccnt[e] > 0:
            evs.append(('c_' + e, S.csem[e], S.ccnt[e], 'x'))
    for q in S.dsem:
        for i in range(S.ring):
            if S.dcnt[q][i] > 0:
                evs.append(('d_%s%d' % (q, i), S.dsem[q][i], S.dcnt[q][i], 'dma'))
    for e in ('pe', 'act', 'dve', 'pool', 'sp'):
        for ev in evs:
            S._ensure(e, ev)


def _const_tables():
    a = np.arange(128)
    le = (a[:, None] <= a[None, :]).astype(np.float32)
    ge = (a[:, None] >= a[None, :]).astype(np.float32)
    gt = (a[:, None] > a[None, :]).astype(np.float32)
    lt = (a[:, None] < a[None, :]).astype(np.float32)
    masks = np.concatenate([le, ge, gt, lt], axis=1)
    pmat = np.zeros((128, 128), np.float32)
    for m in range(64):
        pmat[m + 64, m] = -1.0
        pmat[m, m + 64] = 1.0
    t = np.arange(NLAT)
    r, col = (t // 64).astype(np.float32), (t % 64).astype(np.float32)
    freqs = (np.float32(10000.0) ** (-np.arange(32, dtype=np.float32) / np.float32(32))).astype(np.float32)
    ang = np.concatenate([r[:, None] * freqs[None], col[:, None] * freqs[None]], axis=1).astype(np.float32)
    cos = np.ones((T, 64), np.float32)
    sin = np.zeros((T, 64), np.float32)
    cos[NCTX:] = np.cos(ang)
    sin[NCTX:] = np.sin(ang)
    rope = np.zeros((128, 2, T), np.float32)
    rope[:, 0, :] = np.concatenate([cos.T, cos.T], axis=0)
    rope[:, 1, :] = np.concatenate([sin.T, sin.T], axis=0)
    lg = np.log1p(-np.exp2(-5.0 - np.arange(8, dtype=np.float32))).astype(np.float32)
    return masks, pmat, rope.reshape(128, 2 * T), lg


_CT = _const_tables()
IDENTB = np.eye(128, dtype=np.float32).astype(ml_dtypes.bfloat16)


def lay_wM(w_in_l, s):
    out = np.empty((NCH_M, 128, KC * 128), np.float32)
    for i, (nm, c0) in enumerate(mixer_chunks(s)):
        out[i] = w_in_l[:, c0:c0 + 128].reshape(KC, 128, 128).transpose(1, 0, 2).reshape(128, KC * 128)
    return out


def mixer_in_map(layer, s, inp):
    which = ('ret', 'ssd', 'lru', 'hg')
    masks, pmat, rope, lg = _CT
    m = {'n1w': lay_vec(inp['norm1_w'][layer]), 'wM': lay_wM(inp['w_in'][layer], s)}
    m['retla'] = np.ascontiguousarray(np.broadcast_to(lg[4 * s:4 * s + 4][None, :], (128, 4)))
    if 'ssd' in which:
        cw = inp['ssd_conv_w'][layer]
        cb = inp['ssd_conv_b'][layer]
        ch0 = [(4 * s + pi) * 128 for pi in range(4)] + [1024 + (2 * s + gi) * 128 for gi in range(2)] + \
              [1536 + (2 * s + gi) * 128 for gi in range(2)]
        p = np.zeros((128, 8, 5), np.float32)
        for i, c0 in enumerate(ch0):
            p[:, i, 0:4] = cw[:, c0:c0 + 128].T
            p[:, i, 4] = cb[c0:c0 + 128]
        m['ssd_cw'] = p.reshape(128, 40)
        cols = np.concatenate([OFF['ssd_dt'] + d * 16 + 8 * s + np.arange(8) for d in range(2)])
        wdt = inp['w_in'][layer][:, cols]
        m['ssd_wdt'] = np.ascontiguousarray(wdt.reshape(KC, 128, 16).transpose(1, 0, 2).reshape(128, KC * 16))
        row = np.concatenate([inp['ssd_dt_bias'][layer][:, 8 * s:8 * s + 8].reshape(-1),
                              inp['ssd_a_log'][layer][:, 8 * s:8 * s + 8].reshape(-1)])
        m['ssd_row'] = np.ascontiguousarray(np.broadcast_to(row[None, :], (128, 32)))
        dsk = np.repeat(inp['ssd_d'][layer][8 * s:8 * s + 8], 64)
        m['ssd_dsk'] = np.ascontiguousarray(np.broadcast_to(dsk[None, :], (128, 512)))
        m['ssd_nw'] = np.ascontiguousarray(np.broadcast_to(inp['ssd_norm_w'][layer][512 * s:512 * s + 512][None, :], (128, 512)))
    if 'hg' in which:
        lg_ = inp['hg_lb_logits']
        a = np.zeros((128, 2, 4, DEPTH), np.float32)
        for d in range(2):
            for j in range(4):
                hh = 4 * s + j
                a[:, d, j, :] = lg_[d, :, hh * 128:(hh + 1) * 128].T
        m['hg_lbl'] = a.reshape(128, 32)
        m['hg_nw'] = np.ascontiguousarray(np.broadcast_to(inp['hg_norm_w'][layer][512 * s:512 * s + 512][None, :], (128, 512)))
    if 'lru' in which:
        blk = [4 * s + j for j in range(4)]
        cw = inp['lru_conv_w'][layer]
        cb = inp['lru_conv_b'][layer]
        p = np.zeros((128, 4, 5), np.float32)
        for j, g in enumerate(blk):
            p[:, j, 0:4] = cw[:, g * 128:(g + 1) * 128].T
            p[:, j, 4] = cb[g * 128:(g + 1) * 128]
        m['lru_p'] = p.reshape(128, 20)
        gbv = np.zeros((128, 2, 2, 4), np.float32)
        lw = np.zeros((128, 2, 2, 4, 128), np.float32)
        lam = np.zeros((128, 2, 4), np.float32)
        for d in range(2):
            for j, g in enumerate(blk):
                gbv[:, 0, d, j] = inp['lru_ba'][layer][d, g * 128:(g + 1) * 128]
                gbv[:, 1, d, j] = inp['lru_bx'][layer][d, g * 128:(g + 1) * 128]
                lw[:, 0, d, j, :] = inp['lru_wa'][layer][d, g]
                lw[:, 1, d, j, :] = inp['lru_wx'][layer][d, g]
                lam[:, d, j] = inp['lru_lambda'][layer][d, g * 128:(g + 1) * 128]
        m['lru_gb'] = gbv.reshape(128, 16)
        m['lru_w'] = lw.reshape(128, 16 * 128)
        m['lru_lam'] = lam.reshape(128, 8)
    return m


def build_fused(layers, dbg=False):
    C = Ctx('fused')
    emit_consts(C)
    emit_eps(C)
    modd = emit_ada(C, layers)
    C.pfx = ''
    hT0 = C.din('hT0', [D, T], shared=True)
    hS = C.dint('hS', [D, T])
    brT = C.dint('brT', [4, MIXW, T], BF16)
    yout = C.dout('yout', [D, T])
    hdbg = C.dout('hdbg', [D, T]) if dbg else None
    h_src, hkey = hT0, 'hT0'
    for li, l in enumerate(layers):
        C.push()
        C.pfx = 'L%dm_' % l
        xst = {'xnT': C.sb('xnT', [128, KC, T], BF16)}
        for s in (0, 1):
            emit_mixer(C, l, li, s, h_src, hkey, brT, modd, xst)
        C.pop()
        final = (li == len(layers) - 1)
        emit_token(C, l, li, l % 2 == 1, final, h_src, hkey, hS, 'hS', brT, modd,
                   yout if final else None, hdbg if final else None)
        h_src, hkey = hS, 'hS'
    names = list(C.inputs.keys())
    print('fused program: ninst', C.S.ninst, 'nwait', C.S.nwait)
    return C.close(), names


def fused_in_maps(inp, layers, names):
    masks, pmat, rope, lg = _CT
    sh = {'ident': IDENT, 'masks': masks, 'identb': IDENTB, 'rope': rope, 'pmat': pmat}
    sh['ada_w'] = np.ascontiguousarray(inp['ada_w'][list(layers)])
    sh['ada_b'] = np.ascontiguousarray(np.stack([inp['ada_b'][l].reshape(96, 128).T for l in layers]))
    for li, l in enumerate(layers):
        for s in (0, 1):
            for k, v in mixer_in_map(l, s, inp).items():
                sh['L%ds%d_%s' % (l, s, k)] = v
        for k, v in token_in_map(l, inp, l % 2 == 1, li == len(layers) - 1).items():
            sh['L%d_%s' % (l, k)] = v
    in_maps = []
    for i in range(8):
        b = i % NB
        cond2 = np.stack([inp['c'][b], inp['c_ctx']], axis=0)
        m = {k: sh[k] for k in names if k in sh}
        m['ada_cond'] = np.ascontiguousarray(cond2.T.reshape(KC, 128, 2).transpose(1, 0, 2).reshape(128, KC * 2))
        m['hT0'] = np.ascontiguousarray(np.concatenate([inp['ctx'][b], inp['x'][b]], axis=0).T)
        missing = [k for k in names if k not in m]
        assert not missing, missing
        in_maps.append(m)
    return in_maps


def kernel(**inputs):
    inp = {k: np.asarray(v) for k, v in inputs.items()}
    layers = list(range(DEPTH))
    nc, names = build_fused(layers)
    res = run(nc, fused_in_maps(inp, layers, names))
    out = np.stack([np.ascontiguousarray(res[b]['yout'][:, NCTX:].T) for b in range(NB)], axis=0)
    return out.astype(np.float32)
```
